# Optimizing a Trainium2 kernel written in Bass

```python
import math
import jax, jax.numpy as jnp
from jax import lax
import numpy as np

D_MODEL = 1024
BATCH = 8
SEQ = 4096
DEPTH = 2

HG_HEADS = 4
HG_DK = 128
HG_DV = 128
HG_WIDTH = HG_HEADS * HG_DV
HG_CHUNK = 64
DA_HEADS = 4
DA_HEAD_DIM = 64
DA_V_DIM = 2 * DA_HEAD_DIM
DA_WIDTH = DA_HEADS * DA_V_DIM
Q_BLOCK = 128
D_FF = 2816
N_EXPERTS = 8
TOP_K = 2
D_FF_EXPERT = 3584
N_DENSE = (DEPTH + 1) // 2
N_MOE = DEPTH // 2
ADA_CHUNKS = 6
EPS = 1e-6
IN_SIZES = (HG_HEADS * HG_DK, HG_HEADS * HG_DK, HG_WIDTH, HG_WIDTH,
            DA_HEADS * 2 * DA_HEAD_DIM, DA_HEADS * 2 * DA_HEAD_DIM, DA_WIDTH,
            D_MODEL, D_MODEL)
D_IN = sum(IN_SIZES)

kernel_name = "hybrid_hgrn2_diffattn_moe_adaln"


def rmsnorm(x, g):
    xf = x.astype(jnp.float32)
    y = xf * lax.rsqrt(jnp.mean(xf * xf, axis=-1, keepdims=True) + EPS)
    return y * g.astype(jnp.float32)


def split_points():
    return [int(v) for v in np.cumsum(np.array(IN_SIZES))[:-1]]


def alibi_slopes(n_heads):
    return jnp.asarray(2.0 ** (-8.0 * np.arange(1, n_heads + 1) / n_heads), dtype=jnp.float32)


def hgrn_lower_bounds(lb_logits):
    p = jax.nn.softmax(lb_logits.astype(jnp.float32), axis=0)
    cum = jnp.cumsum(p, axis=0)
    return cum - cum[0:1]


def hgrn2_chunk_scan(q, k, v, log_f):
    B, H, S, dk = q.shape
    dv = v.shape[-1]
    L = HG_CHUNK
    nc = S // L

    def to_chunks(t):
        return jnp.moveaxis(t.reshape(B, H, nc, L, t.shape[-1]), 2, 0)

    causal = jnp.tril(jnp.ones((L, L), dtype=bool))

    def step(state, inp):
        qb, kb, vb, gb = inp
        b = jnp.cumsum(gb, axis=2)
        o_inter = jnp.einsum('bhtk,bhkv->bhtv', qb * jnp.exp(b), state)
        rel = b[:, :, :, None, :] - b[:, :, None, :, :]
        decay = jnp.exp(jnp.where(causal[:, :, None], rel, -jnp.inf))
        scores = jnp.einsum('bhtk,bhsk,bhtsk->bhts', qb, kb, decay)
        o_intra = jnp.einsum('bhts,bhsv->bhtv', scores, vb)
        b_last = b[:, :, -1:, :]
        k_dec = kb * jnp.exp(b_last - b)
        new_state = jnp.exp(b_last[:, :, 0, :])[..., None] * state + jnp.einsum('bhsk,bhsv->bhkv', k_dec, vb)
        return new_state, o_inter + o_intra

    s0 = jnp.zeros((B, H, dk, dv), jnp.float32)
    _, o = lax.scan(step, s0, (to_chunks(q), to_chunks(k), to_chunks(v), to_chunks(log_f)))
    return jnp.moveaxis(o, 0, 2).reshape(B, H, S, dv)


def diff_attention(q, k, v, lam):
    B, H, _, S, d = q.shape
    dv = v.shape[-1]
    nb = S // Q_BLOCK
    scale = d ** -0.5
    slopes = alibi_slopes(H)
    kpos = jnp.arange(S)
    q_blocks = jnp.moveaxis(q.reshape(B, H, 2, nb, Q_BLOCK, d), 3, 0)

    def block(args):
        qb, bi = args
        qpos = bi * Q_BLOCK + jnp.arange(Q_BLOCK)
        dist = qpos[:, None] - kpos[None, :]
        s = jnp.einsum('bhcqd,bhcsd->bhcqs', qb, k) * scale
        s = s - slopes[None, :, None, None, None] * dist.astype(jnp.float32)
        s = jnp.where(dist >= 0, s, -jnp.inf)
        p = jax.nn.softmax(s, axis=-1)
        w = p[:, :, 0] - lam * p[:, :, 1]
        return jnp.einsum('bhqs,bhsv->bhqv', w, v)

    o = lax.map(block, (q_blocks, jnp.arange(nb)))
    return jnp.moveaxis(o, 0, 2).reshape(B, H, S, dv)


def token_mixer(h, layer, w_in, lb, hg_norm_g, qn_g, kn_g, lam_vecs, subln_g, w_pa, w_pb, w_o):
    B, S, _ = h.shape
    dt = h.dtype
    proj = h @ w_in
    hq, hf, hi, hg, dq, dk, dvv, ga, gb = jnp.split(proj, split_points(), axis=-1)

    def heads(t, d):
        return t.reshape(B, S, -1, d).transpose(0, 2, 1, 3).astype(jnp.float32)
    z = heads(hf, HG_DK)
    lbh = lb.reshape(HG_HEADS, 1, HG_DK)
    log_f = jnp.logaddexp(jnp.log(lbh), jnp.log1p(-lbh) + jax.nn.log_sigmoid(z))
    k_in = (1.0 - lbh) * jax.nn.sigmoid(-z)
    o_hg = hgrn2_chunk_scan(heads(hq, HG_DK), k_in, heads(hi, HG_DV), log_f)
    o_hg = rmsnorm(o_hg, hg_norm_g).transpose(0, 2, 1, 3).reshape(B, S, HG_WIDTH)
    o_hg = (o_hg * jax.nn.silu(hg.astype(jnp.float32))).astype(dt)

    q = dq.reshape(B, S, DA_HEADS, 2, DA_HEAD_DIM).transpose(0, 2, 3, 1, 4)
    k = dk.reshape(B, S, DA_HEADS, 2, DA_HEAD_DIM).transpose(0, 2, 3, 1, 4)
    v = dvv.reshape(B, S, DA_HEADS, DA_V_DIM).transpose(0, 2, 1, 3).astype(jnp.float32)
    q = rmsnorm(q, qn_g)
    k = rmsnorm(k, kn_g)
    lam_init = 0.8 - 0.6 * math.exp(-0.3 * layer)
    lv = lam_vecs.astype(jnp.float32)
    lam = jnp.exp(jnp.sum(lv[0] * lv[1])) - jnp.exp(jnp.sum(lv[2] * lv[3])) + lam_init
    o_da = diff_attention(q, k, v, lam)
    o_da = (rmsnorm(o_da, subln_g) * (1.0 - lam_init)).transpose(0, 2, 1, 3).reshape(B, S, DA_WIDTH).astype(dt)

    y = jax.nn.sigmoid(ga) * (o_hg @ w_pa) + jax.nn.sigmoid(gb) * (o_da @ w_pb)
    return y @ w_o


def swiglu(h, w1, w3, w2):
    return (jax.nn.silu(h @ w1) * (h @ w3)) @ w2


def moe_swiglu(h, router, w1, w3, w2):
    B, S, D = h.shape
    t = h.reshape(B * S, D)
    logits = (t @ router).astype(jnp.float32)
    vals, idx = lax.top_k(logits, TOP_K)
    wts = jax.nn.softmax(vals, axis=-1)
    comb = jnp.sum(jax.nn.one_hot(idx, N_EXPERTS, dtype=jnp.float32) * wts[..., None], axis=1)
    out = jnp.zeros_like(t)
    for e in range(N_EXPERTS):
        out = out + comb[:, e:e + 1].astype(t.dtype) * swiglu(t, w1[e], w3[e], w2[e])
    return out.reshape(B, S, D)


def setup_inputs(seed: int = 0) -> dict:
    key = jax.random.key(seed)
    ks = jax.random.split(key, 24)
    f32 = jnp.float32
    D = D_MODEL

    def nrm(k, shape, scale):
        return jax.random.normal(k, shape, f32) * scale

    def gain(k, shape):
        return 1.0 + 0.05 * jax.random.normal(k, shape, f32)

    return {
        "x": nrm(ks[0], (BATCH, SEQ, D), 1.0),
        "c": nrm(ks[1], (BATCH, D), 1.0),
        "ada_w": nrm(ks[2], (DEPTH, D, ADA_CHUNKS * D), 0.5 * D ** -0.5),
        "ada_b": nrm(ks[3], (DEPTH, ADA_CHUNKS * D), 0.02),
        "norm_mix_g": gain(ks[4], (DEPTH, D)),
        "norm_ffn_g": gain(ks[5], (DEPTH, D)),
        "w_in": nrm(ks[6], (DEPTH, D, D_IN), D ** -0.5),
        "hgrn_lb_logits": nrm(ks[7], (DEPTH, HG_HEADS * HG_DK), 0.5),
        "hgrn_norm_g": gain(ks[8], (DEPTH, HG_DV)),
        "da_qnorm_g": gain(ks[9], (DEPTH, DA_HEAD_DIM)),
        "da_knorm_g": gain(ks[10], (DEPTH, DA_HEAD_DIM)),
        "da_lambda": nrm(ks[11], (DEPTH, 4, DA_HEAD_DIM), 0.1),
        "da_subln_g": gain(ks[12], (DEPTH, DA_V_DIM)),
        "w_branch_a": nrm(ks[13], (DEPTH, HG_WIDTH, D), HG_WIDTH ** -0.5),
        "w_branch_b": nrm(ks[14], (DEPTH, DA_WIDTH, D), DA_WIDTH ** -0.5),
        "w_out": nrm(ks[15], (DEPTH, D, D), D ** -0.5),
        "ffn_w1": nrm(ks[16], (N_DENSE, D, D_FF), D ** -0.5),
        "ffn_w3": nrm(ks[17], (N_DENSE, D, D_FF), D ** -0.5),
        "ffn_w2": nrm(ks[18], (N_DENSE, D_FF, D), D_FF ** -0.5),
        "moe_router": nrm(ks[19], (N_MOE, D, N_EXPERTS), D ** -0.5),
        "moe_w1": nrm(ks[20], (N_MOE, N_EXPERTS, D, D_FF_EXPERT), D ** -0.5),
        "moe_w3": nrm(ks[21], (N_MOE, N_EXPERTS, D, D_FF_EXPERT), D ** -0.5),
        "moe_w2": nrm(ks[22], (N_MOE, N_EXPERTS, D_FF_EXPERT, D), D_FF_EXPERT ** -0.5),
    }


def reference(x, c, ada_w, ada_b, norm_mix_g, norm_ffn_g, w_in, hgrn_lb_logits, hgrn_norm_g,
              da_qnorm_g, da_knorm_g, da_lambda, da_subln_g, w_branch_a, w_branch_b, w_out,
              ffn_w1, ffn_w3, ffn_w2, moe_router, moe_w1, moe_w3, moe_w2):
    dt = x.dtype
    lb_all = hgrn_lower_bounds(hgrn_lb_logits)
    cs = jax.nn.silu(c)
    for l in range(DEPTH):
        ada = cs @ ada_w[l] + ada_b[l]
        sh1, sc1, g1, sh2, sc2, g2 = jnp.split(ada[:, None, :], ADA_CHUNKS, axis=-1)
        h = (rmsnorm(x, norm_mix_g[l]) * (1.0 + sc1) + sh1).astype(dt)
        x = x + g1 * token_mixer(h, l, w_in[l], lb_all[l], hgrn_norm_g[l], da_qnorm_g[l], da_knorm_g[l],
                                 da_lambda[l], da_subln_g[l], w_branch_a[l], w_branch_b[l], w_out[l])
        h = (rmsnorm(x, norm_ffn_g[l]) * (1.0 + sc2) + sh2).astype(dt)
        if l % 2 == 0:
            f = swiglu(h, ffn_w1[l // 2], ffn_w3[l // 2], ffn_w2[l // 2])
        else:
            f = moe_swiglu(h, moe_router[l // 2], moe_w1[l // 2], moe_w3[l // 2], moe_w2[l // 2])
        x = x + g2 * f
    return x
```

```python
import math
import contextlib
import numpy as np
import concourse.bass as bass
import concourse.mybir as mybir
from concourse.bass_utils import run_bass_kernel_spmd

F32 = mybir.dt.float32
BF16 = mybir.dt.bfloat16
AF = mybir.ActivationFunctionType
ALU = mybir.AluOpType
AX = mybir.AxisListType

D = 1024
DIN = 5632
NH = 4
DFF = 2816
DFE = 3584
NE = 8
EPS = 1e-6
PL = 332
NPAR = 8 + 2 * PL
C_ID, C_BLK, C_M32, C_TRI, C_RM, C_KB = 0, 128, 256, 384, 512, 1024
C_US, C_TH, C_TV, C_PI = 1152, 1280, 1288, 1352
NCST = 1353
TS = 512
SLOPES = [2.0 ** (-8.0 * (i + 1) / NH) for i in range(NH)]


class MK:
    def __init__(self, nc, es, nds=48):
        self.nc = nc
        self.eng = dict(pe=nc.tensor, act=nc.scalar, dve=nc.vector, pool=nc.gpsimd, sp=nc.sync)
        self.esem = {k: es.enter_context(nc.semaphore("s_" + k)) for k in self.eng}
        self.ecnt = {k: 0 for k in self.eng}
        self.nds = nds
        self.dsem = [es.enter_context(nc.semaphore("d%d" % i)) for i in range(nds)]
        self.dcnt = [0] * nds
        self.dnext = {'sp': 0, 'pool': 0, 'act': 0}
        self.dpool = {'sp': list(range(0, nds - 16)), 'pool': list(range(nds - 16, nds)), 'act': []}
        self.seen = {k: {} for k in self.eng}
        self.lastw = {}
        self.readers = {}
        self.nwaits = 0
        self.nops = 0

    def _sem(self, sk):
        return self.esem[sk[1]] if sk[0] == 'e' else self.dsem[sk[1]]

    def _wait(self, en, ev):
        sk, val = ev
        if val <= 0:
            return
        if sk[0] == 'e' and sk[1] == en and en in ('pe', 'sp'):
            return
        if sk[0] == 'e':
            assert val <= self.ecnt[sk[1]], ("forward wait", en, ev, self.ecnt[sk[1]])
        if self.seen[en].get(sk, 0) >= val:
            return
        self.eng[en].wait_ge(self._sem(sk), val)
        self.seen[en][sk] = val
        self.nwaits += 1

    def _deps(self, en, reads, writes):
        for k in reads:
            ev = self.lastw.get(k)
            if ev is not None:
                self._wait(en, ev)
        for k in writes:
            ev = self.lastw.get(k)
            if ev is not None:
                self._wait(en, ev)
            for sk, val in self.readers.get(k, {}).items():
                self._wait(en, (sk, val))

    def _record(self, ev, reads, writes):
        sk, val = ev
        for k in writes:
            self.lastw[k] = ev
            self.readers[k] = {}
        for k in reads:
            d = self.readers.setdefault(k, {})
            if d.get(sk, 0) < val:
                d[sk] = val

    def op(self, en, fn, reads=(), writes=(), signal=True):
        self._deps(en, reads, writes)
        ins = fn(self.eng[en])
        self.nops += 1
        if signal:
            self.ecnt[en] += 1
            ins.then_inc(self.esem[en], 1)
            ev = (('e', en), self.ecnt[en])
        else:
            ev = (('e', en), self.ecnt[en] + 1)
        self._record(ev, reads, writes)
        return ins

    def dma(self, q, out, in_, reads=(), writes=(), **kw):
        pl = self.dpool[q]
        i = pl[self.dnext[q] % len(pl)]
        self.dnext[q] += 1
        self._wait(q, (('d', i), self.dcnt[i]))
        self._deps(q, reads, writes)
        ins = self.eng[q].dma_start(out=out, in_=in_, **kw)
        ins.then_inc(self.dsem[i], 16)
        self.dcnt[i] += 16
        ev = (('d', i), self.dcnt[i])
        self._record(ev, reads, writes)
        self.nops += 1
        return ins

    def dmai(self, out, out_off, in_, in_off, reads=(), writes=()):
        q = 'pool'
        pl = self.dpool[q]
        i = pl[self.dnext[q] % len(pl)]
        self.dnext[q] += 1
        self._wait(q, (('d', i), self.dcnt[i]))
        self._deps(q, reads, writes)
        ins = self.eng[q].indirect_dma_start(out, out_off, in_, in_off)
        ins.then_inc(self.dsem[i], 16)
        self.dcnt[i] += 16
        ev = (('d', i), self.dcnt[i])
        self._record(ev, reads, writes)
        self.nops += 1
        return ins

    def note_read(self, en, reads):
        self._deps(en, reads, ())

    def barrier(self):
        for en in self.eng:
            for fn in self.eng:
                if fn != en:
                    self._wait(en, (('e', fn), self.ecnt[fn]))
            for i in range(self.nds):
                self._wait(en, (('d', i), self.dcnt[i]))
        self.lastw = {}
        self.readers = {}

    def finish(self):
        for i in range(self.nds):
            self._wait('sp', (('d', i), self.dcnt[i]))
        for fn in self.eng:
            if fn != 'sp':
                self._wait('sp', (('e', fn), self.ecnt[fn]))


_UID = [0]


def _uname(name):
    _UID[0] += 1
    return "%s_u%d" % (name, _UID[0])


class Ring:
    def __init__(self, es, nc, name, shape, dt, n):
        self.t = [es.enter_context(nc.sbuf_tensor(_uname("%s%d" % (name, i)), shape, dt)) for i in range(n)]
        self.k = [(name, i) for i in range(n)]
        self.i = 0

    def get(self):
        i = self.i
        self.i = (i + 1) % len(self.t)
        return self.t[i], self.k[i]


def build(S=4096, dbg=(), nlayers=2, phases=None):
    NB = S // 512
    NT = S // 128
    NCH = S // 32
    nc = bass.Bass("TRN2", target_bir_lowering=False)

    def din(name, shape):
        return nc.dram_tensor(name, shape, F32, kind="ExternalInput").ap()

    x = din("x", [S, D])
    par = din("par", [128, NPAR])
    cst = din("cst", [128, NCST])
    qrows = din("qrows", [2, NH * 512])
    ada_w = din("ada_w", [2, D, 6 * D])
    w_in = din("w_in", [2, D, DIN])
    w_pa = din("w_pa", [2, 512, D])
    w_pb = din("w_pb", [2, 512, D])
    w_o = din("w_o", [2, D, D])
    f_w1 = din("ffn_w1", [1, D, DFF])
    f_w3 = din("ffn_w3", [1, D, DFF])
    f_w2 = din("ffn_w2", [1, DFF, D])
    router = din("router", [1, D, NE])
    m_w1 = din("moe_w1", [1, NE, D, DFE])
    m_w3 = din("moe_w3", [1, NE, D, DFE])
    m_w2 = din("moe_w2", [1, NE, DFE, D])
    out = nc.dram_tensor("out", [S, D], F32, kind="ExternalOutput").ap()

    def scr(name, shape, dt):
        kind = "ExternalOutput" if name in dbg else "Internal"
        return nc.dram_tensor(name, shape, dt, kind=kind).ap()

    xT = scr("xT", [D, S], F32)
    QH = scr("QH", [NH, 128, S], BF16)
    KD = scr("KD", [NH, 128, S], BF16)
    KDEC = scr("KDEC", [NH, 128, S], BF16)
    VHT = scr("VHT", [NH, 128, S], BF16)
    SG = scr("SG", [NH, 128, S], BF16)
    EBL = scr("EBL", [NH, 128, NCH], F32)
    QA = scr("QA", [NH, 2, 64, S], BF16)
    KA = scr("KA", [NH, 2, 64, S], BF16)
    VA = scr("VA", [S, 512], BF16)
    GA = scr("GA", [D, S], BF16)
    GB = scr("GB", [D, S], BF16)
    OHG = scr("OHG", [NH, 128, S], BF16)
    ODA = scr("ODA", [NH, 128, S], BF16)
    H2 = scr("H2", [D, S], BF16)
    TMAX = -(-(2 * S + NE * (TS - 1)) // TS)
    NSLOT = TMAX * TS
    H2TOK = scr("H2TOK", [S, D], BF16)
    HSLOT = scr("HSLOT", [NSLOT, D], BF16)
    YSLOT = scr("YSLOT", [NSLOT, D], F32)
    MFG = 4
    MNFG = DFE // (MFG * 128)
    W1C = scr("W1C", [NE * MNFG * 128, 8 * MFG * 128], BF16)
    W3C = scr("W3C", [NE * MNFG * 128, 8 * MFG * 128], BF16)
    W2C = scr("W2C", [NE * MNFG * 128, MFG * D], BF16)

    es0 = contextlib.ExitStack()
    with es0:
        M = MK(nc, es0)
        ps = [es0.enter_context(nc.psum_tensor("ps%d" % i, [128, 512], F32)) for i in range(8)]
        PK = [("ps", i) for i in range(8)]
        csem = es0.enter_context(nc.semaphore("csem"))
        cconv = [0]

        conv_list = [(e_, fg) for e_ in range(NE) for fg in range(MNFG)] if nlayers > 1 else []
        conv_pos = [0]

        def conv_step(n=1):
            FWc = MFG * 128
            for _ in range(n):
                if conv_pos[0] >= len(conv_list):
                    return
                e_, fg = conv_list[conv_pos[0]]
                conv_pos[0] += 1
                if True:
                    r0 = (e_ * MNFG + fg) * 128
                    for (dst, src) in ((W1C, m_w1), (W3C, m_w3)):
                        nc.gpsimd.dma_start(out=dst[r0:r0 + 128, :].rearrange("p (kc f) -> p kc f", kc=8),
                                            in_=src[0, e_].rearrange("(kc p) f -> p kc f", p=128)[:, :, fg * FWc:(fg + 1) * FWc]).then_inc(csem, 16)
                        cconv[0] += 16
                    nc.gpsimd.dma_start(out=W2C[r0:r0 + 128, :].rearrange("p (fc d) -> p fc d", fc=MFG),
                                        in_=m_w2[0, e_][fg * FWc:(fg + 1) * FWc, :].rearrange("(fc p) d -> p fc d", p=128)).then_inc(csem, 16)
                    cconv[0] += 16

        def sb0(name, shape, dt):
            return es0.enter_context(nc.sbuf_tensor(name, shape, dt))

        cst32 = sb0("cst32", [128, NCST], F32)
        par_sb = sb0("par_sb", [128, NPAR], F32)
        id16 = sb0("id16", [128, 128], BF16)
        blk16 = sb0("blk16", [128, 128], BF16)
        tri16 = sb0("tri16", [128, 128], BF16)
        ones16 = sb0("ones16", [128, 128], BF16)
        qrow16 = sb0("qrow16", [66, NH * 512], BF16)
        dv = sb0("dv", [128, 2, 64], F32)
        ada_sb = sb0("ada_sb", [128, 2, 48], F32)
        cs32 = sb0("cs32", [128, 8], F32)
        us16 = sb0("us16", [128, 128], BF16)
        rank_all = sb0("rank_all", [128, NT, NE], F32)
        sel_all = sb0("sel_all", [128, NT, NE], F32)
        m1_all = sb0("m1_all", [128, NT, NE], F32)
        comb_all = sb0("comb_all", [128, NT, NE], F32)
        rbase = sb0("rbase", [128, NE], F32)
        M.dma('sp', cst32[:], cst[:, :], writes=["cst"])
        M.dma('sp', par_sb[:], par[:, :], writes=["par"])
        M.dma('pool', id16[:], cst[:, C_ID:C_ID + 128], writes=["id16"])
        M.dma('pool', blk16[:], cst[:, C_BLK:C_BLK + 128], writes=["blk16"])
        M.dma('pool', tri16[:], cst[:, C_TRI:C_TRI + 128], writes=["tri16"])
        M.dma('pool', us16[:], cst[:, C_US:C_US + 128], writes=["us16"])
        M.dma('pool', qrow16[64:66, :], qrows[:, :], writes=["qrow16"])
        M.op('dve', lambda e: e.memset(ones16[:], 1.0), [], ["ones16"])
        id32 = cst32[:, C_ID:C_ID + 128]
        m32 = cst32[0:32, C_M32:C_M32 + 128]
        rmask = cst32[:, C_RM:C_RM + 512]
        kbias = cst32[:, C_KB:C_KB + 128]
        M.op('act', lambda e: e.activation(out=cs32[:], in_=par_sb[:, 0:8], func=AF.Silu), ["par"], ["cs32"])

        def pcol(l, off, n=1):
            b = 8 + l * PL + off
            return par_sb[:, b:b + n]

        DV_A1, DV_A2, DV_LB, DV_OML, DV_QSC, DV_GSUB, DV_NLAM, DV_T = 0, 8, 16, 20, 24, 25, 26, 27

        def ada_group(l, g, awr, bank):
            awv = ada_w[l].rearrange("(kc p) f -> p kc f", p=128)
            aw, awk = awr.get()
            M.dma('pool', aw[:], awv[:, :, g * 768:(g + 1) * 768], writes=[awk])
            for jj in range(6):
                j = g * 6 + jj
                for kc in range(8):
                    M.op('pe', lambda e, aw=aw, jj=jj, kc=kc, j=j: e.matmul(
                        ps[bank][:, j:j + 1], aw[:, kc, jj * 128:(jj + 1) * 128], cs32[:, kc:kc + 1],
                        start=(kc == 0), stop=(kc == 7), skip_group_check=True),
                        [awk, "cs32"], [PK[bank]], signal=(kc == 7))

        def ada_finish(l, lt, bank):
            M.op('dve', lambda e, l=l: e.tensor_tensor(out=ada_sb[:, l, :], in0=ps[bank][:, 0:48], in1=pcol(l, 0, 48), op=ALU.add),
                 [PK[bank], "par"], [("ada", l)])
            M.op('dve', lambda e, l=l: e.scalar_tensor_tensor(out=dv[:, l, DV_A1:DV_A1 + 8], in0=ada_sb[:, l, 8:16], scalar=1.0,
                                                              in1=pcol(l, 48, 8), op0=ALU.add, op1=ALU.mult), [("ada", l), "par"], [("dv", l)])
            M.op('dve', lambda e, l=l: e.scalar_tensor_tensor(out=dv[:, l, DV_A2:DV_A2 + 8], in0=ada_sb[:, l, 32:40], scalar=1.0,
                                                              in1=pcol(l, 56, 8), op0=ALU.add, op1=ALU.mult), [("ada", l), "par"], [("dv", l)])
            if l == 0:
                M.op('dve', lambda e, l=l: e.memset(dv[:, l, DV_LB:DV_LB + 4], 0.0), [], [("dv", l)])
                M.op('dve', lambda e, l=l: e.memset(dv[:, l, DV_OML:DV_OML + 4], 1.0), [], [("dv", l)])
            else:
                M.op('dve', lambda e, l=l: e.tensor_tensor(out=lt[:, 0:4], in0=pcol(l, 68, 4), in1=pcol(l, 64, 4), op=ALU.subtract), ["par"], ["lt"])
                M.op('act', lambda e, l=l: e.activation(out=dv[:, l, DV_LB:DV_LB + 4], in_=lt[:, 0:4], func=AF.Sigmoid), ["lt"], [("dv", l)])
                M.op('dve', lambda e, l=l: e.tensor_scalar(out=dv[:, l, DV_OML:DV_OML + 4], in0=dv[:, l, DV_LB:DV_LB + 4], scalar1=-1.0, scalar2=1.0,
                                                           op0=ALU.mult, op1=ALU.add), [("dv", l)], [("dv", l)])
            lam_init = 0.8 - 0.6 * math.exp(-0.3 * l)
            M.op('dve', lambda e, l=l: e.tensor_scalar(out=dv[:, l, DV_QSC:DV_QSC + 1], in0=pcol(l, 73), scalar1=0.125, scalar2=None, op0=ALU.mult), ["par"], [("dv", l)])
            M.op('dve', lambda e, l=l, li=lam_init: e.tensor_scalar(out=dv[:, l, DV_GSUB:DV_GSUB + 1], in0=pcol(l, 75), scalar1=1.0 - li, scalar2=None, op0=ALU.mult), ["par"], [("dv", l)])
            M.op('dve', lambda e, l=l: e.tensor_tensor(out=lt[:, 0:64], in0=pcol(l, 76, 64), in1=pcol(l, 140, 64), op=ALU.mult), ["par"], ["lt"])
            M.op('dve', lambda e, l=l: e.reduce_sum(out=dv[:, l, DV_T:DV_T + 1], in_=lt[:, 0:64], axis=AX.X), ["lt"], [("dv", l)])
            M.op('dve', lambda e, l=l: e.tensor_tensor(out=lt[:, 0:64], in0=pcol(l, 204, 64), in1=pcol(l, 268, 64), op=ALU.mult), ["par", ("dv", l)], ["lt"])
            M.op('dve', lambda e, l=l: e.reduce_sum(out=dv[:, l, DV_T + 1:DV_T + 2], in_=lt[:, 0:64], axis=AX.X), ["lt"], [("dv", l)])
            M.op('act', lambda e, l=l: e.activation(out=dv[:, l, DV_T:DV_T + 2], in_=dv[:, l, DV_T:DV_T + 2], func=AF.Exp), [("dv", l)], [("dv", l)])
            M.op('dve', lambda e, l=l: e.tensor_tensor(out=dv[:, l, DV_NLAM:DV_NLAM + 1], in0=dv[:, l, DV_T + 1:DV_T + 2], in1=dv[:, l, DV_T:DV_T + 1], op=ALU.subtract), [("dv", l)], [("dv", l)])
            M.op('dve', lambda e, l=l, li=lam_init: e.tensor_scalar(out=dv[:, l, DV_NLAM:DV_NLAM + 1], in0=dv[:, l, DV_NLAM:DV_NLAM + 1], scalar1=-li, scalar2=None, op0=ALU.add), [("dv", l)], [("dv", l)])

        win_state = {}

        def win_prefetch(l):
            wes = contextlib.ExitStack()
            wt = wes.enter_context(nc.sbuf_tensor(_uname("win"), [128, 8, DIN], BF16))
            wv = w_in[l].rearrange("(kc p) n -> p kc n", p=128)
            for kc in range(8):
                M.dma('pool', wt[:, kc, :], wv[:, kc, :], writes=[("win", kc)])
            win_state[l] = (wes, wt)

        if phases is None or "p1" in phases:
            win_prefetch(0)
        with contextlib.ExitStack() as es:
          if True:
            xin_r = Ring(es, nc, "xin", [128, D], F32, 2)
            xo_r = Ring(es, nc, "xo", [128, 8, 512], F32, 2)
            xTv = xT.rearrange("(kc p) s -> p kc s", p=128)
            for j in range(NB):
                xo, xok = xo_r.get()
                for tt in range(4):
                    t = j * 4 + tt
                    xin, xink = xin_r.get()
                    M.dma('sp', xin[:], x[t * 128:(t + 1) * 128, :], writes=[xink])
                    for half in range(2):
                        b = 4 + 2 * (tt % 2) + half
                        for q in range(4):
                            kc = half * 4 + q
                            M.op('pe', lambda e, b=b, q=q, kc=kc, xin=xin: e.transpose(ps[b][:, q * 128:(q + 1) * 128], xin[:, kc * 128:(kc + 1) * 128], id32),
                                 [xink, "cst"], [PK[b]], signal=(q == 3))
                        eng = 'act' if half == 0 else 'dve'
                        if eng == 'act':
                            M.op('act', lambda e, b=b, half=half, tt=tt, xo=xo: e.copy(
                                out=xo[:, half * 4:half * 4 + 4, tt * 128:(tt + 1) * 128], in_=ps[b][:].rearrange("p (q t) -> p q t", q=4)),
                                [PK[b]], [xok])
                        else:
                            M.op('dve', lambda e, b=b, half=half, tt=tt, xo=xo: e.tensor_copy(
                                out=xo[:, half * 4:half * 4 + 4, tt * 128:(tt + 1) * 128], in_=ps[b][:].rearrange("p (q t) -> p q t", q=4)),
                                [PK[b]], [xok])
                M.dma('sp', xTv[:, :, j * 512:(j + 1) * 512], xo[:], reads=[xok], writes=[("xT", j)])

          if True:
            awr = Ring(es, nc, "aw", [128, 8, 768], F32, 2)
            lt = es.enter_context(nc.sbuf_tensor(_uname("lt"), [128, 64], F32))
            for l in range(1):
                for g in range(8):
                    ada_group(l, g, awr, 0)
                ada_finish(l, lt, 0)
            M.barrier()

        def K8(k):
            return [(k, i) for i in range(8)]

        def norm_block(xs, xsk, A, B, Akey, hT, hTk, sq, sqk, r32, psb, rsr, h32=None):
            for kc in range(8):
                M.op('act', lambda e, kc=kc: e.activation(out=sq[:, kc, :], in_=xs[:, kc, :], func=AF.Square), [(xsk, kc)], [(sqk, kc)])
            for kc in range(8):
                M.op('pe', lambda e, kc=kc: e.matmul(ps[psb][:], ones16[:], sq[:, kc, :], start=(kc == 0), stop=(kc == 7)),
                     [(sqk, kc), "ones16"], [PK[psb]], signal=(kc == 7))
            rs, rsk = rsr.get()
            M.op('act', lambda e: e.activation(out=rs[:], in_=ps[psb][:], func=AF.Ln, bias=EPS, scale=1.0 / D), [PK[psb]], [rsk])
            M.op('act', lambda e: e.activation(out=rs[:], in_=rs[:], func=AF.Exp, scale=-0.5), [rsk], [rsk])
            for kc in range(8):
                t, tk = r32.get()
                M.op('dve', lambda e, kc=kc, t=t: e.scalar_tensor_tensor(out=t[:], in0=xs[:, kc, :], scalar=A[:, kc:kc + 1], in1=rs[:],
                                                                        op0=ALU.mult, op1=ALU.mult), [(xsk, kc), rsk, Akey], [tk])
                if h32 is None:
                    M.op('act', lambda e, kc=kc, t=t: e.activation(out=hT[:, kc, :], in_=t[:], func=AF.Identity, bias=B[:, kc:kc + 1], scale=1.0),
                         [tk, Akey], [(hTk, kc)])
                else:
                    M.op('act', lambda e, kc=kc, t=t: e.activation(out=h32[0][:, kc, :], in_=t[:], func=AF.Identity, bias=B[:, kc:kc + 1], scale=1.0),
                         [tk, Akey], [(h32[1], kc)])
                    M.op('pool', lambda e, kc=kc: e.tensor_copy(out=hT[:, kc, :], in_=h32[0][:, kc, :]), [(h32[1], kc)], [(hTk, kc)])

        QHv = QH.rearrange("h p s -> p h s")
        KDv = KD.rearrange("h p s -> p h s")
        KDECv = KDEC.rearrange("h p s -> p h s")
        VHTv = VHT.rearrange("h p s -> p h s")
        SGv = SG.rearrange("h p s -> p h s")
        EBLv = EBL.rearrange("h p c -> p h c")
        OHGv = OHG.rearrange("h p s -> p h s")
        ODAv = ODA.rearrange("h p s -> p h s")
        xTv = xT.rearrange("(kc p) s -> p kc s", p=128)
        GAv = GA.rearrange("(kc p) s -> p kc s", p=128)
        GBv = GB.rearrange("(kc p) s -> p kc s", p=128)
        H2v = H2.rearrange("(kc p) s -> p kc s", p=128)
        VAv = VA.rearrange("(t p) c -> p t c", p=128)

        def want(ph):
            return phases is None or ph in phases

        for l in range(nlayers):
            A1 = dv[:, l, DV_A1:DV_A1 + 8]
            B1 = ada_sb[:, l, 0:8]
            G1 = ada_sb[:, l, 16:24]
            A2 = dv[:, l, DV_A2:DV_A2 + 8]
            B2 = ada_sb[:, l, 24:32]
            G2 = ada_sb[:, l, 40:48]
            LB = dv[:, l, DV_LB:DV_LB + 4]
            OML = dv[:, l, DV_OML:DV_OML + 4]
            QSC = dv[:, l, DV_QSC:DV_QSC + 1]
            KSC = pcol(l, 74)
            GSUB = dv[:, l, DV_GSUB:DV_GSUB + 1]
            NLAM = dv[:, l, DV_NLAM:DV_NLAM + 1]
            HGN = pcol(l, 72)
            PKEY = [("dv", l), ("ada", l), "par"]

            if want("p1"):
              if l not in win_state:
                  win_prefetch(l)
              wes_, win = win_state.pop(l)
              with contextlib.ExitStack() as es:
                xs_r = Ring(es, nc, "xs", [128, 8, 512], F32, 2)
                sq = es.enter_context(nc.sbuf_tensor(_uname("sq"), [128, 8, 512], BF16))
                hT_r = Ring(es, nc, "hT", [128, 8, 512], BF16, 2)
                r32 = Ring(es, nc, "r32", [128, 512], F32, 8)
                r16 = Ring(es, nc, "r16", [128, 512], BF16, 8)
                rbl = Ring(es, nc, "rbl", [128, 16], F32, 4)
                rsr = Ring(es, nc, "rsr", [128, 512], F32, 2)
                WINK = [("win", kc) for kc in range(8)]
                pring = [2, 3, 4, 5, 6, 7]
                pri = [0]

                def nextbank():
                    b = pring[pri[0] % len(pring)]
                    pri[0] += 1
                    return b

                def p1_load(jn):
                    xs_, xsk_ = xs_r.get()
                    M.dma('sp', xs_[:], xTv[:, :, jn * 512:(jn + 1) * 512], reads=[("xT", jn)], writes=K8(xsk_))
                    return xs_, xsk_

                def p1_norm(ld):
                    hT_, hTk_ = hT_r.get()
                    norm_block(ld[0], ld[1], A1, B1, PKEY[0], hT_, hTk_, sq, "sq", r32, 0, rsr)
                    return hT_, hTk_

                ld_cur = p1_load(0)
                h_cur = p1_norm(ld_cur)
                for j in range(NB):
                    cols = slice(j * 512, (j + 1) * 512)
                    ld_nxt = p1_load(j + 1) if j + 1 < NB else None
                    if l == 1:
                        conv_step(2)
                    hT, hTk = h_cur

                    def proj_fm(oc):
                        b = nextbank()
                        for kc in range(8):
                            M.op('pe', lambda e, kc=kc, b=b, oc=oc: e.matmul(ps[b][:], win[:, kc, oc * 128:(oc + 1) * 128], hT[:, kc, :],
                                                                           start=(kc == 0), stop=(kc == 7)),
                                 [WINK[kc], (hTk, kc)], [PK[b]], signal=(kc == 7))
                        return b

                    for h in range(NH):
                        bz = proj_fm(4 + h)
                        bq = proj_fm(h)
                        sg, sgk = r32.get()
                        sn, snk = r32.get()
                        M.op('act', lambda e, sg=sg, bz=bz: e.activation(out=sg[:], in_=ps[bz][:], func=AF.Sigmoid), [PK[bz]], [sgk])
                        M.op('act', lambda e, sn=sn, bz=bz: e.activation(out=sn[:], in_=ps[bz][:], func=AF.Sigmoid, scale=-1.0), [PK[bz]], [snk])
                        M.op('dve', lambda e, sg=sg, h=h: e.tensor_scalar(out=sg[:], in0=sg[:], scalar1=OML[:, h:h + 1], scalar2=LB[:, h:h + 1],
                                                                        op0=ALU.mult, op1=ALU.add), [sgk, PKEY[0]], [sgk])
                        M.op('act', lambda e, sg=sg: e.activation(out=sg[:], in_=sg[:], func=AF.Ln), [sgk], [sgk])
                        bT, bTk = r32.get()
                        M.op('dve', lambda e, sg=sg, bT=bT: e.tensor_tensor_scan(out=bT[:], data0=rmask, data1=sg[:], initial=0.0, op0=ALU.mult, op1=ALU.add),
                             [sgk, "cst"], [bTk])
                        M.op('dve', lambda e, sn=sn, h=h: e.tensor_scalar(out=sn[:], in0=sn[:], scalar1=OML[:, h:h + 1], scalar2=None, op0=ALU.mult),
                             [snk, PKEY[0]], [snk])
                        e1, e1k = r32.get()
                        M.op('act', lambda e, e1=e1, bT=bT: e.activation(out=e1[:], in_=bT[:], func=AF.Exp, scale=-1.0), [bTk], [e1k])
                        kd, kdk = r16.get()
                        M.op('dve', lambda e, kd=kd, sn=sn, e1=e1: e.tensor_tensor(out=kd[:], in0=sn[:], in1=e1[:], op=ALU.mult), [snk, e1k], [kdk])
                        M.dma('sp', KDv[:, h, cols], kd[:], reads=[kdk], writes=[("KD", j)])
                        bT3 = bT[:].rearrange("p (c s) -> p c s", s=32)
                        M.op('dve', lambda e, e1=e1, bT3=bT3: e.tensor_tensor(out=e1[:].rearrange("p (c s) -> p c s", s=32),
                                                                               in0=bT3[:, :, 31:32].to_broadcast([128, 16, 32]), in1=bT3, op=ALU.subtract),
                             [bTk], [e1k])
                        M.op('act', lambda e, e1=e1: e.activation(out=e1[:], in_=e1[:], func=AF.Exp), [e1k], [e1k])
                        kdec, kdeck = r16.get()
                        M.op('dve', lambda e, kdec=kdec, sn=sn, e1=e1: e.tensor_tensor(out=kdec[:], in0=sn[:], in1=e1[:], op=ALU.mult), [snk, e1k], [kdeck])
                        M.dma('sp', KDECv[:, h, cols], kdec[:], reads=[kdeck], writes=[("KDEC", j)])
                        ebl, eblk = rbl.get()
                        M.op('act', lambda e, ebl=ebl, bT3=bT3: e.activation(out=ebl[:], in_=bT3[:, :, 31], func=AF.Exp), [bTk], [eblk])
                        M.dma('sp', EBLv[:, h, j * 16:(j + 1) * 16], ebl[:], reads=[eblk], writes=[("EBL", j)])
                        M.op('act', lambda e, bT=bT: e.activation(out=bT[:], in_=bT[:], func=AF.Exp), [bTk], [bTk])
                        qe, qek = r16.get()
                        M.op('dve', lambda e, qe=qe, bq=bq, bT=bT: e.tensor_tensor(out=qe[:], in0=ps[bq][:], in1=bT[:], op=ALU.mult), [PK[bq], bTk], [qek])
                        M.dma('sp', QHv[:, h, cols], qe[:], reads=[qek], writes=[("QH", j)])
                    for h in range(NH):
                        b = proj_fm(8 + h)
                        t, tk = r16.get()
                        M.op('act', lambda e, t=t, b=b: e.copy(out=t[:], in_=ps[b][:]), [PK[b]], [tk])
                        M.dma('sp', VHTv[:, h, cols], t[:], reads=[tk], writes=[("VHT", j)])
                    for h in range(NH):
                        b = proj_fm(12 + h)
                        t, tk = r16.get()
                        M.op('act', lambda e, t=t, b=b: e.activation(out=t[:], in_=ps[b][:], func=AF.Silu), [PK[b]], [tk])
                        M.dma('sp', SGv[:, h, cols], t[:], reads=[tk], writes=[("SG", j)])
                    for (base, scol, dst, dkey) in ((16, QSC, QA, "QA"), (20, KSC, KA, "KA")):
                        for h in range(NH):
                            b = proj_fm(base + h)
                            s2, s2k = r16.get()
                            M.op('act', lambda e, s2=s2, b=b: e.activation(out=s2[:], in_=ps[b][:], func=AF.Square), [PK[b]], [s2k])
                            M.op('pe', lambda e, s2=s2: e.matmul(ps[1][:], blk16[:], s2[:], start=True, stop=True), [s2k, "blk16"], [PK[1]])
                            rr, rrk = r32.get()
                            M.op('act', lambda e, rr=rr: e.activation(out=rr[:], in_=ps[1][:], func=AF.Ln, bias=EPS, scale=1.0 / 64), [PK[1]], [rrk])
                            M.op('act', lambda e, rr=rr: e.activation(out=rr[:], in_=rr[:], func=AF.Exp, scale=-0.5), [rrk], [rrk])
                            qn, qnk = r16.get()
                            M.op('dve', lambda e, qn=qn, b=b, rr=rr, scol=scol: e.scalar_tensor_tensor(out=qn[:], in0=ps[b][:], scalar=scol, in1=rr[:],
                                                                                                      op0=ALU.mult, op1=ALU.mult),
                                 [PK[b], rrk] + PKEY, [qnk])
                            M.dma('sp', dst[h].rearrange("c d s -> (c d) s")[:, cols], qn[:], reads=[qnk], writes=[(dkey, j)])
                    for tt in range(4):
                        b = nextbank()
                        for kc in range(8):
                            M.op('pe', lambda e, kc=kc, b=b, tt=tt: e.matmul(ps[b][:], hT[:, kc, tt * 128:(tt + 1) * 128], win[:, kc, 3072:3584],
                                                                           start=(kc == 0), stop=(kc == 7)),
                                 [WINK[kc], (hTk, kc)], [PK[b]], signal=(kc == 7))
                        t, tk = r16.get()
                        M.op('act', lambda e, t=t, b=b: e.copy(out=t[:], in_=ps[b][:]), [PK[b]], [tk])
                        r0 = j * 512 + tt * 128
                        M.dma('sp', VA[r0:r0 + 128, :], t[:], reads=[tk], writes=[("VA", j)])
                    if ld_nxt is not None:
                        h_cur = p1_norm(ld_nxt)
                    for (base, dstv, dkey) in ((28, GAv, "GA"), (36, GBv, "GB")):
                        for kc2 in range(8):
                            b = proj_fm(base + kc2)
                            t, tk = r16.get()
                            M.op('act', lambda e, t=t, b=b: e.activation(out=t[:], in_=ps[b][:], func=AF.Sigmoid), [PK[b]], [tk])
                            M.dma('sp', dstv[:, kc2, cols], t[:], reads=[tk], writes=[(dkey, j)])
                M.barrier()
              wes_.close()

            pf3 = pfv = None
            if want("p2a") and want("p2b") and want("p3"):
                pf3 = contextlib.ExitStack()
                wpa = pf3.enter_context(nc.sbuf_tensor(_uname("wpa"), [128, 4, D], BF16))
                wpb = pf3.enter_context(nc.sbuf_tensor(_uname("wpb"), [128, 4, D], BF16))
                wo = pf3.enter_context(nc.sbuf_tensor(_uname("wo"), [128, 8, D], BF16))
                M.dma('pool', wpa[:], w_pa[l].rearrange("(kc p) n -> p kc n", p=128), writes=["wpa"])
                M.dma('pool', wpb[:], w_pb[l].rearrange("(kc p) n -> p kc n", p=128), writes=["wpb"])
                M.dma('pool', wo[:], w_o[l].rearrange("(kc p) n -> p kc n", p=128), writes=["wo"])
                pfv = contextlib.ExitStack()
                vsb = pfv.enter_context(nc.sbuf_tensor(_uname("vsb"), [128, NT, 512], BF16))
                M.dma('sp', vsb[:], VAv[:, :, :], writes=["vsb"])
            if want("p2a"):
              with contextlib.ExitStack() as es:
                qe_r = Ring(es, nc, "hq", [128, NH, 512], BF16, 2)
                kd_r = Ring(es, nc, "hkd", [128, NH, 512], BF16, 2)
                kc_r = Ring(es, nc, "hkc", [128, NH, 512], BF16, 2)
                vt_r = Ring(es, nc, "hvt", [128, NH, 512], BF16, 2)
                sg_r = Ring(es, nc, "hsg", [128, NH, 512], BF16, 2)
                eb_r = Ring(es, nc, "heb", [128, NH, 16], F32, 2)
                sm_r = Ring(es, nc, "hsm", [32, 128], BF16, 4)
                tk_r = Ring(es, nc, "htk", [32, 1024], BF16, 4)
                Sst = es.enter_context(nc.sbuf_tensor(_uname("Sst"), [128, NH, 128], F32))
                Sbf = [Ring(es, nc, "Sbf%d" % h, [128, 128], BF16, 2) for h in range(NH)]
                r32 = Ring(es, nc, "r32", [128, 512], F32, 4)
                r16 = Ring(es, nc, "r16", [128, 512], BF16, 4)
                M.op('dve', lambda e: e.memset(Sst[:], 0.0), [], [("S", h) for h in range(NH)])
                scur = []
                for h in range(NH):
                    s0, s0k = Sbf[h].get()
                    M.op('dve', lambda e, s0=s0: e.memset(s0[:], 0.0), [], [s0k])
                    scur.append((s0, s0k))
                psS, psE = 0, 7
                psTl = [1, 2]
                psUb = [3, 6]
                psOb = [4, 5]
                blk = {}

                def load_block(j):
                    cols = slice(j * 512, (j + 1) * 512)
                    d = {}
                    for nm, ring, src, key in (("qe", qe_r, QHv, "QH"), ("kd", kd_r, KDv, "KD"), ("kc", kc_r, KDECv, "KDEC"),
                                               ("vt", vt_r, VHTv, "VHT"), ("sg", sg_r, SGv, "SG")):
                        t, k = ring.get()
                        M.dma('sp', t[:], src[:, :, cols], reads=[(key, j)], writes=[k])
                        d[nm] = (t, k)
                    t, k = eb_r.get()
                    M.dma('sp', t[:], EBLv[:, :, j * 16:(j + 1) * 16], reads=[("EBL", j)], writes=[k])
                    d["eb"] = (t, k)
                    blk[j] = d

                def stage_a(g):
                    j, c = divmod(g, 16)
                    d = blk[j]
                    (qe, qek), (kd, kdk), (kc_, kck), (vt, vtk) = d["qe"], d["kd"], d["kc"], d["vt"]
                    cc = slice(c * 32, (c + 1) * 32)
                    for h in range(NH):
                        M.op('pe', lambda e, h=h: e.matmul(ps[psS][0:32, h * 32:(h + 1) * 32], kd[:, h, cc], qe[:, h, cc], start=True, stop=True),
                             [kdk, qek], [PK[psS]], signal=(h == NH - 1))
                    sm, smk = sm_r.get()
                    M.op('dve', lambda e: e.tensor_tensor(out=sm[:], in0=ps[psS][0:32, 0:128], in1=m32, op=ALU.mult), [PK[psS], "cst"], [smk])
                    pT = psTl[g % 2]
                    psTb = ps[pT][:].bitcast(BF16)
                    for h in range(NH):
                        M.op('pe', lambda e, h=h: e.transpose(psTb[0:32, h * 128:(h + 1) * 128], kc_[:, h, cc], id16[:]),
                             [kck, "id16"], [PK[pT]], signal=False)
                    for h in range(NH):
                        M.op('pe', lambda e, h=h: e.transpose(psTb[0:32, 512 + h * 128:512 + (h + 1) * 128], vt[:, h, cc], id16[:]),
                             [vtk, "id16"], [PK[pT]], signal=(h == NH - 1))
                    tk, tkk = tk_r.get()
                    M.op('act', lambda e: e.copy(out=tk[:, 0:512], in_=psTb[0:32, 0:512]), [PK[pT]], [(tkk, 0)])
                    M.op('dve', lambda e: e.tensor_copy(out=tk[:, 512:1024], in_=psTb[0:32, 512:1024]), [PK[pT], (tkk, 0)], [(tkk, 1)])
                    return sm, smk, tk, tkk

                def stage_b(g, a_out):
                    j, c = divmod(g, 16)
                    sm, smk, tk, tkk = a_out
                    d = blk[j]
                    (qe, qek), (eb, ebk) = d["qe"], d["eb"]
                    cc = slice(c * 32, (c + 1) * 32)
                    for h in range(NH):
                        s_bf, s_bfk = scur[h]
                        bo = psOb[h // 2]
                        oc = slice((h % 2) * 256 + (c % 8) * 32, (h % 2) * 256 + (c % 8) * 32 + 32)
                        firstw = (c % 8 == 0) and (h % 2 == 0)
                        M.op('pe', lambda e, h=h: e.matmul(ps[bo][:, oc], s_bf[:], qe[:, h, cc], start=firstw, stop=False, skip_group_check=True),
                             [s_bfk, qek], [PK[bo]], signal=False)
                        M.op('pe', lambda e, h=h: e.matmul(ps[bo][:, oc], tk[:, 512 + h * 128:512 + (h + 1) * 128], sm[:, h * 32:(h + 1) * 32],
                                                          start=False, stop=True, skip_group_check=True),
                             [(tkk, 1), smk], [PK[bo]], signal=False)
                        psU = psUb[h % 2]
                        M.op('pe', lambda e, h=h: e.matmul(ps[psU][:, 0:128], tk[:, h * 128:(h + 1) * 128], tk[:, 512 + h * 128:512 + (h + 1) * 128],
                                                          start=True, stop=True), [(tkk, 0), (tkk, 1)], [PK[psU]], signal=True)
                        M.op('dve', lambda e, h=h: e.scalar_tensor_tensor(out=Sst[:, h, :], in0=Sst[:, h, :], scalar=eb[:, h, c:c + 1],
                                                                        in1=ps[psU][:, 0:128], op0=ALU.mult, op1=ALU.add),
                             [("S", h), ebk, PK[psU]], [("S", h)])
                        s_n, s_nk = Sbf[h].get()
                        M.op('act', lambda e, h=h: e.copy(out=s_n[:], in_=Sst[:, h, :]), [("S", h)], [s_nk])
                        scur[h] = (s_n, s_nk)
                    if c % 8 == 7:
                        half = c // 8
                        (sg, sgk) = d["sg"]
                        tcols = slice(j * 512 + half * 256, j * 512 + half * 256 + 256)
                        for hp in range(2):
                            bo = psOb[hp]
                            oq, oqk = r16.get()
                            M.op('act', lambda e: e.activation(out=oq[:], in_=ps[bo][:], func=AF.Square), [PK[bo]], [oqk])
                            M.op('pe', lambda e: e.matmul(ps[psE][:], ones16[:], oq[:], start=True, stop=True), [oqk, "ones16"], [PK[psE]])
                            rr, rrk = r32.get()
                            M.op('act', lambda e: e.activation(out=rr[:], in_=ps[psE][:], func=AF.Ln, bias=EPS, scale=1.0 / 128), [PK[psE]], [rrk])
                            M.op('act', lambda e: e.activation(out=rr[:], in_=rr[:], func=AF.Exp, scale=-0.5), [rrk], [rrk])
                            t, tk2 = r32.get()
                            M.op('dve', lambda e: e.scalar_tensor_tensor(out=t[:], in0=ps[bo][:], scalar=HGN, in1=rr[:], op0=ALU.mult, op1=ALU.mult),
                                 [PK[bo], rrk, "par"], [tk2])
                            o16, o16k = r16.get()
                            M.op('dve', lambda e: e.tensor_tensor(out=o16[:].rearrange("p (h t) -> p h t", h=2), in0=t[:].rearrange("p (h t) -> p h t", h=2),
                                                                  in1=sg[:, 2 * hp:2 * hp + 2, half * 256:(half + 1) * 256], op=ALU.mult), [tk2, sgk], [o16k])
                            M.dma('sp', OHGv[:, 2 * hp:2 * hp + 2, tcols], o16[:].rearrange("p (h t) -> p h t", h=2), reads=[o16k], writes=[("OHG", j, half, hp)])

                NG = NB * 16
                load_block(0)
                a_cur = stage_a(0)
                for g in range(NG):
                    j, c = divmod(g, 16)
                    if c == 0 and j + 1 < NB:
                        load_block(j + 1)
                    a_nxt = stage_a(g + 1) if g + 1 < NG else None
                    stage_b(g, a_cur)
                    if g % 4 == 3:
                        conv_step(1)
                    a_cur = a_nxt
                M.barrier()

            if want("p2b"):
              with contextlib.ExitStack() as es:
                kp_r = Ring(es, nc, "kp", [66, 2, S], BF16, 2)
                qp_r = Ring(es, nc, "qp", [66, 2, 512], BF16, 3)
                if pfv is None:
                    vsb = es.enter_context(nc.sbuf_tensor(_uname("vsb"), [128, NT, 512], BF16))
                pT_r = Ring(es, nc, "pT", [128, 512], BF16, 4)
                rr_r = Ring(es, nc, "arr", [128, 512], F32, 3)
                tn_r = Ring(es, nc, "atn", [128, 512], F32, 4)
                o_r = Ring(es, nc, "ao", [128, 512], F32, 2)
                r16 = Ring(es, nc, "r16", [128, 512], BF16, 3)
                if pfv is None:
                    M.dma('sp', vsb[:], VAv[:, :, :], reads=[("VA", j) for j in range(NB)], writes=["vsb"])
                for t_, k_ in zip(kp_r.t, kp_r.k):
                    M.op('dve', lambda e, t_=t_: e.memset(t_[64:66, :, :], 1.0), [], [k_])
                scb = [0, 1, 2]
                sci = [0]
                pairs = [(3, 4), (5, 6)]
                defer = [None]
                for h in range(NH):
                    kp, kpk = kp_r.get()
                    M.dma('sp', kp[0:64, :, :], KA[h].rearrange("c d s -> d c s"), reads=[("KA", j) for j in range(NB)], writes=[kpk])
                    for j in range(NB):
                        cols = slice(j * 512, (j + 1) * 512)
                        qp, qpk = qp_r.get()
                        M.dma('sp', qp[0:64, :, :], QA[h].rearrange("c d s -> d c s")[:, :, cols], reads=[("QA", j)], writes=[qpk])
                        for c in range(2):
                            M.op('pool', lambda e, c=c, qp=qp, h=h: e.tensor_copy(out=qp[64:66, c, :], in_=qrow16[64:66, h * 512:(h + 1) * 512]),
                                 ["qrow16"], [qpk])
                        steps = [(c, kt) for c in range(2) for kt in range(4 * j + 4)]
                        tn = [tn_r.get(), tn_r.get()]
                        conv_step(1)

                        def emit_sc(st):
                            c, kt = st
                            m = kt - 4 * j
                            c0 = 128 * m if m > 0 else 0
                            b = scb[sci[0] % 3]
                            sci[0] += 1
                            M.op('pe', lambda e: e.matmul(ps[b][:, c0:512], kp[:, c, kt * 128:(kt + 1) * 128], qp[:, c, c0:512], start=True, stop=True),
                                 [kpk, qpk], [PK[b]])
                            return b, c0, m

                        def emit_rest(st, info):
                            c, kt = st
                            b, c0, m = info
                            bO_, bL_ = pairs[c]
                            pT, pTk = pT_r.get()
                            idx = kt - 4 * j + 28
                            M.op('act', lambda e: e.activation(out=pT[:, c0:512], in_=ps[b][:, c0:512], func=AF.Exp,
                                                               bias=kbias[:, h * 32 + idx:h * 32 + idx + 1], scale=1.0), [PK[b], "cst"], [pTk])
                            if m >= 0:
                                M.op('dve', lambda e: e.tensor_tensor(out=pT[:, c0:c0 + 128], in0=pT[:, c0:c0 + 128], in1=tri16[:], op=ALU.mult),
                                     [pTk, "tri16"], [pTk])
                            first = (kt == 0)
                            last = (kt == 4 * j + 3)
                            M.op('pe', lambda e: e.matmul(ps[bO_][:, c0:512], vsb[:, kt, h * 128:(h + 1) * 128], pT[:, c0:512], start=first, stop=last,
                                                          skip_group_check=True), ["vsb", pTk], [PK[bO_]], signal=False)
                            M.op('pe', lambda e: e.matmul(ps[bL_][:, c0:512], ones16[:], pT[:, c0:512], start=first, stop=last,
                                                          skip_group_check=True), ["ones16", pTk], [PK[bL_]], signal=True)
                            if last:
                                t_, tk_ = tn[c]
                                M.op('dve', lambda e: e.reciprocal(out=t_[:], in_=ps[bL_][:]), [PK[bL_]], [tk_])
                                M.op('dve', lambda e: e.tensor_tensor(out=t_[:], in0=ps[bO_][:], in1=t_[:], op=ALU.mult), [PK[bO_], tk_], [tk_])

                        def mk_epi(h=h, cols=cols, tn=tn, j=j):
                            def f():
                                (r0, r0k), (r1, r1k) = tn
                                o, ok_ = o_r.get()
                                M.op('dve', lambda e: e.scalar_tensor_tensor(out=o[:], in0=r1[:], scalar=NLAM, in1=r0[:], op0=ALU.mult, op1=ALU.add),
                                     [r0k, r1k] + PKEY, [ok_])
                                oq, oqk = r16.get()
                                M.op('act', lambda e: e.activation(out=oq[:], in_=o[:], func=AF.Square), [ok_], [oqk])
                                M.op('pe', lambda e: e.matmul(ps[7][:], ones16[:], oq[:], start=True, stop=True), [oqk, "ones16"], [PK[7]])
                                rr, rrk = rr_r.get()
                                M.op('act', lambda e: e.activation(out=rr[:], in_=ps[7][:], func=AF.Ln, bias=EPS, scale=1.0 / 128), [PK[7]], [rrk])
                                M.op('act', lambda e: e.activation(out=rr[:], in_=rr[:], func=AF.Exp, scale=-0.5), [rrk], [rrk])
                                o16, o16k = r16.get()
                                M.op('dve', lambda e: e.scalar_tensor_tensor(out=o16[:], in0=o[:], scalar=GSUB, in1=rr[:], op0=ALU.mult, op1=ALU.mult),
                                     [ok_, rrk] + PKEY, [o16k])
                                M.dma('sp', ODAv[:, h, cols], o16[:], reads=[o16k], writes=[("ODA", j)])
                            return f

                        LA = 2
                        infos = [emit_sc(steps[i]) for i in range(min(LA, len(steps)))]
                        for i, st in enumerate(steps):
                            if i + LA < len(steps):
                                infos.append(emit_sc(steps[i + LA]))
                            emit_rest(st, infos[i])
                            if i == 3 and defer[0] is not None:
                                defer[0]()
                                defer[0] = None
                        if defer[0] is not None:
                            defer[0]()
                        defer[0] = mk_epi()
                defer[0]()
                M.barrier()
              if pfv is not None:
                  pfv.close()

            moe = (l % 2 == 1)
            if want("p3"):
              with contextlib.ExitStack() as es:
                if pf3 is None:
                    wpa = es.enter_context(nc.sbuf_tensor(_uname("wpa"), [128, 4, D], BF16))
                    wpb = es.enter_context(nc.sbuf_tensor(_uname("wpb"), [128, 4, D], BF16))
                    wo = es.enter_context(nc.sbuf_tensor(_uname("wo"), [128, 8, D], BF16))
                    M.dma('pool', wpa[:], w_pa[l].rearrange("(kc p) n -> p kc n", p=128), writes=["wpa"])
                    M.dma('pool', wpb[:], w_pb[l].rearrange("(kc p) n -> p kc n", p=128), writes=["wpb"])
                    M.dma('pool', wo[:], w_o[l].rearrange("(kc p) n -> p kc n", p=128), writes=["wo"])
                if moe:
                    rt32 = es.enter_context(nc.sbuf_tensor(_uname("rt32"), [128, 8, NE], F32))
                    M.dma('sp', rt32[:], router[l // 2].rearrange("(kc p) e -> p kc e", p=128), writes=["rt32"])
                    h32_r = Ring(es, nc, "h32", [128, 8, 512], F32, 1)
                    rs8 = Ring(es, nc, "rs8", [128, 8], F32, 8)
                    rs1 = Ring(es, nc, "rs1", [128, 1], F32, 8)
                    rs16 = Ring(es, nc, "rs16", [128, 8], BF16, 4)
                    zt = es.enter_context(nc.sbuf_tensor(_uname("zt"), [128, 2, D], BF16))
                    M.op('dve', lambda e: e.memset(zt[:], 0.0), [], ["zt"])
                    HSz = HSLOT.rearrange("(n p) d -> p n d", p=128)
                    for n0 in range(0, NSLOT // 128, 2):
                        M.dma('sp', HSz[:, n0:n0 + 2, :], zt[:], reads=["zt"], writes=[("HSZ", n0)])
                    htok_r = Ring(es, nc, "htok", [128, D], BF16, 2)
                    M.op('dve', lambda e: e.memset(rbase[:], 0.0), [], ["rbase"])
                fold_ada = (l == 0 and nlayers > 1)
                if fold_ada:
                    awr2 = Ring(es, nc, "aw2", [128, 8, 768], F32, 1)
                    lt2 = es.enter_context(nc.sbuf_tensor(_uname("lt2"), [128, 64], F32))
                oh_r = Ring(es, nc, "oh", [128, 4, 512], BF16, 2)
                od_r = Ring(es, nc, "od", [128, 4, 512], BF16, 2)
                ga_r = Ring(es, nc, "ga", [128, 8, 512], BF16, 2)
                gb_r = Ring(es, nc, "gb", [128, 8, 512], BF16, 2)
                xs_r = Ring(es, nc, "xs", [128, 8, 512], F32, 2)
                yT_r = Ring(es, nc, "yT", [128, 8, 512], BF16, 2)
                sq = es.enter_context(nc.sbuf_tensor(_uname("sq"), [128, 8, 512], BF16))
                hT_r = Ring(es, nc, "hT", [128, 8, 512], BF16, 2)
                r32 = Ring(es, nc, "r32", [128, 512], F32, 4)
                rsr = Ring(es, nc, "rsr", [128, 512], F32, 1)
                pb = [1, 2, 3, 4, 5, 6]
                pbi = [0]

                def nb_():
                    b = pb[pbi[0] % len(pb)]
                    pbi[0] += 1
                    return b

                def p3_load(j):
                    cols = slice(j * 512, (j + 1) * 512)
                    oh, ohk = oh_r.get()
                    od, odk = od_r.get()
                    ga, gak = ga_r.get()
                    gb, gbk = gb_r.get()
                    xs, xsk = xs_r.get()
                    M.dma('sp', oh[:], OHGv[:, :, cols], reads=[("OHG", j, hf_, hp_) for hf_ in range(2) for hp_ in range(2)], writes=[ohk])
                    M.dma('sp', od[:], ODAv[:, :, cols], reads=[("ODA", j)], writes=[odk])
                    M.dma('sp', ga[:], GAv[:, :, cols], reads=[("GA", j)], writes=[gak])
                    M.dma('sp', gb[:], GBv[:, :, cols], reads=[("GB", j)], writes=[gbk])
                    M.dma('sp', xs[:], xTv[:, :, cols], reads=[("xT", j)], writes=K8(xsk))
                    return (j, cols, oh, ohk, od, odk, ga, gak, gb, gbk, xs, xsk)
                def p3_mix(L):
                    j, cols, oh, ohk, od, odk, ga, gak, gb, gbk, xs, xsk = L
                    yT, yTk = yT_r.get()
                    for dc in range(8):
                        ba = nb_()
                        bb = nb_()
                        for kc in range(4):
                            M.op('pe', lambda e, kc=kc, dc=dc, ba=ba: e.matmul(ps[ba][:], wpa[:, kc, dc * 128:(dc + 1) * 128], oh[:, kc, :], start=(kc == 0), stop=(kc == 3)),
                                 ["wpa", ohk], [PK[ba]], signal=(kc == 3))
                        for kc in range(4):
                            M.op('pe', lambda e, kc=kc, dc=dc, bb=bb: e.matmul(ps[bb][:], wpb[:, kc, dc * 128:(dc + 1) * 128], od[:, kc, :], start=(kc == 0), stop=(kc == 3)),
                                 ["wpb", odk], [PK[bb]], signal=(kc == 3))
                        t1, t1k = r32.get()
                        t2, t2k = r32.get()
                        M.op('dve', lambda e, t1=t1, ba=ba, dc=dc: e.tensor_tensor(out=t1[:], in0=ps[ba][:], in1=ga[:, dc, :], op=ALU.mult), [PK[ba], gak], [t1k])
                        M.op('dve', lambda e, t2=t2, bb=bb, dc=dc: e.tensor_tensor(out=t2[:], in0=ps[bb][:], in1=gb[:, dc, :], op=ALU.mult), [PK[bb], gbk], [t2k])
                        M.op('pool', lambda e, t1=t1, t2=t2, dc=dc: e.tensor_tensor(out=yT[:, dc, :], in0=t1[:], in1=t2[:], op=ALU.add), [t1k, t2k], [(yTk, dc)])
                    return (yT, yTk)
                def p3_out(L, Y):
                    j, cols, oh, ohk, od, odk, ga, gak, gb, gbk, xs, xsk = L
                    yT, yTk = Y
                    for dc in range(8):
                        b = nb_()
                        for kc in range(8):
                            M.op('pe', lambda e, kc=kc, dc=dc, b=b: e.matmul(ps[b][:], wo[:, kc, dc * 128:(dc + 1) * 128], yT[:, kc, :], start=(kc == 0), stop=(kc == 7)),
                                 ["wo", (yTk, kc)], [PK[b]], signal=(kc == 7))
                        M.op('dve', lambda e, dc=dc, b=b: e.scalar_tensor_tensor(out=xs[:, dc, :], in0=ps[b][:], scalar=G1[:, dc:dc + 1], in1=xs[:, dc, :],
                                                                              op0=ALU.mult, op1=ALU.add), [PK[b], (xsk, dc)] + PKEY, [(xsk, dc)])
                    M.dma('sp', xTv[:, :, cols], xs[:], reads=K8(xsk), writes=[("xT", j)])
                def p3_norm(L):
                    j, cols, oh, ohk, od, odk, ga, gak, gb, gbk, xs, xsk = L
                    hT, hTk = hT_r.get()
                    if moe:
                        h32, h32k = h32_r.get()
                        norm_block(xs, xsk, A2, B2, PKEY[0], hT, hTk, sq, "sq", r32, 0, rsr, h32=(h32, h32k))
                    else:
                        norm_block(xs, xsk, A2, B2, PKEY[0], hT, hTk, sq, "sq", r32, 0, rsr)
                    if not moe:
                        M.dma('sp', H2v[:, :, cols], hT[:], reads=K8(hTk), writes=[("H2", j)])
                    if moe:
                        for tt in range(4):
                            t_ = j * 4 + tt
                            for kc in range(8):
                                M.op('pe', lambda e, kc=kc, tt=tt: e.matmul(ps[7][:, 0:NE], h32[:, kc, tt * 128:(tt + 1) * 128], rt32[:, kc, :],
                                                                          start=(kc == 0), stop=(kc == 7)), [(h32k, kc), "rt32"], [PK[7]], signal=(kc == 7))
                            lg, lgk = rs8.get()
                            M.op('dve', lambda e, lg=lg: e.tensor_copy(out=lg[:], in_=ps[7][:, 0:NE]), [PK[7]], [lgk])
                            m1, m1k = rs1.get()
                            M.op('dve', lambda e, lg=lg, m1=m1: e.reduce_max(out=m1[:], in_=lg[:], axis=AX.X), [lgk], [m1k])
                            RK = ("route", t_)
                            M.op('dve', lambda e, lg=lg, m1=m1: e.tensor_scalar(out=m1_all[:, t_, :], in0=lg[:], scalar1=m1[:, 0:1], scalar2=None, op0=ALU.is_equal), [lgk, m1k], [RK])
                            eq, eqk = rs8.get()
                            M.op('dve', lambda e, lg=lg, eq=eq: e.scalar_tensor_tensor(out=eq[:], in0=m1_all[:, t_, :], scalar=-1e30, in1=lg[:], op0=ALU.mult, op1=ALU.add), [lgk, RK], [eqk])
                            m2, m2k = rs1.get()
                            M.op('dve', lambda e, eq=eq, m2=m2: e.reduce_max(out=m2[:], in_=eq[:], axis=AX.X), [eqk], [m2k])
                            M.op('dve', lambda e, lg=lg, m2=m2: e.tensor_scalar(out=sel_all[:, t_, :], in0=lg[:], scalar1=m2[:, 0:1], scalar2=None, op0=ALU.is_ge), [lgk, m2k], [RK])
                            M.op('dve', lambda e, m1=m1: e.tensor_scalar(out=m1[:], in0=m1[:], scalar1=-1.0, scalar2=None, op0=ALU.mult), [m1k], [m1k])
                            ex, exk = rs8.get()
                            M.op('act', lambda e, lg=lg, m1=m1, ex=ex: e.activation(out=ex[:], in_=lg[:], func=AF.Exp, bias=m1[:, 0:1], scale=1.0), [lgk, m1k], [exk])
                            M.op('dve', lambda e, ex=ex: e.tensor_tensor(out=ex[:], in0=ex[:], in1=sel_all[:, t_, :], op=ALU.mult), [exk, RK], [exk])
                            M.op('dve', lambda e, ex=ex, m2=m2: e.reduce_sum(out=m2[:], in_=ex[:], axis=AX.X), [exk, m2k], [m2k])
                            M.op('dve', lambda e, m2=m2: e.reciprocal(out=m2[:], in_=m2[:]), [m2k], [m2k])
                            M.op('dve', lambda e, ex=ex, m2=m2: e.tensor_scalar(out=comb_all[:, t_, :], in0=ex[:], scalar1=m2[:, 0:1], scalar2=None, op0=ALU.mult), [exk, m2k], [RK])
                            s16, s16k = rs16.get()
                            M.op('dve', lambda e, s16=s16: e.tensor_copy(out=s16[:], in_=sel_all[:, t_, :]), [RK], [s16k])
                            M.op('pe', lambda e, s16=s16: e.matmul(ps[7][:, 16:16 + NE], us16[:], s16[:], start=True, stop=True), [s16k, "us16"], [PK[7]], signal=False)
                            M.op('pe', lambda e, s16=s16: e.matmul(ps[7][:, 32:32 + NE], ones16[:], s16[:], start=True, stop=True), [s16k, "ones16"], [PK[7]])
                            M.op('dve', lambda e: e.tensor_tensor(out=rank_all[:, t_, :], in0=ps[7][:, 16:16 + NE], in1=rbase[:], op=ALU.add), [PK[7], "rbase"], [RK])
                            M.op('dve', lambda e: e.tensor_tensor(out=rbase[:], in0=ps[7][:, 32:32 + NE], in1=rbase[:], op=ALU.add), [PK[7], "rbase"], ["rbase"])
                            ht, htk = htok_r.get()
                            psb16 = ps[7][:].bitcast(BF16)
                            for kc in range(8):
                                M.op('pe', lambda e, kc=kc, tt=tt: e.transpose(psb16[:, kc * 128:(kc + 1) * 128], hT[:, kc, tt * 128:(tt + 1) * 128], id16[:]),
                                     [(hTk, kc), "id16"], [PK[7]], signal=(kc == 7))
                            M.op('act', lambda e, ht=ht: e.copy(out=ht[:], in_=psb16[:, 0:1024]), [PK[7]], [htk])
                            M.dma('sp', H2TOK[t_ * 128:(t_ + 1) * 128, :], ht[:], reads=[htk], writes=[("H2TOK", t_)])
                L_cur = p3_load(0)
                Y_cur = p3_mix(L_cur)
                for j in range(NB):
                    if l == 0:
                        conv_step(2)
                    if fold_ada:
                        gl = ([j] if j < 8 else []) if NB >= 8 else [g_ for g_ in range(8) if g_ % NB == j]
                        for g_ in gl:
                            ada_group(1, g_, awr2, 7)
                        if j == NB - 1:
                            ada_finish(1, lt2, 7)
                    L_nxt = p3_load(j + 1) if j + 1 < NB else None
                    p3_out(L_cur, Y_cur)
                    Y_nxt = p3_mix(L_nxt) if L_nxt is not None else None
                    p3_norm(L_cur)
                    L_cur, Y_cur = L_nxt, Y_nxt
                M.barrier()
              if pf3 is not None:
                  pf3.close()

            if want("p4") and not moe:
              last = (l == nlayers - 1)
              if not last and want("p1"):
                  win_prefetch(l + 1)
              with contextlib.ExitStack() as es:
                TBF = min(S, 1024)
                NSB = TBF // 512
                if moe:
                    nexp, dff, FG = NE, DFE, 4
                    W1 = lambda e_: m_w1[l // 2, e_]
                    W3 = lambda e_: m_w3[l // 2, e_]
                    W2 = lambda e_: m_w2[l // 2, e_]
                else:
                    nexp, dff, FG = 1, DFF, 2
                    W1 = lambda e_: f_w1[l // 2]
                    W3 = lambda e_: f_w3[l // 2]
                    W2 = lambda e_: f_w2[l // 2]
                NFG = dff // (FG * 128)
                FW = FG * 128
                h2 = es.enter_context(nc.sbuf_tensor(_uname("h2"), [128, 8, TBF], BF16))
                yacc = es.enter_context(nc.sbuf_tensor(_uname("yacc"), [128, 8, TBF], F32))
                w1_r = Ring(es, nc, "w1g", [128, 8, FW], BF16, 2)
                w3_r = Ring(es, nc, "w3g", [128, 8, FW], BF16, 2)
                w2_r = Ring(es, nc, "w2g", [128, FG, D], BF16, 2)
                aT_r = Ring(es, nc, "aT", [128, FG, 512], BF16, 2)
                s_r = Ring(es, nc, "sil", [128, 512], F32, 2)
                if moe:
                    t_r = Ring(es, nc, "tt", [128, 512], F32, 3)
                    cb_r = Ring(es, nc, "cb", [128, TBF], F32, 2)
                if last:
                    xs_r = Ring(es, nc, "xs", [128, 8, 512], F32, 1)
                    ot_r = Ring(es, nc, "ot", [128, D], F32, 2)
                else:
                    xc_r = Ring(es, nc, "xc", [128, 512], F32, 3)
                ub = [0, 1, 2, 3]
                ubi = [0]
                yb = [4, 5, 6]
                ybi = [0]
                for p in range(S // TBF):
                    tc0 = p * TBF
                    M.dma('sp', h2[:], H2v[:, :, tc0:tc0 + TBF], reads=[("H2", jj) for jj in range(NB)], writes=["h2"])
                    yfirst = [True] * (NSB * 8)
                    groups = [(e_, fg) for e_ in range(nexp) for fg in range(NFG)]
                    pend = None
                    cb = cbk = None
                    for gi, (e_, fg) in enumerate(groups):
                        if gi % 2 == 0:
                            conv_step(1)
                        if moe and fg == 0:
                            cb, cbk = cb_r.get()
                            M.dma('sp', cb[:], COMBT[e_:e_ + 1, tc0:tc0 + TBF].partition_broadcast(128), reads=[("COMBT", jj) for jj in range(NB)], writes=[cbk])
                        w1g, w1k = w1_r.get()
                        w3g, w3k = w3_r.get()
                        w2g, w2k = w2_r.get()
                        fcs = slice(fg * FW, (fg + 1) * FW)
                        M.dma('pool', w1g[:], W1(e_).rearrange("(kc p) f -> p kc f", p=128)[:, :, fcs], writes=[w1k])
                        M.dma('pool', w3g[:], W3(e_).rearrange("(kc p) f -> p kc f", p=128)[:, :, fcs], writes=[w3k])
                        M.dma('pool', w2g[:], W2(e_)[fg * FW:(fg + 1) * FW, :].rearrange("(fc p) d -> p fc d", p=128), writes=[w2k])
                        for sbi in range(NSB):
                            sc_ = slice(sbi * 512, (sbi + 1) * 512)
                            aT, aTk = aT_r.get()
                            for fc in range(FG):
                                b1 = ub[ubi[0] % 4]
                                b3 = ub[(ubi[0] + 1) % 4]
                                ubi[0] += 2
                                for kc in range(8):
                                    M.op('pe', lambda e, kc=kc, fc=fc, b1=b1, w1g=w1g: e.matmul(ps[b1][:], w1g[:, kc, fc * 128:(fc + 1) * 128], h2[:, kc, sc_],
                                                                                           start=(kc == 0), stop=(kc == 7)), [w1k, "h2"], [PK[b1]], signal=(kc == 7))
                                for kc in range(8):
                                    M.op('pe', lambda e, kc=kc, fc=fc, b3=b3, w3g=w3g: e.matmul(ps[b3][:], w3g[:, kc, fc * 128:(fc + 1) * 128], h2[:, kc, sc_],
                                                                                           start=(kc == 0), stop=(kc == 7)), [w3k, "h2"], [PK[b3]], signal=(kc == 7))
                                s, sk = s_r.get()
                                M.op('act', lambda e, s=s, b1=b1: e.activation(out=s[:], in_=ps[b1][:], func=AF.Silu), [PK[b1]], [sk])
                                if moe:
                                    t, tk = t_r.get()
                                    M.op('dve', lambda e, s=s, b3=b3, t=t: e.tensor_tensor(out=t[:], in0=ps[b3][:], in1=s[:], op=ALU.mult), [PK[b3], sk], [tk])
                                    M.op('pool', lambda e, t=t, fc=fc, aT=aT, cb=cb: e.tensor_tensor(out=aT[:, fc, :], in0=t[:], in1=cb[:, sc_], op=ALU.mult), [tk, cbk], [aTk])
                                else:
                                    M.op('dve', lambda e, s=s, b3=b3, fc=fc, aT=aT: e.tensor_tensor(out=aT[:, fc, :], in0=ps[b3][:], in1=s[:], op=ALU.mult), [PK[b3], sk], [aTk])
                            if pend is not None:
                                pend()

                            def mk_pend(aT=aT, aTk=aTk, w2g=w2g, w2k=w2k, sbi=sbi, sc_=sc_):
                                def f():
                                    for dc in range(8):
                                        b = yb[ybi[0] % 3]
                                        ybi[0] += 1
                                        for fc in range(FG):
                                            M.op('pe', lambda e, fc=fc, dc=dc, b=b: e.matmul(ps[b][:], w2g[:, fc, dc * 128:(dc + 1) * 128], aT[:, fc, :],
                                                                                           start=(fc == 0), stop=(fc == FG - 1)), [w2k, aTk], [PK[b]], signal=(fc == FG - 1))
                                        yk = ("yacc", sbi, dc)
                                        if yfirst[sbi * 8 + dc]:
                                            yfirst[sbi * 8 + dc] = False
                                            M.op('dve', lambda e, dc=dc, b=b: e.tensor_copy(out=yacc[:, dc, sc_], in_=ps[b][:]), [PK[b]], [yk])
                                        else:
                                            M.op('dve', lambda e, dc=dc, b=b: e.tensor_tensor(out=yacc[:, dc, sc_], in0=ps[b][:], in1=yacc[:, dc, sc_], op=ALU.add),
                                                 [PK[b], yk], [yk])
                                return f
                            pend = mk_pend()
                    pend()
                    for sbi in range(NSB):
                        sc_ = slice(sbi * 512, (sbi + 1) * 512)
                        jb = (tc0 // 512) + sbi
                        gcols = slice(tc0 + sbi * 512, tc0 + (sbi + 1) * 512)
                        if not last:
                            for dc in range(8):
                                xc, xck = xc_r.get()
                                M.dma('sp', xc[:], xTv[:, dc, gcols], reads=[("xT", jb)], writes=[xck])
                                M.op('dve', lambda e, dc=dc, xc=xc: e.scalar_tensor_tensor(out=yacc[:, dc, sc_], in0=yacc[:, dc, sc_], scalar=G2[:, dc:dc + 1], in1=xc[:],
                                                                                        op0=ALU.mult, op1=ALU.add), [("yacc", sbi, dc), xck] + PKEY, [("yacc", sbi, dc)])
                            M.dma('sp', xTv[:, :, gcols], yacc[:, :, sc_], reads=[("yacc", sbi, dc) for dc in range(8)], writes=[("xT", jb)])
                            continue
                        xs, xsk = xs_r.get()
                        M.dma('sp', xs[:], xTv[:, :, gcols], reads=[("xT", jb)], writes=K8(xsk))
                        for dc in range(8):
                            M.op('dve', lambda e, dc=dc: e.scalar_tensor_tensor(out=xs[:, dc, :], in0=yacc[:, dc, sc_], scalar=G2[:, dc:dc + 1], in1=xs[:, dc, :],
                                                                             op0=ALU.mult, op1=ALU.add), [("yacc", sbi, dc), (xsk, dc)] + PKEY, [(xsk, dc)])
                        if True:
                            for tt in range(4):
                                ot, otk = ot_r.get()
                                for half in range(2):
                                    b = ub[ubi[0] % 4]
                                    ubi[0] += 1
                                    for q in range(4):
                                        dc = half * 4 + q
                                        M.op('pe', lambda e, b=b, q=q, dc=dc, tt=tt: e.transpose(ps[b][:, q * 128:(q + 1) * 128], xs[:, dc, tt * 128:(tt + 1) * 128], id32),
                                             [(xsk, dc), "cst"], [PK[b]], signal=(q == 3))
                                    if half == 0:
                                        M.op('act', lambda e, b=b, ot=ot: e.copy(out=ot[:, 0:512], in_=ps[b][:]), [PK[b]], [otk])
                                    else:
                                        M.op('dve', lambda e, b=b, ot=ot: e.tensor_copy(out=ot[:, 512:1024], in_=ps[b][:]), [PK[b]], [otk])
                                r0 = tc0 + sbi * 512 + tt * 128
                                M.dma('sp', out[r0:r0 + 128, :], ot[:], reads=[otk], writes=[("out", r0)])
                M.barrier()
            if want("p4") and moe:
              assert l == nlayers - 1
              with contextlib.ExitStack() as es:
                nexp, dff, FG = NE, DFE, 4
                NFG = dff // (FG * 128)
                FW = FG * 128
                NSUB = TS // 128
                ei = l // 2
                conv_step(10 ** 6)
                assert FG == MFG and cconv[0] == NE * MNFG * 3 * 16, cconv[0]
                for en_ in ('pool',):
                    nc.gpsimd.wait_ge(csem, cconv[0])
                sm8 = Ring(es, nc, "sm8", [128, NE], F32, 6)
                big = Ring(es, nc, "big", [128, NT, NE], F32, 3)
                t88 = es.enter_context(nc.sbuf_tensor(_uname("t88"), [128, NE, 8], F32))
                tx = es.enter_context(nc.sbuf_tensor(_uname("tx"), [128, TMAX, NE], F32))
                slot1f = es.enter_context(nc.sbuf_tensor(_uname("slot1f"), [128, NT], F32))
                slot2f = es.enter_context(nc.sbuf_tensor(_uname("slot2f"), [128, NT], F32))
                slot1i = es.enter_context(nc.sbuf_tensor(_uname("slot1i"), [128, NT], mybir.dt.int32))
                slot2i = es.enter_context(nc.sbuf_tensor(_uname("slot2i"), [128, NT], mybir.dt.int32))
                c1 = es.enter_context(nc.sbuf_tensor(_uname("c1"), [128, NT], F32))
                c2 = es.enter_context(nc.sbuf_tensor(_uname("c2"), [128, NT], F32))
                texf = es.enter_context(nc.sbuf_tensor(_uname("texf"), [128, TMAX], F32))
                widf = es.enter_context(nc.sbuf_tensor(_uname("widf"), [128, TMAX, NFG], F32))
                widi = es.enter_context(nc.sbuf_tensor(_uname("widi"), [128, TMAX, NFG], mybir.dt.int32))
                th = cst32[:, C_TH:C_TH + 8]
                tv = cst32[:, C_TV:C_TV + TMAX]
                M.op('dve', lambda e: e.tensor_tensor(out=t88[:], in0=rbase[:].unsqueeze(2).to_broadcast([128, NE, 8]),
                                                      in1=th.unsqueeze(1).to_broadcast([128, NE, 8]), op=ALU.is_gt), ["rbase", "cst"], ["t88"])
                pe_, pek = sm8.get()
                M.op('dve', lambda e: e.tensor_reduce(out=pe_[:], in_=t88[:], axis=AX.X, op=ALU.add), ["t88"], [pek])
                M.op('dve', lambda e: e.tensor_scalar(out=pe_[:], in0=pe_[:], scalar1=float(TS), scalar2=None, op0=ALU.mult), [pek], [pek])
                on8, on8k = sm8.get()
                M.op('dve', lambda e: e.memset(on8[:], 1.0), [], [on8k])
                incl, inclk = sm8.get()
                M.op('dve', lambda e: e.tensor_tensor_scan(out=incl[:], data0=on8[:], data1=pe_[:], initial=0.0, op0=ALU.mult, op1=ALU.add), [on8k, pek], [inclk])
                off, offk = sm8.get()
                M.op('dve', lambda e: e.tensor_tensor(out=off[:], in0=incl[:], in1=pe_[:], op=ALU.subtract), [inclk, pek], [offk])
                RKS = [("route", t_) for t_ in range(NT)]
                slot, slotk = big.get()
                M.op('dve', lambda e: e.tensor_tensor(out=slot[:], in0=rank_all[:], in1=off[:].unsqueeze(1).to_broadcast([128, NT, NE]), op=ALU.add), RKS + [offk], [slotk])
                m2a, m2ak = big.get()
                M.op('dve', lambda e: e.tensor_tensor(out=m2a[:], in0=sel_all[:], in1=m1_all[:], op=ALU.subtract), RKS, [m2ak])
                tmp, tmpk = big.get()
                for (dst, dk, a_, ak, b_, bk) in ((slot1f, "slot1f", slot, slotk, m1_all, None), (slot2f, "slot2f", slot, slotk, m2a, m2ak),
                                                  (c1, "c1", comb_all, None, m1_all, None), (c2, "c2", comb_all, None, m2a, m2ak)):
                    rk = [k for k in (ak, bk) if k is not None]
                    M.op('dve', lambda e, a_=a_, b_=b_: e.tensor_tensor(out=tmp[:], in0=a_[:], in1=b_[:], op=ALU.mult), rk, [tmpk])
                    M.op('dve', lambda e, dst=dst: e.tensor_reduce(out=dst[:], in_=tmp[:], axis=AX.X, op=ALU.add), [tmpk], [dk])
                M.op('dve', lambda e: e.tensor_copy(out=slot1i[:], in_=slot1f[:]), ["slot1f"], ["slot1i"])
                M.op('dve', lambda e: e.tensor_copy(out=slot2i[:], in_=slot2f[:]), ["slot2f"], ["slot2i"])
                M.op('dve', lambda e: e.tensor_tensor(out=tx[:], in0=incl[:].unsqueeze(1).to_broadcast([128, TMAX, NE]),
                                                      in1=tv.unsqueeze(2).to_broadcast([128, TMAX, NE]), op=ALU.is_le), [inclk, "cst"], ["tx"])
                M.op('dve', lambda e: e.tensor_reduce(out=texf[:], in_=tx[:], axis=AX.X, op=ALU.add), ["tx"], ["texf"])
                M.op('dve', lambda e: e.tensor_scalar(out=texf[:], in0=texf[:], scalar1=float(NE - 1), scalar2=None, op0=ALU.min), ["texf"], ["texf"])
                M.op('dve', lambda e: e.tensor_scalar(out=texf[:], in0=texf[:], scalar1=float(NFG * 128), scalar2=cst32[:, C_PI:C_PI + 1], op0=ALU.mult, op1=ALU.add),
                     ["texf", "cst"], ["texf"])
                for fg in range(NFG):
                    M.op('dve', lambda e, fg=fg: e.tensor_scalar(out=widf[:, :, fg], in0=texf[:], scalar1=float(fg * 128), scalar2=None, op0=ALU.add), ["texf"], ["widf"])
                M.op('dve', lambda e: e.tensor_copy(out=widi[:], in_=widf[:]), ["widf"], ["widi"])
                with contextlib.ExitStack() as es2:
                  htk_r = Ring(es2, nc, "htk2", [128, D], BF16, 3)
                  HSZK = []
                  for t_ in range(NT):
                      ht, htk = htk_r.get()
                      M.dma('sp', ht[:], H2TOK[t_ * 128:(t_ + 1) * 128, :], writes=[htk])
                      for (si, sk) in ((slot1i, "slot1i"), (slot2i, "slot2i")):
                          M.dmai(HSLOT[:, :], bass.IndirectOffsetOnAxis(si[:, t_:t_ + 1], 0), ht[:], None, reads=[htk, sk] + HSZK, writes=[("HSLOT", t_, sk)])
                  M.barrier()
                with contextlib.ExitStack() as es2:
                  hs_r = Ring(es2, nc, "hs", [128, NSUB, D], BF16, 2)
                  h2_r = Ring(es2, nc, "h2s", [128, 8, TS], BF16, 2)
                  ya_r = Ring(es2, nc, "yacs", [128, 8, TS], F32, 2)
                  w1_r = Ring(es2, nc, "w1g", [128, 8, FW], BF16, 2)
                  w3_r = Ring(es2, nc, "w3g", [128, 8, FW], BF16, 2)
                  w2_r = Ring(es2, nc, "w2g", [128, FG, D], BF16, 2)
                  aT_r = Ring(es2, nc, "aT", [128, FG, 512], BF16, 2)
                  s_r = Ring(es2, nc, "sil", [128, 512], F32, 3)
                  ys_r = Ring(es2, nc, "ysb", [128, D], F32, 2)
                  ub = [0, 1, 2, 3]
                  ubi = [0]
                  yb = [4, 5, 6]
                  ybi = [0]
                  HSv = HSLOT.rearrange("(i u p) d -> i p u d", p=128, u=NSUB)
                  ps7b = ps[7][:].bitcast(BF16)

                  def load_hs(i):
                      hs, hsk = hs_r.get()
                      M.dma('sp', hs[:], HSv[i], writes=[hsk])
                      return hs, hsk

                  nxt_hs = load_hs(0)
                  pend = None
                  fin = None
                  for i in range(TMAX):
                      hs, hsk = nxt_hs
                      if i + 1 < TMAX:
                          nxt_hs = load_hs(i + 1)
                      h2, h2k = h2_r.get()
                      for u in range(NSUB):
                          for kc in range(8):
                              M.op('pe', lambda e, u=u, kc=kc: e.transpose(ps7b[:, kc * 128:(kc + 1) * 128], hs[:, u, kc * 128:(kc + 1) * 128], id16[:]),
                                   [hsk, "id16"], [PK[7]], signal=(kc == 7))
                          M.op('act', lambda e, u=u: e.copy(out=h2[:, :, u * 128:(u + 1) * 128], in_=ps7b[:, 0:1024].rearrange("p (k t) -> p k t", k=8)),
                               [PK[7]], [(h2k, u)])
                      H2K = [(h2k, u) for u in range(NSUB)]
                      yacc, yak = ya_r.get()
                      yfirst = [True] * 8
                      for fg in range(NFG):
                          w1g, w1k = w1_r.get()
                          w3g, w3k = w3_r.get()
                          w2g, w2k = w2_r.get()
                          fcs = slice(fg * FW, (fg + 1) * FW)
                          ioff = bass.IndirectOffsetOnAxis(widi[:, i, fg:fg + 1], 0)
                          M.dmai(w1g[:].rearrange("p a b -> p (a b)"), None, W1C[:, :], ioff, reads=["widi"], writes=[w1k])
                          M.dmai(w3g[:].rearrange("p a b -> p (a b)"), None, W3C[:, :], ioff, reads=["widi"], writes=[w3k])
                          M.dmai(w2g[:].rearrange("p a b -> p (a b)"), None, W2C[:, :], ioff, reads=["widi"], writes=[w2k])
                          for sbi in range(TS // 512):
                              sc_ = slice(sbi * 512, (sbi + 1) * 512)
                              aT, aTk = aT_r.get()
                              for fc in range(FG):
                                  b1 = ub[ubi[0] % 4]
                                  b3 = ub[(ubi[0] + 1) % 4]
                                  ubi[0] += 2
                                  for kc in range(8):
                                      M.op('pe', lambda e, kc=kc, fc=fc, b1=b1: e.matmul(ps[b1][:], w1g[:, kc, fc * 128:(fc + 1) * 128], h2[:, kc, sc_],
                                                                                       start=(kc == 0), stop=(kc == 7)), [w1k] + H2K, [PK[b1]], signal=(kc == 7))
                                  for kc in range(8):
                                      M.op('pe', lambda e, kc=kc, fc=fc, b3=b3: e.matmul(ps[b3][:], w3g[:, kc, fc * 128:(fc + 1) * 128], h2[:, kc, sc_],
                                                                                       start=(kc == 0), stop=(kc == 7)), [w3k] + H2K, [PK[b3]], signal=(kc == 7))
                                  s_, sk_ = s_r.get()
                                  M.op('act', lambda e, s_=s_, b1=b1: e.activation(out=s_[:], in_=ps[b1][:], func=AF.Silu), [PK[b1]], [sk_])
                                  M.op('dve', lambda e, s_=s_, b3=b3, fc=fc: e.tensor_tensor(out=aT[:, fc, :], in0=ps[b3][:], in1=s_[:], op=ALU.mult), [PK[b3], sk_], [(aTk, fc)])
                              if pend is not None:
                                  pend()
                              if fin is not None:
                                  fin()
                                  fin = None

                              def mk_pend(aT=aT, aTk=aTk, w2g=w2g, w2k=w2k, sc_=sc_, yacc=yacc, yak=yak, yfirst=yfirst):
                                  def f():
                                      for dc in range(8):
                                          b = yb[ybi[0] % 3]
                                          ybi[0] += 1
                                          for fc in range(FG):
                                              M.op('pe', lambda e, fc=fc, dc=dc, b=b: e.matmul(ps[b][:], w2g[:, fc, dc * 128:(dc + 1) * 128], aT[:, fc, :],
                                                                                             start=(fc == 0), stop=(fc == FG - 1)), [w2k, (aTk, fc)], [PK[b]], signal=(fc == FG - 1))
                                          yk = (yak, dc)
                                          if yfirst[dc]:
                                              yfirst[dc] = False
                                              M.op('dve', lambda e, dc=dc, b=b: e.tensor_copy(out=yacc[:, dc, sc_], in_=ps[b][:]), [PK[b]], [yk])
                                          else:
                                              M.op('dve', lambda e, dc=dc, b=b: e.tensor_tensor(out=yacc[:, dc, sc_], in0=ps[b][:], in1=yacc[:, dc, sc_], op=ALU.add),
                                                   [PK[b], yk], [yk])
                                  return f
                              pend = mk_pend()

                      def mk_fin(i=i, yacc=yacc, yak=yak):
                          def f():
                              for dc in range(8):
                                  M.op('act', lambda e, dc=dc: e.activation(out=yacc[:, dc, :], in_=yacc[:, dc, :], func=AF.Copy, scale=G2[:, dc:dc + 1]),
                                       [(yak, dc)] + PKEY, [(yak, dc)])
                              for u in range(NSUB):
                                  ysb, ysk = ys_r.get()
                                  for half in range(2):
                                      for q in range(4):
                                          dc = half * 4 + q
                                          M.op('pe', lambda e, q=q, dc=dc, u=u: e.transpose(ps[7][:, q * 128:(q + 1) * 128], yacc[:, dc, u * 128:(u + 1) * 128], id32),
                                               [(yak, dc), "cst"], [PK[7]], signal=(q == 3))
                                      M.op('act', lambda e, half=half, ysb=ysb: e.copy(out=ysb[:, half * 512:(half + 1) * 512], in_=ps[7][:]), [PK[7]], [ysk])
                                  r0 = i * TS + u * 128
                                  M.dma('sp', YSLOT[r0:r0 + 128, :], ysb[:], reads=[ysk], writes=[("YSLOT", i, u)])
                          return f
                      fin = mk_fin()
                  pend()
                  fin()
                  M.barrier()
                with contextlib.ExitStack() as es2:
                  xs_r = Ring(es2, nc, "xs", [128, 8, 512], F32, 2)
                  y1_r = Ring(es2, nc, "y1", [128, D], F32, 4)
                  y2_r = Ring(es2, nc, "y2", [128, D], F32, 4)
                  ot_r = Ring(es2, nc, "ot", [128, D], F32, 3)
                  for j in range(NB):
                      xs, xsk = xs_r.get()
                      M.dma('sp', xs[:], xTv[:, :, j * 512:(j + 1) * 512], writes=[xsk])
                      for tt in range(4):
                          t_ = j * 4 + tt
                          y1, y1k = y1_r.get()
                          y2, y2k = y2_r.get()
                          M.dmai(y1[:], None, YSLOT[:, :], bass.IndirectOffsetOnAxis(slot1i[:, t_:t_ + 1], 0), reads=["slot1i"], writes=[y1k])
                          M.dmai(y2[:], None, YSLOT[:, :], bass.IndirectOffsetOnAxis(slot2i[:, t_:t_ + 1], 0), reads=["slot2i"], writes=[y2k])
                          ot, otk = ot_r.get()
                          for half in range(2):
                              b = ub[ubi[0] % 4]
                              ubi[0] += 1
                              for q in range(4):
                                  dc = half * 4 + q
                                  M.op('pe', lambda e, b=b, q=q, dc=dc, tt=tt: e.transpose(ps[b][:, q * 128:(q + 1) * 128], xs[:, dc, tt * 128:(tt + 1) * 128], id32),
                                       [xsk, "cst"], [PK[b]], signal=(q == 3))
                              hc = slice(half * 512, (half + 1) * 512)
                              M.op('dve', lambda e, b=b, hc=hc: e.scalar_tensor_tensor(out=ot[:, hc], in0=y1[:, hc], scalar=c1[:, t_:t_ + 1], in1=ps[b][:], op0=ALU.mult, op1=ALU.add),
                                   [y1k, "c1", PK[b]], [(otk, half)])
                              M.op('dve', lambda e, hc=hc: e.scalar_tensor_tensor(out=ot[:, hc], in0=y2[:, hc], scalar=c2[:, t_:t_ + 1], in1=ot[:, hc], op0=ALU.mult, op1=ALU.add),
                                   [y2k, "c2", (otk, half)], [(otk, half)])
                          M.dma('sp', out[t_ * 128:(t_ + 1) * 128, :], ot[:], reads=[(otk, 0), (otk, 1)], writes=[("out", t_)])
                  M.barrier()
        M.finish()
    build.stats = (M.nops, M.nwaits)
    return nc


def make_consts():
    c = np.zeros((128, NCST), np.float32)
    p = np.arange(128)
    c[:, C_ID:C_ID + 128] = np.eye(128, dtype=np.float32)
    c[:, C_BLK:C_BLK + 128] = (p[:, None] // 64 == p[None, :] // 64).astype(np.float32)
    s = np.arange(32)
    m = (s[:, None] <= s[None, :]).astype(np.float32)
    c[0:32, C_M32:C_M32 + 128] = np.tile(m, (1, 4))
    c[:, C_TRI:C_TRI + 128] = (p[None, :] >= p[:, None]).astype(np.float32)
    rm = np.ones(512, np.float32)
    rm[0::32] = 0.0
    c[:, C_RM:C_RM + 512] = rm[None, :]
    for h in range(NH):
        for idx in range(32):
            c[:, C_KB + h * 32 + idx] = SLOPES[h] * (p + 128.0 * (idx - 28))
    c[:, C_US:C_US + 128] = (p[:, None] < p[None, :]).astype(np.float32)
    c[:, C_TH:C_TH + 8] = (np.arange(8) * TS).astype(np.float32)[None, :]
    c[:, C_TV:C_TV + 64] = (np.arange(64) * TS).astype(np.float32)[None, :]
    c[:, C_PI] = p.astype(np.float32)
    qi = np.arange(512)
    lo = (qi % 256).astype(np.float32)
    hi = (qi - qi % 256).astype(np.float32)
    qr = np.zeros((2, NH * 512), np.float32)
    for h in range(NH):
        qr[0, h * 512:(h + 1) * 512] = -SLOPES[h] * lo
        qr[1, h * 512:(h + 1) * 512] = -SLOPES[h] * hi
    return c, qr


def make_par(b, c, ada_b, norm_mix_g, norm_ffn_g, lb_logits, hgrn_norm_g, qn_g, kn_g, lam, subln_g):
    par = np.zeros((128, NPAR), np.float32)
    par[:, 0:8] = c[b].reshape(8, 128).T
    for l in range(2):
        o = 8 + l * PL
        par[:, o:o + 48] = ada_b[l].reshape(48, 128).T
        par[:, o + 48:o + 56] = norm_mix_g[l].reshape(8, 128).T
        par[:, o + 56:o + 64] = norm_ffn_g[l].reshape(8, 128).T
        par[:, o + 64:o + 68] = lb_logits[0].reshape(4, 128).T
        par[:, o + 68:o + 72] = lb_logits[1].reshape(4, 128).T
        par[:, o + 72] = hgrn_norm_g[l]
        par[:, o + 73] = np.tile(qn_g[l], 2)
        par[:, o + 74] = np.tile(kn_g[l], 2)
        par[:, o + 75] = subln_g[l]
        par[:, o + 76:o + 332] = lam[l].reshape(1, 256)
    return par


_NC_CACHE = {}


def kernel(x, c, ada_w, ada_b, norm_mix_g, norm_ffn_g, w_in, hgrn_lb_logits, hgrn_norm_g,
           da_qnorm_g, da_knorm_g, da_lambda, da_subln_g, w_branch_a, w_branch_b, w_out,
           ffn_w1, ffn_w3, ffn_w2, moe_router, moe_w1, moe_w3, moe_w2):
    f = lambda a: np.ascontiguousarray(np.asarray(a, dtype=np.float32))
    x = f(x)
    B, S, _ = x.shape
    cst, qr = make_consts()
    if S not in _NC_CACHE:
        _NC_CACHE[S] = build(S)
    nc = _NC_CACHE[S]
    shared = dict(cst=cst, qrows=qr, ada_w=f(ada_w), w_in=f(w_in), w_pa=f(w_branch_a), w_pb=f(w_branch_b), w_o=f(w_out),
                  ffn_w1=f(ffn_w1), ffn_w3=f(ffn_w3), ffn_w2=f(ffn_w2), router=f(moe_router),
                  moe_w1=f(moe_w1), moe_w3=f(moe_w3), moe_w2=f(moe_w2))
    args = [f(a) for a in (c, ada_b, norm_mix_g, norm_ffn_g, hgrn_lb_logits, hgrn_norm_g, da_qnorm_g, da_knorm_g, da_lambda, da_subln_g)]
    in_maps = []
    for b in range(B):
        m = dict(shared)
        m["x"] = x[b]
        m["par"] = make_par(b, *args)
        in_maps.append(m)
    res = run_bass_kernel_spmd(nc, in_maps, core_ids=list(range(B)))
    return np.stack([np.asarray(r["out"], dtype=np.float32) for r in res.results], axis=0)
```

```python
import math
import contextlib
import numpy as np
import concourse.bass as bass
import concourse.mybir as mybir
from concourse.bass_utils import run_bass_kernel_spmd

F32 = mybir.dt.float32
BF16 = mybir.dt.bfloat16
AF = mybir.ActivationFunctionType
ALU = mybir.AluOpType
AX = mybir.AxisListType

D = 1024
DIN = 5632
NH = 4
DFF = 2816
DFE = 3584
NE = 8
EPS = 1e-6
PL = 332
NPAR = 8 + 2 * PL
C_ID, C_BLK, C_M32, C_TRI, C_RM, C_KB = 0, 128, 256, 384, 512, 1024
C_US, C_TH, C_TV, C_PI = 1152, 1280, 1288, 1352
NCST = 1353
TS = 512
SLOPES = [2.0 ** (-8.0 * (i + 1) / NH) for i in range(NH)]


class MK:
    def __init__(self, nc, es, nds=48):
        self.nc = nc
        self.eng = dict(pe=nc.tensor, act=nc.scalar, dve=nc.vector, pool=nc.gpsimd, sp=nc.sync)
        self.esem = {k: es.enter_context(nc.semaphore("s_" + k)) for k in self.eng}
        self.ecnt = {k: 0 for k in self.eng}
        self.nds = nds
        self.dsem = [es.enter_context(nc.semaphore("d%d" % i)) for i in range(nds)]
        self.dcnt = [0] * nds
        self.dnext = {'sp': 0, 'pool': 0, 'act': 0}
        self.dpool = {'sp': list(range(0, nds - 16)), 'pool': list(range(nds - 16, nds)), 'act': []}
        self.seen = {k: {} for k in self.eng}
        self.lastw = {}
        self.readers = {}
        self.nwaits = 0
        self.nops = 0

    def _sem(self, sk):
        return self.esem[sk[1]] if sk[0] == 'e' else self.dsem[sk[1]]

    def _wait(self, en, ev):
        sk, val = ev
        if val <= 0:
            return
        if sk[0] == 'e' and sk[1] == en and en in ('pe', 'sp'):
            return
        if sk[0] == 'e':
            assert val <= self.ecnt[sk[1]], ("forward wait", en, ev, self.ecnt[sk[1]])
        if self.seen[en].get(sk, 0) >= val:
            return
        self.eng[en].wait_ge(self._sem(sk), val)
        self.seen[en][sk] = val
        self.nwaits += 1

    def _deps(self, en, reads, writes):
        for k in reads:
            ev = self.lastw.get(k)
            if ev is not None:
                self._wait(en, ev)
        for k in writes:
            ev = self.lastw.get(k)
            if ev is not None:
                self._wait(en, ev)
            for sk, val in self.readers.get(k, {}).items():
                self._wait(en, (sk, val))

    def _record(self, ev, reads, writes):
        sk, val = ev
        for k in writes:
            self.lastw[k] = ev
            self.readers[k] = {}
        for k in reads:
            d = self.readers.setdefault(k, {})
            if d.get(sk, 0) < val:
                d[sk] = val

    def op(self, en, fn, reads=(), writes=(), signal=True):
        self._deps(en, reads, writes)
        ins = fn(self.eng[en])
        self.nops += 1
        if signal:
            self.ecnt[en] += 1
            ins.then_inc(self.esem[en], 1)
            ev = (('e', en), self.ecnt[en])
        else:
            ev = (('e', en), self.ecnt[en] + 1)
        self._record(ev, reads, writes)
        return ins

    def dma(self, q, out, in_, reads=(), writes=(), **kw):
        pl = self.dpool[q]
        i = pl[self.dnext[q] % len(pl)]
        self.dnext[q] += 1
        self._wait(q, (('d', i), self.dcnt[i]))
        self._deps(q, reads, writes)
        ins = self.eng[q].dma_start(out=out, in_=in_, **kw)
        ins.then_inc(self.dsem[i], 16)
        self.dcnt[i] += 16
        ev = (('d', i), self.dcnt[i])
        self._record(ev, reads, writes)
        self.nops += 1
        return ins

    def dmai(self, out, out_off, in_, in_off, reads=(), writes=()):
        q = 'pool'
        pl = self.dpool[q]
        i = pl[self.dnext[q] % len(pl)]
        self.dnext[q] += 1
        self._wait(q, (('d', i), self.dcnt[i]))
        self._deps(q, reads, writes)
        ins = self.eng[q].indirect_dma_start(out, out_off, in_, in_off)
        ins.then_inc(self.dsem[i], 16)
        self.dcnt[i] += 16
        ev = (('d', i), self.dcnt[i])
        self._record(ev, reads, writes)
        self.nops += 1
        return ins

    def note_read(self, en, reads):
        self._deps(en, reads, ())

    def barrier(self):
        for en in self.eng:
            for fn in self.eng:
                if fn != en:
                    self._wait(en, (('e', fn), self.ecnt[fn]))
            for i in range(self.nds):
                self._wait(en, (('d', i), self.dcnt[i]))
        self.lastw = {}
        self.readers = {}

    def finish(self):
        for i in range(self.nds):
            self._wait('sp', (('d', i), self.dcnt[i]))
        for fn in self.eng:
            if fn != 'sp':
                self._wait('sp', (('e', fn), self.ecnt[fn]))


_UID = [0]


def _uname(name):
    _UID[0] += 1
    return "%s_u%d" % (name, _UID[0])


class Ring:
    def __init__(self, es, nc, name, shape, dt, n):
        self.t = [es.enter_context(nc.sbuf_tensor(_uname("%s%d" % (name, i)), shape, dt)) for i in range(n)]
        self.k = [(name, i) for i in range(n)]
        self.i = 0

    def get(self):
        i = self.i
        self.i = (i + 1) % len(self.t)
        return self.t[i], self.k[i]


def build(S=4096, dbg=(), nlayers=2, phases=None):
    NB = S // 512
    NT = S // 128
    NCH = S // 32
    nc = bass.Bass("TRN2", target_bir_lowering=False)

    def din(name, shape):
        return nc.dram_tensor(name, shape, F32, kind="ExternalInput").ap()

    x = din("x", [S, D])
    par = din("par", [128, NPAR])
    cst = din("cst", [128, NCST])
    qrows = din("qrows", [2, NH * 512])
    ada_w = din("ada_w", [2, D, 6 * D])
    w_in = din("w_in", [2, D, DIN])
    w_pa = din("w_pa", [2, 512, D])
    w_pb = din("w_pb", [2, 512, D])
    w_o = din("w_o", [2, D, D])
    f_w1 = din("ffn_w1", [1, D, DFF])
    f_w3 = din("ffn_w3", [1, D, DFF])
    f_w2 = din("ffn_w2", [1, DFF, D])
    router = din("router", [1, D, NE])
    m_w1 = din("moe_w1", [1, NE, D, DFE])
    m_w3 = din("moe_w3", [1, NE, D, DFE])
    m_w2 = din("moe_w2", [1, NE, DFE, D])
    out = nc.dram_tensor("out", [S, D], F32, kind="ExternalOutput").ap()

    def scr(name, shape, dt):
        kind = "ExternalOutput" if name in dbg else "Internal"
        return nc.dram_tensor(name, shape, dt, kind=kind).ap()

    xT = scr("xT", [D, S], F32)
    QH = scr("QH", [NH, 128, S], BF16)
    KD = scr("KD", [NH, 128, S], BF16)
    KDEC = scr("KDEC", [NH, 128, S], BF16)
    VHT = scr("VHT", [NH, 128, S], BF16)
    SG = scr("SG", [NH, 128, S], BF16)
    EBL = scr("EBL", [NH, 128, NCH], F32)
    QA = scr("QA", [NH, 2, 64, S], BF16)
    KA = scr("KA", [NH, 2, 64, S], BF16)
    VA = scr("VA", [S, 512], BF16)
    GA = scr("GA", [D, S], BF16)
    GB = scr("GB", [D, S], BF16)
    OHG = scr("OHG", [NH, 128, S], BF16)
    ODA = scr("ODA", [NH, 128, S], BF16)
    H2 = scr("H2", [D, S], BF16)
    TMAX = -(-(2 * S + NE * (TS - 1)) // TS)
    NSLOT = TMAX * TS
    H2TOK = scr("H2TOK", [S, D], BF16)
    HSLOT = scr("HSLOT", [NSLOT, D], BF16)
    YSLOT = scr("YSLOT", [NSLOT, D], F32)
    MFG = 4
    MNFG = DFE // (MFG * 128)
    W1C = scr("W1C", [NE * MNFG * 128, 8 * MFG * 128], BF16)
    W3C = scr("W3C", [NE * MNFG * 128, 8 * MFG * 128], BF16)
    W2C = scr("W2C", [NE * MNFG * 128, MFG * D], BF16)

    es0 = contextlib.ExitStack()
    with es0:
        M = MK(nc, es0)
        ps = [es0.enter_context(nc.psum_tensor("ps%d" % i, [128, 512], F32)) for i in range(8)]
        PK = [("ps", i) for i in range(8)]
        csem = es0.enter_context(nc.semaphore("csem"))
        cconv = [0]

        conv_list = [(e_, fg) for e_ in range(NE) for fg in range(MNFG)] if nlayers > 1 else []
        conv_pos = [0]

        def conv_step(n=1):
            FWc = MFG * 128
            for _ in range(n):
                if conv_pos[0] >= len(conv_list):
                    return
                e_, fg = conv_list[conv_pos[0]]
                conv_pos[0] += 1
                if True:
                    r0 = (e_ * MNFG + fg) * 128
                    for (dst, src) in ((W1C, m_w1), (W3C, m_w3)):
                        nc.gpsimd.dma_start(out=dst[r0:r0 + 128, :].rearrange("p (kc f) -> p kc f", kc=8),
                                            in_=src[0, e_].rearrange("(kc p) f -> p kc f", p=128)[:, :, fg * FWc:(fg + 1) * FWc]).then_inc(csem, 16)
                        cconv[0] += 16
                    nc.gpsimd.dma_start(out=W2C[r0:r0 + 128, :].rearrange("p (fc d) -> p fc d", fc=MFG),
                                        in_=m_w2[0, e_][fg * FWc:(fg + 1) * FWc, :].rearrange("(fc p) d -> p fc d", p=128)).then_inc(csem, 16)
                    cconv[0] += 16

        def sb0(name, shape, dt):
            return es0.enter_context(nc.sbuf_tensor(name, shape, dt))

        cst32 = sb0("cst32", [128, NCST], F32)
        par_sb = sb0("par_sb", [128, NPAR], F32)
        id16 = sb0("id16", [128, 128], BF16)
        blk16 = sb0("blk16", [128, 128], BF16)
        tri16 = sb0("tri16", [128, 128], BF16)
        ones16 = sb0("ones16", [128, 128], BF16)
        qrow16 = sb0("qrow16", [66, NH * 512], BF16)
        dv = sb0("dv", [128, 2, 64], F32)
        ada_sb = sb0("ada_sb", [128, 2, 48], F32)
        cs32 = sb0("cs32", [128, 8], F32)
        us16 = sb0("us16", [128, 128], BF16)
        rank_all = sb0("rank_all", [128, NT, NE], F32)
        sel_all = sb0("sel_all", [128, NT, NE], F32)
        m1_all = sb0("m1_all", [128, NT, NE], F32)
        comb_all = sb0("comb_all", [128, NT, NE], F32)
        rbase = sb0("rbase", [128, NE], F32)
        M.dma('sp', cst32[:], cst[:, :], writes=["cst"])
        M.dma('sp', par_sb[:], par[:, :], writes=["par"])
        M.dma('pool', id16[:], cst[:, C_ID:C_ID + 128], writes=["id16"])
        M.dma('pool', blk16[:], cst[:, C_BLK:C_BLK + 128], writes=["blk16"])
        M.dma('pool', tri16[:], cst[:, C_TRI:C_TRI + 128], writes=["tri16"])
        M.dma('pool', us16[:], cst[:, C_US:C_US + 128], writes=["us16"])
        M.dma('pool', qrow16[64:66, :], qrows[:, :], writes=["qrow16"])
        M.op('dve', lambda e: e.memset(ones16[:], 1.0), [], ["ones16"])
        id32 = cst32[:, C_ID:C_ID + 128]
        m32 = cst32[0:32, C_M32:C_M32 + 128]
        rmask = cst32[:, C_RM:C_RM + 512]
        kbias = cst32[:, C_KB:C_KB + 128]
        M.op('act', lambda e: e.activation(out=cs32[:], in_=par_sb[:, 0:8], func=AF.Silu), ["par"], ["cs32"])

        def pcol(l, off, n=1):
            b = 8 + l * PL + off
            return par_sb[:, b:b + n]

        DV_A1, DV_A2, DV_LB, DV_OML, DV_QSC, DV_GSUB, DV_NLAM, DV_T = 0, 8, 16, 20, 24, 25, 26, 27

        def ada_group(l, g, awr, bank):
            awv = ada_w[l].rearrange("(kc p) f -> p kc f", p=128)
            aw, awk = awr.get()
            M.dma('pool', aw[:], awv[:, :, g * 768:(g + 1) * 768], writes=[awk])
            for jj in range(6):
                j = g * 6 + jj
                for kc in range(8):
                    M.op('pe', lambda e, aw=aw, jj=jj, kc=kc, j=j: e.matmul(
                        ps[bank][:, j:j + 1], aw[:, kc, jj * 128:(jj + 1) * 128], cs32[:, kc:kc + 1],
                        start=(kc == 0), stop=(kc == 7), skip_group_check=True),
                        [awk, "cs32"], [PK[bank]], signal=(kc == 7))

        def ada_finish(l, lt, bank):
            M.op('dve', lambda e, l=l: e.tensor_tensor(out=ada_sb[:, l, :], in0=ps[bank][:, 0:48], in1=pcol(l, 0, 48), op=ALU.add),
                 [PK[bank], "par"], [("ada", l)])
            M.op('dve', lambda e, l=l: e.scalar_tensor_tensor(out=dv[:, l, DV_A1:DV_A1 + 8], in0=ada_sb[:, l, 8:16], scalar=1.0,
                                                              in1=pcol(l, 48, 8), op0=ALU.add, op1=ALU.mult), [("ada", l), "par"], [("dv", l)])
            M.op('dve', lambda e, l=l: e.scalar_tensor_tensor(out=dv[:, l, DV_A2:DV_A2 + 8], in0=ada_sb[:, l, 32:40], scalar=1.0,
                                                              in1=pcol(l, 56, 8), op0=ALU.add, op1=ALU.mult), [("ada", l), "par"], [("dv", l)])
            if l == 0:
                M.op('dve', lambda e, l=l: e.memset(dv[:, l, DV_LB:DV_LB + 4], 0.0), [], [("dv", l)])
                M.op('dve', lambda e, l=l: e.memset(dv[:, l, DV_OML:DV_OML + 4], 1.0), [], [("dv", l)])
            else:
                M.op('dve', lambda e, l=l: e.tensor_tensor(out=lt[:, 0:4], in0=pcol(l, 68, 4), in1=pcol(l, 64, 4), op=ALU.subtract), ["par"], ["lt"])
                M.op('act', lambda e, l=l: e.activation(out=dv[:, l, DV_LB:DV_LB + 4], in_=lt[:, 0:4], func=AF.Sigmoid), ["lt"], [("dv", l)])
                M.op('dve', lambda e, l=l: e.tensor_scalar(out=dv[:, l, DV_OML:DV_OML + 4], in0=dv[:, l, DV_LB:DV_LB + 4], scalar1=-1.0, scalar2=1.0,
                                                           op0=ALU.mult, op1=ALU.add), [("dv", l)], [("dv", l)])
            lam_init = 0.8 - 0.6 * math.exp(-0.3 * l)
            M.op('dve', lambda e, l=l: e.tensor_scalar(out=dv[:, l, DV_QSC:DV_QSC + 1], in0=pcol(l, 73), scalar1=0.125, scalar2=None, op0=ALU.mult), ["par"], [("dv", l)])
            M.op('dve', lambda e, l=l, li=lam_init: e.tensor_scalar(out=dv[:, l, DV_GSUB:DV_GSUB + 1], in0=pcol(l, 75), scalar1=1.0 - li, scalar2=None, op0=ALU.mult), ["par"], [("dv", l)])
            M.op('dve', lambda e, l=l: e.tensor_tensor(out=lt[:, 0:64], in0=pcol(l, 76, 64), in1=pcol(l, 140, 64), op=ALU.mult), ["par"], ["lt"])
            M.op('dve', lambda e, l=l: e.reduce_sum(out=dv[:, l, DV_T:DV_T + 1], in_=lt[:, 0:64], axis=AX.X), ["lt"], [("dv", l)])
            M.op('dve', lambda e, l=l: e.tensor_tensor(out=lt[:, 0:64], in0=pcol(l, 204, 64), in1=pcol(l, 268, 64), op=ALU.mult), ["par", ("dv", l)], ["lt"])
            M.op('dve', lambda e, l=l: e.reduce_sum(out=dv[:, l, DV_T + 1:DV_T + 2], in_=lt[:, 0:64], axis=AX.X), ["lt"], [("dv", l)])
            M.op('act', lambda e, l=l: e.activation(out=dv[:, l, DV_T:DV_T + 2], in_=dv[:, l, DV_T:DV_T + 2], func=AF.Exp), [("dv", l)], [("dv", l)])
            M.op('dve', lambda e, l=l: e.tensor_tensor(out=dv[:, l, DV_NLAM:DV_NLAM + 1], in0=dv[:, l, DV_T + 1:DV_T + 2], in1=dv[:, l, DV_T:DV_T + 1], op=ALU.subtract), [("dv", l)], [("dv", l)])
            M.op('dve', lambda e, l=l, li=lam_init: e.tensor_scalar(out=dv[:, l, DV_NLAM:DV_NLAM + 1], in0=dv[:, l, DV_NLAM:DV_NLAM + 1], scalar1=-li, scalar2=None, op0=ALU.add), [("dv", l)], [("dv", l)])

        win_state = {}

        def win_prefetch(l):
            wes = contextlib.ExitStack()
            wt = wes.enter_context(nc.sbuf_tensor(_uname("win"), [128, 8, DIN], BF16))
            wv = w_in[l].rearrange("(kc p) n -> p kc n", p=128)
            for kc in range(8):
                M.dma('pool', wt[:, kc, :], wv[:, kc, :], writes=[("win", kc)])
            win_state[l] = (wes, wt)

        if phases is None or "p1" in phases:
            win_prefetch(0)
        with contextlib.ExitStack() as es:
          if True:
            xin_r = Ring(es, nc, "xin", [128, D], F32, 2)
            xo_r = Ring(es, nc, "xo", [128, 8, 512], F32, 2)
            xTv = xT.rearrange("(kc p) s -> p kc s", p=128)
            for j in range(NB):
                xo, xok = xo_r.get()
                for tt in range(4):
                    t = j * 4 + tt
                    xin, xink = xin_r.get()
                    M.dma('sp', xin[:], x[t * 128:(t + 1) * 128, :], writes=[xink])
                    for half in range(2):
                        b = 4 + 2 * (tt % 2) + half
                        for q in range(4):
                            kc = half * 4 + q
                            M.op('pe', lambda e, b=b, q=q, kc=kc, xin=xin: e.transpose(ps[b][:, q * 128:(q + 1) * 128], xin[:, kc * 128:(kc + 1) * 128], id32),
                                 [xink, "cst"], [PK[b]], signal=(q == 3))
                        eng = 'act' if half == 0 else 'dve'
                        if eng == 'act':
                            M.op('act', lambda e, b=b, half=half, tt=tt, xo=xo: e.copy(
                                out=xo[:, half * 4:half * 4 + 4, tt * 128:(tt + 1) * 128], in_=ps[b][:].rearrange("p (q t) -> p q t", q=4)),
                                [PK[b]], [xok])
                        else:
                            M.op('dve', lambda e, b=b, half=half, tt=tt, xo=xo: e.tensor_copy(
                                out=xo[:, half * 4:half * 4 + 4, tt * 128:(tt + 1) * 128], in_=ps[b][:].rearrange("p (q t) -> p q t", q=4)),
                                [PK[b]], [xok])
                M.dma('sp', xTv[:, :, j * 512:(j + 1) * 512], xo[:], reads=[xok], writes=[("xT", j)])

          if True:
            awr = Ring(es, nc, "aw", [128, 8, 768], F32, 2)
            lt = es.enter_context(nc.sbuf_tensor(_uname("lt"), [128, 64], F32))
            for l in range(1):
                for g in range(8):
                    ada_group(l, g, awr, 0)
                ada_finish(l, lt, 0)
            M.barrier()

        def K8(k):
            return [(k, i) for i in range(8)]

        def norm_block(xs, xsk, A, B, Akey, hT, hTk, sq, sqk, r32, psb, rsr, h32=None):
            for kc in range(8):
                M.op('act', lambda e, kc=kc: e.activation(out=sq[:, kc, :], in_=xs[:, kc, :], func=AF.Square), [(xsk, kc)], [(sqk, kc)])
            for kc in range(8):
                M.op('pe', lambda e, kc=kc: e.matmul(ps[psb][:], ones16[:], sq[:, kc, :], start=(kc == 0), stop=(kc == 7)),
                     [(sqk, kc), "ones16"], [PK[psb]], signal=(kc == 7))
            rs, rsk = rsr.get()
            M.op('act', lambda e: e.activation(out=rs[:], in_=ps[psb][:], func=AF.Ln, bias=EPS, scale=1.0 / D), [PK[psb]], [rsk])
            M.op('act', lambda e: e.activation(out=rs[:], in_=rs[:], func=AF.Exp, scale=-0.5), [rsk], [rsk])
            for kc in range(8):
                t, tk = r32.get()
                M.op('dve', lambda e, kc=kc, t=t: e.scalar_tensor_tensor(out=t[:], in0=xs[:, kc, :], scalar=A[:, kc:kc + 1], in1=rs[:],
                                                                        op0=ALU.mult, op1=ALU.mult), [(xsk, kc), rsk, Akey], [tk])
                if h32 is None:
                    M.op('act', lambda e, kc=kc, t=t: e.activation(out=hT[:, kc, :], in_=t[:], func=AF.Identity, bias=B[:, kc:kc + 1], scale=1.0),
                         [tk, Akey], [(hTk, kc)])
                else:
                    M.op('act', lambda e, kc=kc, t=t: e.activation(out=h32[0][:, kc, :], in_=t[:], func=AF.Identity, bias=B[:, kc:kc + 1], scale=1.0),
                         [tk, Akey], [(h32[1], kc)])
                    M.op('pool', lambda e, kc=kc: e.tensor_copy(out=hT[:, kc, :], in_=h32[0][:, kc, :]), [(h32[1], kc)], [(hTk, kc)])

        QHv = QH.rearrange("h p s -> p h s")
        KDv = KD.rearrange("h p s -> p h s")
        KDECv = KDEC.rearrange("h p s -> p h s")
        VHTv = VHT.rearrange("h p s -> p h s")
        SGv = SG.rearrange("h p s -> p h s")
        EBLv = EBL.rearrange("h p c -> p h c")
        OHGv = OHG.rearrange("h p s -> p h s")
        ODAv = ODA.rearrange("h p s -> p h s")
        xTv = xT.rearrange("(kc p) s -> p kc s", p=128)
        GAv = GA.rearrange("(kc p) s -> p kc s", p=128)
        GBv = GB.rearrange("(kc p) s -> p kc s", p=128)
        H2v = H2.rearrange("(kc p) s -> p kc s", p=128)
        VAv = VA.rearrange("(t p) c -> p t c", p=128)

        def want(ph):
            return phases is None or ph in phases

        for l in range(nlayers):
            A1 = dv[:, l, DV_A1:DV_A1 + 8]
            B1 = ada_sb[:, l, 0:8]
            G1 = ada_sb[:, l, 16:24]
            A2 = dv[:, l, DV_A2:DV_A2 + 8]
            B2 = ada_sb[:, l, 24:32]
            G2 = ada_sb[:, l, 40:48]
            LB = dv[:, l, DV_LB:DV_LB + 4]
            OML = dv[:, l, DV_OML:DV_OML + 4]
            QSC = dv[:, l, DV_QSC:DV_QSC + 1]
            KSC = pcol(l, 74)
            GSUB = dv[:, l, DV_GSUB:DV_GSUB + 1]
            NLAM = dv[:, l, DV_NLAM:DV_NLAM + 1]
            HGN = pcol(l, 72)
            PKEY = [("dv", l), ("ada", l), "par"]

            if want("p1"):
              if l not in win_state:
                  win_prefetch(l)
              wes_, win = win_state.pop(l)
              with contextlib.ExitStack() as es:
                xs_r = Ring(es, nc, "xs", [128, 8, 512], F32, 2)
                sq = es.enter_context(nc.sbuf_tensor(_uname("sq"), [128, 8, 512], BF16))
                hT_r = Ring(es, nc, "hT", [128, 8, 512], BF16, 2)
                r32 = Ring(es, nc, "r32", [128, 512], F32, 8)
                r16 = Ring(es, nc, "r16", [128, 512], BF16, 8)
                rbl = Ring(es, nc, "rbl", [128, 16], F32, 4)
                rsr = Ring(es, nc, "rsr", [128, 512], F32, 2)
                WINK = [("win", kc) for kc in range(8)]
                pring = [2, 3, 4, 5, 6, 7]
                pri = [0]

                def nextbank():
                    b = pring[pri[0] % len(pring)]
                    pri[0] += 1
                    return b

                def p1_load(jn):
                    xs_, xsk_ = xs_r.get()
                    M.dma('sp', xs_[:], xTv[:, :, jn * 512:(jn + 1) * 512], reads=[("xT", jn)], writes=K8(xsk_))
                    return xs_, xsk_

                def p1_norm(ld):
                    hT_, hTk_ = hT_r.get()
                    norm_block(ld[0], ld[1], A1, B1, PKEY[0], hT_, hTk_, sq, "sq", r32, 0, rsr)
                    return hT_, hTk_

                ld_cur = p1_load(0)
                h_cur = p1_norm(ld_cur)
                for j in range(NB):
                    cols = slice(j * 512, (j + 1) * 512)
                    ld_nxt = p1_load(j + 1) if j + 1 < NB else None
                    if l == 1:
                        conv_step(2)
                    hT, hTk = h_cur

                    def proj_fm(oc):
                        b = nextbank()
                        for kc in range(8):
                            M.op('pe', lambda e, kc=kc, b=b, oc=oc: e.matmul(ps[b][:], win[:, kc, oc * 128:(oc + 1) * 128], hT[:, kc, :],
                                                                           start=(kc == 0), stop=(kc == 7)),
                                 [WINK[kc], (hTk, kc)], [PK[b]], signal=(kc == 7))
                        return b

                    for h in range(NH):
                        bz = proj_fm(4 + h)
                        bq = proj_fm(h)
                        sg, sgk = r32.get()
                        sn, snk = r32.get()
                        M.op('act', lambda e, sn=sn, bz=bz: e.activation(out=sn[:], in_=ps[bz][:], func=AF.Exp, scale=-1.0), [PK[bz]], [snk])
                        M.op('dve', lambda e, sg=sg, sn=sn: e.tensor_scalar(out=sg[:], in0=sn[:], scalar1=1.0, scalar2=None, op0=ALU.add), [snk], [sgk])
                        M.op('dve', lambda e, sg=sg: e.reciprocal(out=sg[:], in_=sg[:]), [sgk], [sgk])
                        M.op('dve', lambda e, sg=sg, sn=sn: e.tensor_tensor(out=sn[:], in0=sn[:], in1=sg[:], op=ALU.mult), [snk, sgk], [snk])
                        M.op('dve', lambda e, sg=sg, h=h: e.tensor_scalar(out=sg[:], in0=sg[:], scalar1=OML[:, h:h + 1], scalar2=LB[:, h:h + 1],
                                                                        op0=ALU.mult, op1=ALU.add), [sgk, PKEY[0]], [sgk])
                        M.op('act', lambda e, sg=sg: e.activation(out=sg[:], in_=sg[:], func=AF.Ln), [sgk], [sgk])
                        bT, bTk = r32.get()
                        M.op('dve', lambda e, sg=sg, bT=bT: e.tensor_tensor_scan(out=bT[:], data0=rmask, data1=sg[:], initial=0.0, op0=ALU.mult, op1=ALU.add),
                             [sgk, "cst"], [bTk])
                        M.op('dve', lambda e, sn=sn, h=h: e.tensor_scalar(out=sn[:], in0=sn[:], scalar1=OML[:, h:h + 1], scalar2=None, op0=ALU.mult),
                             [snk, PKEY[0]], [snk])
                        e1, e1k = r32.get()
                        M.op('act', lambda e, e1=e1, bT=bT: e.activation(out=e1[:], in_=bT[:], func=AF.Exp, scale=-1.0), [bTk], [e1k])
                        kd, kdk = r16.get()
                        M.op('dve', lambda e, kd=kd, sn=sn, e1=e1: e.tensor_tensor(out=kd[:], in0=sn[:], in1=e1[:], op=ALU.mult), [snk, e1k], [kdk])
                        M.dma('sp', KDv[:, h, cols], kd[:], reads=[kdk], writes=[("KD", j)])
                        bT3 = bT[:].rearrange("p (c s) -> p c s", s=32)
                        M.op('dve', lambda e, e1=e1, bT3=bT3: e.tensor_tensor(out=e1[:].rearrange("p (c s) -> p c s", s=32),
                                                                               in0=bT3[:, :, 31:32].to_broadcast([128, 16, 32]), in1=bT3, op=ALU.subtract),
                             [bTk], [e1k])
                        M.op('act', lambda e, e1=e1: e.activation(out=e1[:], in_=e1[:], func=AF.Exp), [e1k], [e1k])
                        kdec, kdeck = r16.get()
                        M.op('dve', lambda e, kdec=kdec, sn=sn, e1=e1: e.tensor_tensor(out=kdec[:], in0=sn[:], in1=e1[:], op=ALU.mult), [snk, e1k], [kdeck])
                        M.dma('sp', KDECv[:, h, cols], kdec[:], reads=[kdeck], writes=[("KDEC", j)])
                        ebl, eblk = rbl.get()
                        M.op('act', lambda e, ebl=ebl, bT3=bT3: e.activation(out=ebl[:], in_=bT3[:, :, 31], func=AF.Exp), [bTk], [eblk])
                        M.dma('sp', EBLv[:, h, j * 16:(j + 1) * 16], ebl[:], reads=[eblk], writes=[("EBL", j)])
                        M.op('act', lambda e, bT=bT: e.activation(out=bT[:], in_=bT[:], func=AF.Exp), [bTk], [bTk])
                        qe, qek = r16.get()
                        M.op('dve', lambda e, qe=qe, bq=bq, bT=bT: e.tensor_tensor(out=qe[:], in0=ps[bq][:], in1=bT[:], op=ALU.mult), [PK[bq], bTk], [qek])
                        M.dma('sp', QHv[:, h, cols], qe[:], reads=[qek], writes=[("QH", j)])
                    for h in range(NH):
                        b = proj_fm(8 + h)
                        t, tk = r16.get()
                        M.op('act', lambda e, t=t, b=b: e.copy(out=t[:], in_=ps[b][:]), [PK[b]], [tk])
                        M.dma('sp', VHTv[:, h, cols], t[:], reads=[tk], writes=[("VHT", j)])
                    for h in range(NH):
                        b = proj_fm(12 + h)
                        t, tk = r16.get()
                        M.op('act', lambda e, t=t, b=b: e.activation(out=t[:], in_=ps[b][:], func=AF.Silu), [PK[b]], [tk])
                        M.dma('sp', SGv[:, h, cols], t[:], reads=[tk], writes=[("SG", j)])
                    for (base, scol, dst, dkey) in ((16, QSC, QA, "QA"), (20, KSC, KA, "KA")):
                        for h in range(NH):
                            b = proj_fm(base + h)
                            s2, s2k = r16.get()
                            M.op('act', lambda e, s2=s2, b=b: e.activation(out=s2[:], in_=ps[b][:], func=AF.Square), [PK[b]], [s2k])
                            M.op('pe', lambda e, s2=s2: e.matmul(ps[1][:], blk16[:], s2[:], start=True, stop=True), [s2k, "blk16"], [PK[1]])
                            rr, rrk = r32.get()
                            M.op('act', lambda e, rr=rr: e.activation(out=rr[:], in_=ps[1][:], func=AF.Ln, bias=EPS, scale=1.0 / 64), [PK[1]], [rrk])
                            M.op('act', lambda e, rr=rr: e.activation(out=rr[:], in_=rr[:], func=AF.Exp, scale=-0.5), [rrk], [rrk])
                            qn, qnk = r16.get()
                            M.op('dve', lambda e, qn=qn, b=b, rr=rr, scol=scol: e.scalar_tensor_tensor(out=qn[:], in0=ps[b][:], scalar=scol, in1=rr[:],
                                                                                                      op0=ALU.mult, op1=ALU.mult),
                                 [PK[b], rrk] + PKEY, [qnk])
                            M.dma('sp', dst[h].rearrange("c d s -> (c d) s")[:, cols], qn[:], reads=[qnk], writes=[(dkey, j)])
                    for tt in range(4):
                        b = nextbank()
                        for kc in range(8):
                            M.op('pe', lambda e, kc=kc, b=b, tt=tt: e.matmul(ps[b][:], hT[:, kc, tt * 128:(tt + 1) * 128], win[:, kc, 3072:3584],
                                                                           start=(kc == 0), stop=(kc == 7)),
                                 [WINK[kc], (hTk, kc)], [PK[b]], signal=(kc == 7))
                        t, tk = r16.get()
                        M.op('act', lambda e, t=t, b=b: e.copy(out=t[:], in_=ps[b][:]), [PK[b]], [tk])
                        r0 = j * 512 + tt * 128
                        M.dma('sp', VA[r0:r0 + 128, :], t[:], reads=[tk], writes=[("VA", j)])
                    if ld_nxt is not None:
                        h_cur = p1_norm(ld_nxt)
                    for (base, dstv, dkey) in ((28, GAv, "GA"), (36, GBv, "GB")):
                        for kc2 in range(8):
                            b = proj_fm(base + kc2)
                            t, tk = r16.get()
                            M.op('act', lambda e, t=t, b=b: e.activation(out=t[:], in_=ps[b][:], func=AF.Sigmoid), [PK[b]], [tk])
                            M.dma('sp', dstv[:, kc2, cols], t[:], reads=[tk], writes=[(dkey, j)])
                M.barrier()
              wes_.close()

            if want("p2a"):
              with contextlib.ExitStack() as es:
                qe_r = Ring(es, nc, "hq", [128, NH, 512], BF16, 2)
                kd_r = Ring(es, nc, "hkd", [128, NH, 512], BF16, 2)
                kc_r = Ring(es, nc, "hkc", [128, NH, 512], BF16, 2)
                vt_r = Ring(es, nc, "hvt", [128, NH, 512], BF16, 2)
                sg_r = Ring(es, nc, "hsg", [128, NH, 512], BF16, 2)
                eb_r = Ring(es, nc, "heb", [128, NH, 16], F32, 2)
                sm_r = Ring(es, nc, "hsm", [32, 128], BF16, 4)
                tk_r = Ring(es, nc, "htk", [32, 1024], BF16, 4)
                Sst = es.enter_context(nc.sbuf_tensor(_uname("Sst"), [128, NH, 128], F32))
                Sbf = [Ring(es, nc, "Sbf%d" % h, [128, 128], BF16, 2) for h in range(NH)]
                r32 = Ring(es, nc, "r32", [128, 512], F32, 4)
                r16 = Ring(es, nc, "r16", [128, 512], BF16, 4)
                M.op('dve', lambda e: e.memset(Sst[:], 0.0), [], [("S", h) for h in range(NH)])
                scur = []
                for h in range(NH):
                    s0, s0k = Sbf[h].get()
                    M.op('dve', lambda e, s0=s0: e.memset(s0[:], 0.0), [], [s0k])
                    scur.append((s0, s0k))
                psS, psE = 0, 7
                psTl = [1, 2]
                psUb = [3, 6]
                psOb = [4, 5]
                blk = {}

                def load_block(j):
                    cols = slice(j * 512, (j + 1) * 512)
                    d = {}
                    for nm, ring, src, key in (("qe", qe_r, QHv, "QH"), ("kd", kd_r, KDv, "KD"), ("kc", kc_r, KDECv, "KDEC"),
                                               ("vt", vt_r, VHTv, "VHT"), ("sg", sg_r, SGv, "SG")):
                        t, k = ring.get()
                        M.dma('sp', t[:], src[:, :, cols], reads=[(key, j)], writes=[k])
                        d[nm] = (t, k)
                    t, k = eb_r.get()
                    M.dma('sp', t[:], EBLv[:, :, j * 16:(j + 1) * 16], reads=[("EBL", j)], writes=[k])
                    d["eb"] = (t, k)
                    blk[j] = d

                def stage_a(g):
                    j, c = divmod(g, 16)
                    d = blk[j]
                    (qe, qek), (kd, kdk), (kc_, kck), (vt, vtk) = d["qe"], d["kd"], d["kc"], d["vt"]
                    cc = slice(c * 32, (c + 1) * 32)
                    for h in range(NH):
                        M.op('pe', lambda e, h=h: e.matmul(ps[psS][0:32, h * 32:(h + 1) * 32], kd[:, h, cc], qe[:, h, cc], start=True, stop=True),
                             [kdk, qek], [PK[psS]], signal=(h == NH - 1))
                    sm, smk = sm_r.get()
                    M.op('dve', lambda e: e.tensor_tensor(out=sm[:], in0=ps[psS][0:32, 0:128], in1=m32, op=ALU.mult), [PK[psS], "cst"], [smk])
                    pT = psTl[g % 2]
                    psTb = ps[pT][:].bitcast(BF16)
                    for h in range(NH):
                        M.op('pe', lambda e, h=h: e.transpose(psTb[0:32, h * 128:(h + 1) * 128], kc_[:, h, cc], id16[:]),
                             [kck, "id16"], [PK[pT]], signal=False)
                    for h in range(NH):
                        M.op('pe', lambda e, h=h: e.transpose(psTb[0:32, 512 + h * 128:512 + (h + 1) * 128], vt[:, h, cc], id16[:]),
                             [vtk, "id16"], [PK[pT]], signal=(h == NH - 1))
                    tk, tkk = tk_r.get()
                    M.op('act', lambda e: e.copy(out=tk[:, 0:512], in_=psTb[0:32, 0:512]), [PK[pT]], [(tkk, 0)])
                    M.op('dve', lambda e: e.tensor_copy(out=tk[:, 512:1024], in_=psTb[0:32, 512:1024]), [PK[pT], (tkk, 0)], [(tkk, 1)])
                    return sm, smk, tk, tkk

                def stage_b(g, a_out):
                    j, c = divmod(g, 16)
                    sm, smk, tk, tkk = a_out
                    d = blk[j]
                    (qe, qek), (eb, ebk) = d["qe"], d["eb"]
                    cc = slice(c * 32, (c + 1) * 32)
                    for h in range(NH):
                        s_bf, s_bfk = scur[h]
                        bo = psOb[h // 2]
                        oc = slice((h % 2) * 256 + (c % 8) * 32, (h % 2) * 256 + (c % 8) * 32 + 32)
                        firstw = (c % 8 == 0) and (h % 2 == 0)
                        M.op('pe', lambda e, h=h: e.matmul(ps[bo][:, oc], s_bf[:], qe[:, h, cc], start=firstw, stop=False, skip_group_check=True),
                             [s_bfk, qek], [PK[bo]], signal=False)
                        M.op('pe', lambda e, h=h: e.matmul(ps[bo][:, oc], tk[:, 512 + h * 128:512 + (h + 1) * 128], sm[:, h * 32:(h + 1) * 32],
                                                          start=False, stop=True, skip_group_check=True),
                             [(tkk, 1), smk], [PK[bo]], signal=False)
                        psU = psUb[h % 2]
                        M.op('pe', lambda e, h=h: e.matmul(ps[psU][:, 0:128], tk[:, h * 128:(h + 1) * 128], tk[:, 512 + h * 128:512 + (h + 1) * 128],
                                                          start=True, stop=True), [(tkk, 0), (tkk, 1)], [PK[psU]], signal=True)
                        M.op('dve', lambda e, h=h: e.scalar_tensor_tensor(out=Sst[:, h, :], in0=Sst[:, h, :], scalar=eb[:, h, c:c + 1],
                                                                        in1=ps[psU][:, 0:128], op0=ALU.mult, op1=ALU.add),
                             [("S", h), ebk, PK[psU]], [("S", h)])
                        s_n, s_nk = Sbf[h].get()
                        M.op('act', lambda e, h=h: e.copy(out=s_n[:], in_=Sst[:, h, :]), [("S", h)], [s_nk])
                        scur[h] = (s_n, s_nk)
                    if c % 8 == 7:
                        half = c // 8
                        (sg, sgk) = d["sg"]
                        tcols = slice(j * 512 + half * 256, j * 512 + half * 256 + 256)
                        for hp in range(2):
                            bo = psOb[hp]
                            oq, oqk = r16.get()
                            M.op('act', lambda e: e.activation(out=oq[:], in_=ps[bo][:], func=AF.Square), [PK[bo]], [oqk])
                            M.op('pe', lambda e: e.matmul(ps[psE][:], ones16[:], oq[:], start=True, stop=True), [oqk, "ones16"], [PK[psE]])
                            rr, rrk = r32.get()
                            M.op('act', lambda e: e.activation(out=rr[:], in_=ps[psE][:], func=AF.Ln, bias=EPS, scale=1.0 / 128), [PK[psE]], [rrk])
                            M.op('act', lambda e: e.activation(out=rr[:], in_=rr[:], func=AF.Exp, scale=-0.5), [rrk], [rrk])
                            t, tk2 = r32.get()
                            M.op('dve', lambda e: e.scalar_tensor_tensor(out=t[:], in0=ps[bo][:], scalar=HGN, in1=rr[:], op0=ALU.mult, op1=ALU.mult),
                                 [PK[bo], rrk, "par"], [tk2])
                            o16, o16k = r16.get()
                            M.op('dve', lambda e: e.tensor_tensor(out=o16[:].rearrange("p (h t) -> p h t", h=2), in0=t[:].rearrange("p (h t) -> p h t", h=2),
                                                                  in1=sg[:, 2 * hp:2 * hp + 2, half * 256:(half + 1) * 256], op=ALU.mult), [tk2, sgk], [o16k])
                            M.dma('sp', OHGv[:, 2 * hp:2 * hp + 2, tcols], o16[:].rearrange("p (h t) -> p h t", h=2), reads=[o16k], writes=[("OHG", j, half, hp)])

                NG = NB * 16
                load_block(0)
                a_cur = stage_a(0)
                for g in range(NG):
                    j, c = divmod(g, 16)
                    if c == 0 and j + 1 < NB:
                        load_block(j + 1)
                    a_nxt = stage_a(g + 1) if g + 1 < NG else None
                    stage_b(g, a_cur)
                    if g % 4 == 3:
                        conv_step(1)
                    a_cur = a_nxt
                M.barrier()

            if want("p2b"):
              with contextlib.ExitStack() as es:
                kp_r = Ring(es, nc, "kp", [66, 2, S], BF16, 2)
                qp_r = Ring(es, nc, "qp", [66, 2, 512], BF16, 3)
                vsb = es.enter_context(nc.sbuf_tensor(_uname("vsb"), [128, NT, 512], BF16))
                pT_r = Ring(es, nc, "pT", [128, 512], BF16, 4)
                rr_r = Ring(es, nc, "arr", [128, 512], F32, 3)
                tn_r = Ring(es, nc, "atn", [128, 512], F32, 4)
                o_r = Ring(es, nc, "ao", [128, 512], F32, 2)
                r16 = Ring(es, nc, "r16", [128, 512], BF16, 3)
                M.dma('sp', vsb[:], VAv[:, :, :], reads=[("VA", j) for j in range(NB)], writes=["vsb"])
                for t_, k_ in zip(kp_r.t, kp_r.k):
                    M.op('dve', lambda e, t_=t_: e.memset(t_[64:66, :, :], 1.0), [], [k_])
                scb = [0, 1, 2]
                sci = [0]
                pairs = [(3, 4), (5, 6)]
                defer = [None]
                for h in range(NH):
                    kp, kpk = kp_r.get()
                    M.dma('sp', kp[0:64, :, :], KA[h].rearrange("c d s -> d c s"), reads=[("KA", j) for j in range(NB)], writes=[kpk])
                    for j in range(NB):
                        cols = slice(j * 512, (j + 1) * 512)
                        qp, qpk = qp_r.get()
                        M.dma('sp', qp[0:64, :, :], QA[h].rearrange("c d s -> d c s")[:, :, cols], reads=[("QA", j)], writes=[qpk])
                        for c in range(2):
                            M.op('pool', lambda e, c=c, qp=qp, h=h: e.tensor_copy(out=qp[64:66, c, :], in_=qrow16[64:66, h * 512:(h + 1) * 512]),
                                 ["qrow16"], [qpk])
                        steps = [(c, kt) for c in range(2) for kt in range(4 * j + 4)]
                        tn = [tn_r.get(), tn_r.get()]
                        conv_step(1)

                        def emit_sc(st):
                            c, kt = st
                            m = kt - 4 * j
                            c0 = 128 * m if m > 0 else 0
                            b = scb[sci[0] % 3]
                            sci[0] += 1
                            M.op('pe', lambda e: e.matmul(ps[b][:, c0:512], kp[:, c, kt * 128:(kt + 1) * 128], qp[:, c, c0:512], start=True, stop=True),
                                 [kpk, qpk], [PK[b]])
                            return b, c0, m

                        def emit_rest(st, info):
                            c, kt = st
                            b, c0, m = info
                            bO_, bL_ = pairs[c]
                            pT, pTk = pT_r.get()
                            idx = kt - 4 * j + 28
                            M.op('act', lambda e: e.activation(out=pT[:, c0:512], in_=ps[b][:, c0:512], func=AF.Exp,
                                                               bias=kbias[:, h * 32 + idx:h * 32 + idx + 1], scale=1.0), [PK[b], "cst"], [pTk])
                            if m >= 0:
                                M.op('dve', lambda e: e.tensor_tensor(out=pT[:, c0:c0 + 128], in0=pT[:, c0:c0 + 128], in1=tri16[:], op=ALU.mult),
                                     [pTk, "tri16"], [pTk])
                            first = (kt == 0)
                            last = (kt == 4 * j + 3)
                            M.op('pe', lambda e: e.matmul(ps[bO_][:, c0:512], vsb[:, kt, h * 128:(h + 1) * 128], pT[:, c0:512], start=first, stop=last,
                                                          skip_group_check=True), ["vsb", pTk], [PK[bO_]], signal=False)
                            M.op('pe', lambda e: e.matmul(ps[bL_][:, c0:512], ones16[:], pT[:, c0:512], start=first, stop=last,
                                                          skip_group_check=True), ["ones16", pTk], [PK[bL_]], signal=True)
                            if last:
                                t_, tk_ = tn[c]
                                M.op('dve', lambda e: e.reciprocal(out=t_[:], in_=ps[bL_][:]), [PK[bL_]], [tk_])
                                M.op('dve', lambda e: e.tensor_tensor(out=t_[:], in0=ps[bO_][:], in1=t_[:], op=ALU.mult), [PK[bO_], tk_], [tk_])

                        def mk_epi(h=h, cols=cols, tn=tn, j=j):
                            def f():
                                (r0, r0k), (r1, r1k) = tn
                                o, ok_ = o_r.get()
                                M.op('dve', lambda e: e.scalar_tensor_tensor(out=o[:], in0=r1[:], scalar=NLAM, in1=r0[:], op0=ALU.mult, op1=ALU.add),
                                     [r0k, r1k] + PKEY, [ok_])
                                oq, oqk = r16.get()
                                M.op('act', lambda e: e.activation(out=oq[:], in_=o[:], func=AF.Square), [ok_], [oqk])
                                M.op('pe', lambda e: e.matmul(ps[7][:], ones16[:], oq[:], start=True, stop=True), [oqk, "ones16"], [PK[7]])
                                rr, rrk = rr_r.get()
                                M.op('act', lambda e: e.activation(out=rr[:], in_=ps[7][:], func=AF.Ln, bias=EPS, scale=1.0 / 128), [PK[7]], [rrk])
                                M.op('act', lambda e: e.activation(out=rr[:], in_=rr[:], func=AF.Exp, scale=-0.5), [rrk], [rrk])
                                o16, o16k = r16.get()
                                M.op('dve', lambda e: e.scalar_tensor_tensor(out=o16[:], in0=o[:], scalar=GSUB, in1=rr[:], op0=ALU.mult, op1=ALU.mult),
                                     [ok_, rrk] + PKEY, [o16k])
                                M.dma('sp', ODAv[:, h, cols], o16[:], reads=[o16k], writes=[("ODA", j)])
                            return f

                        LA = 2
                        infos = [emit_sc(steps[i]) for i in range(min(LA, len(steps)))]
                        for i, st in enumerate(steps):
                            if i + LA < len(steps):
                                infos.append(emit_sc(steps[i + LA]))
                            emit_rest(st, infos[i])
                            if i == 3 and defer[0] is not None:
                                defer[0]()
                                defer[0] = None
                        if defer[0] is not None:
                            defer[0]()
                        defer[0] = mk_epi()
                defer[0]()
                M.barrier()

            moe = (l % 2 == 1)
            if want("p3"):
              with contextlib.ExitStack() as es:
                wpa = es.enter_context(nc.sbuf_tensor(_uname("wpa"), [128, 4, D], BF16))
                wpb = es.enter_context(nc.sbuf_tensor(_uname("wpb"), [128, 4, D], BF16))
                wo = es.enter_context(nc.sbuf_tensor(_uname("wo"), [128, 8, D], BF16))
                M.dma('pool', wpa[:], w_pa[l].rearrange("(kc p) n -> p kc n", p=128), writes=["wpa"])
                M.dma('pool', wpb[:], w_pb[l].rearrange("(kc p) n -> p kc n", p=128), writes=["wpb"])
                M.dma('pool', wo[:], w_o[l].rearrange("(kc p) n -> p kc n", p=128), writes=["wo"])
                if moe:
                    rt32 = es.enter_context(nc.sbuf_tensor(_uname("rt32"), [128, 8, NE], F32))
                    M.dma('sp', rt32[:], router[l // 2].rearrange("(kc p) e -> p kc e", p=128), writes=["rt32"])
                    h32_r = Ring(es, nc, "h32", [128, 8, 512], F32, 1)
                    rs8 = Ring(es, nc, "rs8", [128, 8], F32, 8)
                    rs1 = Ring(es, nc, "rs1", [128, 1], F32, 8)
                    rs16 = Ring(es, nc, "rs16", [128, 8], BF16, 4)
                    zt = es.enter_context(nc.sbuf_tensor(_uname("zt"), [128, 2, D], BF16))
                    M.op('dve', lambda e: e.memset(zt[:], 0.0), [], ["zt"])
                    HSz = HSLOT.rearrange("(n p) d -> p n d", p=128)
                    for n0 in range(0, NSLOT // 128, 2):
                        M.dma('sp', HSz[:, n0:n0 + 2, :], zt[:], reads=["zt"], writes=[("HSZ", n0)])
                    htok_r = Ring(es, nc, "htok", [128, D], BF16, 2)
                    M.op('dve', lambda e: e.memset(rbase[:], 0.0), [], ["rbase"])
                fold_ada = (l == 0 and nlayers > 1)
                if fold_ada:
                    awr2 = Ring(es, nc, "aw2", [128, 8, 768], F32, 1)
                    lt2 = es.enter_context(nc.sbuf_tensor(_uname("lt2"), [128, 64], F32))
                oh_r = Ring(es, nc, "oh", [128, 4, 512], BF16, 2)
                od_r = Ring(es, nc, "od", [128, 4, 512], BF16, 2)
                ga_r = Ring(es, nc, "ga", [128, 8, 512], BF16, 2)
                gb_r = Ring(es, nc, "gb", [128, 8, 512], BF16, 2)
                xs_r = Ring(es, nc, "xs", [128, 8, 512], F32, 2)
                yT_r = Ring(es, nc, "yT", [128, 8, 512], BF16, 2)
                sq = es.enter_context(nc.sbuf_tensor(_uname("sq"), [128, 8, 512], BF16))
                hT_r = Ring(es, nc, "hT", [128, 8, 512], BF16, 2)
                r32 = Ring(es, nc, "r32", [128, 512], F32, 4)
                rsr = Ring(es, nc, "rsr", [128, 512], F32, 1)
                pb = [1, 2, 3, 4, 5, 6]
                pbi = [0]

                def nb_():
                    b = pb[pbi[0] % len(pb)]
                    pbi[0] += 1
                    return b

                def p3_load(j):
                    cols = slice(j * 512, (j + 1) * 512)
                    oh, ohk = oh_r.get()
                    od, odk = od_r.get()
                    ga, gak = ga_r.get()
                    gb, gbk = gb_r.get()
                    xs, xsk = xs_r.get()
                    M.dma('sp', oh[:], OHGv[:, :, cols], reads=[("OHG", j, hf_, hp_) for hf_ in range(2) for hp_ in range(2)], writes=[ohk])
                    M.dma('sp', od[:], ODAv[:, :, cols], reads=[("ODA", j)], writes=[odk])
                    M.dma('sp', ga[:], GAv[:, :, cols], reads=[("GA", j)], writes=[gak])
                    M.dma('sp', gb[:], GBv[:, :, cols], reads=[("GB", j)], writes=[gbk])
                    M.dma('sp', xs[:], xTv[:, :, cols], reads=[("xT", j)], writes=K8(xsk))
                    return (j, cols, oh, ohk, od, odk, ga, gak, gb, gbk, xs, xsk)
                def p3_mix(L):
                    j, cols, oh, ohk, od, odk, ga, gak, gb, gbk, xs, xsk = L
                    yT, yTk = yT_r.get()
                    for dc in range(8):
                        ba = nb_()
                        bb = nb_()
                        for kc in range(4):
                            M.op('pe', lambda e, kc=kc, dc=dc, ba=ba: e.matmul(ps[ba][:], wpa[:, kc, dc * 128:(dc + 1) * 128], oh[:, kc, :], start=(kc == 0), stop=(kc == 3)),
                                 ["wpa", ohk], [PK[ba]], signal=(kc == 3))
                        for kc in range(4):
                            M.op('pe', lambda e, kc=kc, dc=dc, bb=bb: e.matmul(ps[bb][:], wpb[:, kc, dc * 128:(dc + 1) * 128], od[:, kc, :], start=(kc == 0), stop=(kc == 3)),
                                 ["wpb", odk], [PK[bb]], signal=(kc == 3))
                        t1, t1k = r32.get()
                        t2, t2k = r32.get()
                        M.op('dve', lambda e, t1=t1, ba=ba, dc=dc: e.tensor_tensor(out=t1[:], in0=ps[ba][:], in1=ga[:, dc, :], op=ALU.mult), [PK[ba], gak], [t1k])
                        M.op('dve', lambda e, t2=t2, bb=bb, dc=dc: e.tensor_tensor(out=t2[:], in0=ps[bb][:], in1=gb[:, dc, :], op=ALU.mult), [PK[bb], gbk], [t2k])
                        M.op('pool', lambda e, t1=t1, t2=t2, dc=dc: e.tensor_tensor(out=yT[:, dc, :], in0=t1[:], in1=t2[:], op=ALU.add), [t1k, t2k], [(yTk, dc)])
                    return (yT, yTk)
                def p3_out(L, Y):
                    j, cols, oh, ohk, od, odk, ga, gak, gb, gbk, xs, xsk = L
                    yT, yTk = Y
                    for dc in range(8):
                        b = nb_()
                        for kc in range(8):
                            M.op('pe', lambda e, kc=kc, dc=dc, b=b: e.matmul(ps[b][:], wo[:, kc, dc * 128:(dc + 1) * 128], yT[:, kc, :], start=(kc == 0), stop=(kc == 7)),
                                 ["wo", (yTk, kc)], [PK[b]], signal=(kc == 7))
                        M.op('dve', lambda e, dc=dc, b=b: e.scalar_tensor_tensor(out=xs[:, dc, :], in0=ps[b][:], scalar=G1[:, dc:dc + 1], in1=xs[:, dc, :],
                                                                              op0=ALU.mult, op1=ALU.add), [PK[b], (xsk, dc)] + PKEY, [(xsk, dc)])
                    M.dma('sp', xTv[:, :, cols], xs[:], reads=K8(xsk), writes=[("xT", j)])
                def p3_norm(L):
                    j, cols, oh, ohk, od, odk, ga, gak, gb, gbk, xs, xsk = L
                    hT, hTk = hT_r.get()
                    if moe:
                        h32, h32k = h32_r.get()
                        norm_block(xs, xsk, A2, B2, PKEY[0], hT, hTk, sq, "sq", r32, 0, rsr, h32=(h32, h32k))
                    else:
                        norm_block(xs, xsk, A2, B2, PKEY[0], hT, hTk, sq, "sq", r32, 0, rsr)
                    if not moe:
                        M.dma('sp', H2v[:, :, cols], hT[:], reads=K8(hTk), writes=[("H2", j)])
                    if moe:
                        for tt in range(4):
                            t_ = j * 4 + tt
                            for kc in range(8):
                                M.op('pe', lambda e, kc=kc, tt=tt: e.matmul(ps[7][:, 0:NE], h32[:, kc, tt * 128:(tt + 1) * 128], rt32[:, kc, :],
                                                                          start=(kc == 0), stop=(kc == 7)), [(h32k, kc), "rt32"], [PK[7]], signal=(kc == 7))
                            lg, lgk = rs8.get()
                            M.op('dve', lambda e, lg=lg: e.tensor_copy(out=lg[:], in_=ps[7][:, 0:NE]), [PK[7]], [lgk])
                            m1, m1k = rs1.get()
                            M.op('dve', lambda e, lg=lg, m1=m1: e.reduce_max(out=m1[:], in_=lg[:], axis=AX.X), [lgk], [m1k])
                            RK = ("route", t_)
                            M.op('dve', lambda e, lg=lg, m1=m1: e.tensor_scalar(out=m1_all[:, t_, :], in0=lg[:], scalar1=m1[:, 0:1], scalar2=None, op0=ALU.is_equal), [lgk, m1k], [RK])
                            eq, eqk = rs8.get()
                            M.op('dve', lambda e, lg=lg, eq=eq: e.scalar_tensor_tensor(out=eq[:], in0=m1_all[:, t_, :], scalar=-1e30, in1=lg[:], op0=ALU.mult, op1=ALU.add), [lgk, RK], [eqk])
                            m2, m2k = rs1.get()
                            M.op('dve', lambda e, eq=eq, m2=m2: e.reduce_max(out=m2[:], in_=eq[:], axis=AX.X), [eqk], [m2k])
                            M.op('dve', lambda e, lg=lg, m2=m2: e.tensor_scalar(out=sel_all[:, t_, :], in0=lg[:], scalar1=m2[:, 0:1], scalar2=None, op0=ALU.is_ge), [lgk, m2k], [RK])
                            M.op('dve', lambda e, m1=m1: e.tensor_scalar(out=m1[:], in0=m1[:], scalar1=-1.0, scalar2=None, op0=ALU.mult), [m1k], [m1k])
                            ex, exk = rs8.get()
                            M.op('act', lambda e, lg=lg, m1=m1, ex=ex: e.activation(out=ex[:], in_=lg[:], func=AF.Exp, bias=m1[:, 0:1], scale=1.0), [lgk, m1k], [exk])
                            M.op('dve', lambda e, ex=ex: e.tensor_tensor(out=ex[:], in0=ex[:], in1=sel_all[:, t_, :], op=ALU.mult), [exk, RK], [exk])
                            M.op('dve', lambda e, ex=ex, m2=m2: e.reduce_sum(out=m2[:], in_=ex[:], axis=AX.X), [exk, m2k], [m2k])
                            M.op('dve', lambda e, m2=m2: e.reciprocal(out=m2[:], in_=m2[:]), [m2k], [m2k])
                            M.op('dve', lambda e, ex=ex, m2=m2: e.tensor_scalar(out=comb_all[:, t_, :], in0=ex[:], scalar1=m2[:, 0:1], scalar2=None, op0=ALU.mult), [exk, m2k], [RK])
                            s16, s16k = rs16.get()
                            M.op('dve', lambda e, s16=s16: e.tensor_copy(out=s16[:], in_=sel_all[:, t_, :]), [RK], [s16k])
                            M.op('pe', lambda e, s16=s16: e.matmul(ps[7][:, 16:16 + NE], us16[:], s16[:], start=True, stop=True), [s16k, "us16"], [PK[7]], signal=False)
                            M.op('pe', lambda e, s16=s16: e.matmul(ps[7][:, 32:32 + NE], ones16[:], s16[:], start=True, stop=True), [s16k, "ones16"], [PK[7]])
                            M.op('dve', lambda e: e.tensor_tensor(out=rank_all[:, t_, :], in0=ps[7][:, 16:16 + NE], in1=rbase[:], op=ALU.add), [PK[7], "rbase"], [RK])
                            M.op('dve', lambda e: e.tensor_tensor(out=rbase[:], in0=ps[7][:, 32:32 + NE], in1=rbase[:], op=ALU.add), [PK[7], "rbase"], ["rbase"])
                            ht, htk = htok_r.get()
                            psb16 = ps[7][:].bitcast(BF16)
                            for kc in range(8):
                                M.op('pe', lambda e, kc=kc, tt=tt: e.transpose(psb16[:, kc * 128:(kc + 1) * 128], hT[:, kc, tt * 128:(tt + 1) * 128], id16[:]),
                                     [(hTk, kc), "id16"], [PK[7]], signal=(kc == 7))
                            M.op('act', lambda e, ht=ht: e.copy(out=ht[:], in_=psb16[:, 0:1024]), [PK[7]], [htk])
                            M.dma('sp', H2TOK[t_ * 128:(t_ + 1) * 128, :], ht[:], reads=[htk], writes=[("H2TOK", t_)])
                L_cur = p3_load(0)
                Y_cur = p3_mix(L_cur)
                for j in range(NB):
                    if l == 0:
                        conv_step(2)
                    if fold_ada:
                        gl = ([j] if j < 8 else []) if NB >= 8 else [g_ for g_ in range(8) if g_ % NB == j]
                        for g_ in gl:
                            ada_group(1, g_, awr2, 7)
                        if j == NB - 1:
                            ada_finish(1, lt2, 7)
                    L_nxt = p3_load(j + 1) if j + 1 < NB else None
                    p3_out(L_cur, Y_cur)
                    Y_nxt = p3_mix(L_nxt) if L_nxt is not None else None
                    p3_norm(L_cur)
                    L_cur, Y_cur = L_nxt, Y_nxt
                M.barrier()

            if want("p4") and not moe:
              last = (l == nlayers - 1)
              if not last and want("p1"):
                  win_prefetch(l + 1)
              with contextlib.ExitStack() as es:
                TBF = min(S, 1024)
                NSB = TBF // 512
                if moe:
                    nexp, dff, FG = NE, DFE, 4
                    W1 = lambda e_: m_w1[l // 2, e_]
                    W3 = lambda e_: m_w3[l // 2, e_]
                    W2 = lambda e_: m_w2[l // 2, e_]
                else:
                    nexp, dff, FG = 1, DFF, 2
                    W1 = lambda e_: f_w1[l // 2]
                    W3 = lambda e_: f_w3[l // 2]
                    W2 = lambda e_: f_w2[l // 2]
                NFG = dff // (FG * 128)
                FW = FG * 128
                h2 = es.enter_context(nc.sbuf_tensor(_uname("h2"), [128, 8, TBF], BF16))
                yacc = es.enter_context(nc.sbuf_tensor(_uname("yacc"), [128, 8, TBF], F32))
                w1_r = Ring(es, nc, "w1g", [128, 8, FW], BF16, 2)
                w3_r = Ring(es, nc, "w3g", [128, 8, FW], BF16, 2)
                w2_r = Ring(es, nc, "w2g", [128, FG, D], BF16, 2)
                aT_r = Ring(es, nc, "aT", [128, FG, 512], BF16, 2)
                s_r = Ring(es, nc, "sil", [128, 512], F32, 2)
                if moe:
                    t_r = Ring(es, nc, "tt", [128, 512], F32, 3)
                    cb_r = Ring(es, nc, "cb", [128, TBF], F32, 2)
                if last:
                    xs_r = Ring(es, nc, "xs", [128, 8, 512], F32, 1)
                    ot_r = Ring(es, nc, "ot", [128, D], F32, 2)
                else:
                    xc_r = Ring(es, nc, "xc", [128, 512], F32, 3)
                ub = [0, 1, 2, 3]
                ubi = [0]
                yb = [4, 5, 6]
                ybi = [0]
                for p in range(S // TBF):
                    tc0 = p * TBF
                    M.dma('sp', h2[:], H2v[:, :, tc0:tc0 + TBF], reads=[("H2", jj) for jj in range(NB)], writes=["h2"])
                    yfirst = [True] * (NSB * 8)
                    groups = [(e_, fg) for e_ in range(nexp) for fg in range(NFG)]
                    pend = None
                    cb = cbk = None
                    for gi, (e_, fg) in enumerate(groups):
                        if gi % 2 == 0:
                            conv_step(1)
                        if moe and fg == 0:
                            cb, cbk = cb_r.get()
                            M.dma('sp', cb[:], COMBT[e_:e_ + 1, tc0:tc0 + TBF].partition_broadcast(128), reads=[("COMBT", jj) for jj in range(NB)], writes=[cbk])
                        w1g, w1k = w1_r.get()
                        w3g, w3k = w3_r.get()
                        w2g, w2k = w2_r.get()
                        fcs = slice(fg * FW, (fg + 1) * FW)
                        M.dma('pool', w1g[:], W1(e_).rearrange("(kc p) f -> p kc f", p=128)[:, :, fcs], writes=[w1k])
                        M.dma('pool', w3g[:], W3(e_).rearrange("(kc p) f -> p kc f", p=128)[:, :, fcs], writes=[w3k])
                        M.dma('pool', w2g[:], W2(e_)[fg * FW:(fg + 1) * FW, :].rearrange("(fc p) d -> p fc d", p=128), writes=[w2k])
                        for sbi in range(NSB):
                            sc_ = slice(sbi * 512, (sbi + 1) * 512)
                            aT, aTk = aT_r.get()
                            for fc in range(FG):
                                b1 = ub[ubi[0] % 4]
                                b3 = ub[(ubi[0] + 1) % 4]
                                ubi[0] += 2
                                for kc in range(8):
                                    M.op('pe', lambda e, kc=kc, fc=fc, b1=b1, w1g=w1g: e.matmul(ps[b1][:], w1g[:, kc, fc * 128:(fc + 1) * 128], h2[:, kc, sc_],
                                                                                           start=(kc == 0), stop=(kc == 7)), [w1k, "h2"], [PK[b1]], signal=(kc == 7))
                                for kc in range(8):
                                    M.op('pe', lambda e, kc=kc, fc=fc, b3=b3, w3g=w3g: e.matmul(ps[b3][:], w3g[:, kc, fc * 128:(fc + 1) * 128], h2[:, kc, sc_],
                                                                                           start=(kc == 0), stop=(kc == 7)), [w3k, "h2"], [PK[b3]], signal=(kc == 7))
                                s, sk = s_r.get()
                                M.op('act', lambda e, s=s, b1=b1: e.activation(out=s[:], in_=ps[b1][:], func=AF.Silu), [PK[b1]], [sk])
                                if moe:
                                    t, tk = t_r.get()
                                    M.op('dve', lambda e, s=s, b3=b3, t=t: e.tensor_tensor(out=t[:], in0=ps[b3][:], in1=s[:], op=ALU.mult), [PK[b3], sk], [tk])
                                    M.op('pool', lambda e, t=t, fc=fc, aT=aT, cb=cb: e.tensor_tensor(out=aT[:, fc, :], in0=t[:], in1=cb[:, sc_], op=ALU.mult), [tk, cbk], [aTk])
                                else:
                                    M.op('dve', lambda e, s=s, b3=b3, fc=fc, aT=aT: e.tensor_tensor(out=aT[:, fc, :], in0=ps[b3][:], in1=s[:], op=ALU.mult), [PK[b3], sk], [aTk])
                            if pend is not None:
                                pend()

                            def mk_pend(aT=aT, aTk=aTk, w2g=w2g, w2k=w2k, sbi=sbi, sc_=sc_):
                                def f():
                                    for dc in range(8):
                                        b = yb[ybi[0] % 3]
                                        ybi[0] += 1
                                        for fc in range(FG):
                                            M.op('pe', lambda e, fc=fc, dc=dc, b=b: e.matmul(ps[b][:], w2g[:, fc, dc * 128:(dc + 1) * 128], aT[:, fc, :],
                                                                                           start=(fc == 0), stop=(fc == FG - 1)), [w2k, aTk], [PK[b]], signal=(fc == FG - 1))
                                        yk = ("yacc", sbi, dc)
                                        if yfirst[sbi * 8 + dc]:
                                            yfirst[sbi * 8 + dc] = False
                                            M.op('dve', lambda e, dc=dc, b=b: e.tensor_copy(out=yacc[:, dc, sc_], in_=ps[b][:]), [PK[b]], [yk])
                                        else:
                                            M.op('dve', lambda e, dc=dc, b=b: e.tensor_tensor(out=yacc[:, dc, sc_], in0=ps[b][:], in1=yacc[:, dc, sc_], op=ALU.add),
                                                 [PK[b], yk], [yk])
                                return f
                            pend = mk_pend()
                    pend()
                    for sbi in range(NSB):
                        sc_ = slice(sbi * 512, (sbi + 1) * 512)
                        jb = (tc0 // 512) + sbi
                        gcols = slice(tc0 + sbi * 512, tc0 + (sbi + 1) * 512)
                        if not last:
                            for dc in range(8):
                                xc, xck = xc_r.get()
                                M.dma('sp', xc[:], xTv[:, dc, gcols], reads=[("xT", jb)], writes=[xck])
                                M.op('dve', lambda e, dc=dc, xc=xc: e.scalar_tensor_tensor(out=yacc[:, dc, sc_], in0=yacc[:, dc, sc_], scalar=G2[:, dc:dc + 1], in1=xc[:],
                                                                                        op0=ALU.mult, op1=ALU.add), [("yacc", sbi, dc), xck] + PKEY, [("yacc", sbi, dc)])
                            M.dma('sp', xTv[:, :, gcols], yacc[:, :, sc_], reads=[("yacc", sbi, dc) for dc in range(8)], writes=[("xT", jb)])
                            continue
                        xs, xsk = xs_r.get()
                        M.dma('sp', xs[:], xTv[:, :, gcols], reads=[("xT", jb)], writes=K8(xsk))
                        for dc in range(8):
                            M.op('dve', lambda e, dc=dc: e.scalar_tensor_tensor(out=xs[:, dc, :], in0=yacc[:, dc, sc_], scalar=G2[:, dc:dc + 1], in1=xs[:, dc, :],
                                                                             op0=ALU.mult, op1=ALU.add), [("yacc", sbi, dc), (xsk, dc)] + PKEY, [(xsk, dc)])
                        if True:
                            for tt in range(4):
                                ot, otk = ot_r.get()
                                for half in range(2):
                                    b = ub[ubi[0] % 4]
                                    ubi[0] += 1
                                    for q in range(4):
                                        dc = half * 4 + q
                                        M.op('pe', lambda e, b=b, q=q, dc=dc, tt=tt: e.transpose(ps[b][:, q * 128:(q + 1) * 128], xs[:, dc, tt * 128:(tt + 1) * 128], id32),
                                             [(xsk, dc), "cst"], [PK[b]], signal=(q == 3))
                                    if half == 0:
                                        M.op('act', lambda e, b=b, ot=ot: e.copy(out=ot[:, 0:512], in_=ps[b][:]), [PK[b]], [otk])
                                    else:
                                        M.op('dve', lambda e, b=b, ot=ot: e.tensor_copy(out=ot[:, 512:1024], in_=ps[b][:]), [PK[b]], [otk])
                                r0 = tc0 + sbi * 512 + tt * 128
                                M.dma('sp', out[r0:r0 + 128, :], ot[:], reads=[otk], writes=[("out", r0)])
                M.barrier()
            if want("p4") and moe:
              assert l == nlayers - 1
              with contextlib.ExitStack() as es:
                nexp, dff, FG = NE, DFE, 4
                NFG = dff // (FG * 128)
                FW = FG * 128
                NSUB = TS // 128
                ei = l // 2
                conv_step(10 ** 6)
                assert FG == MFG and cconv[0] == NE * MNFG * 3 * 16, cconv[0]
                for en_ in ('pool',):
                    nc.gpsimd.wait_ge(csem, cconv[0])
                sm8 = Ring(es, nc, "sm8", [128, NE], F32, 6)
                big = Ring(es, nc, "big", [128, NT, NE], F32, 3)
                t88 = es.enter_context(nc.sbuf_tensor(_uname("t88"), [128, NE, 8], F32))
                tx = es.enter_context(nc.sbuf_tensor(_uname("tx"), [128, TMAX, NE], F32))
                slot1f = es.enter_context(nc.sbuf_tensor(_uname("slot1f"), [128, NT], F32))
                slot2f = es.enter_context(nc.sbuf_tensor(_uname("slot2f"), [128, NT], F32))
                slot1i = es.enter_context(nc.sbuf_tensor(_uname("slot1i"), [128, NT], mybir.dt.int32))
                slot2i = es.enter_context(nc.sbuf_tensor(_uname("slot2i"), [128, NT], mybir.dt.int32))
                c1 = es.enter_context(nc.sbuf_tensor(_uname("c1"), [128, NT], F32))
                c2 = es.enter_context(nc.sbuf_tensor(_uname("c2"), [128, NT], F32))
                texf = es.enter_context(nc.sbuf_tensor(_uname("texf"), [128, TMAX], F32))
                widf = es.enter_context(nc.sbuf_tensor(_uname("widf"), [128, TMAX, NFG], F32))
                widi = es.enter_context(nc.sbuf_tensor(_uname("widi"), [128, TMAX, NFG], mybir.dt.int32))
                th = cst32[:, C_TH:C_TH + 8]
                tv = cst32[:, C_TV:C_TV + TMAX]
                M.op('dve', lambda e: e.tensor_tensor(out=t88[:], in0=rbase[:].unsqueeze(2).to_broadcast([128, NE, 8]),
                                                      in1=th.unsqueeze(1).to_broadcast([128, NE, 8]), op=ALU.is_gt), ["rbase", "cst"], ["t88"])
                pe_, pek = sm8.get()
                M.op('dve', lambda e: e.tensor_reduce(out=pe_[:], in_=t88[:], axis=AX.X, op=ALU.add), ["t88"], [pek])
                M.op('dve', lambda e: e.tensor_scalar(out=pe_[:], in0=pe_[:], scalar1=float(TS), scalar2=None, op0=ALU.mult), [pek], [pek])
                on8, on8k = sm8.get()
                M.op('dve', lambda e: e.memset(on8[:], 1.0), [], [on8k])
                incl, inclk = sm8.get()
                M.op('dve', lambda e: e.tensor_tensor_scan(out=incl[:], data0=on8[:], data1=pe_[:], initial=0.0, op0=ALU.mult, op1=ALU.add), [on8k, pek], [inclk])
                off, offk = sm8.get()
                M.op('dve', lambda e: e.tensor_tensor(out=off[:], in0=incl[:], in1=pe_[:], op=ALU.subtract), [inclk, pek], [offk])
                RKS = [("route", t_) for t_ in range(NT)]
                slot, slotk = big.get()
                M.op('dve', lambda e: e.tensor_tensor(out=slot[:], in0=rank_all[:], in1=off[:].unsqueeze(1).to_broadcast([128, NT, NE]), op=ALU.add), RKS + [offk], [slotk])
                m2a, m2ak = big.get()
                M.op('dve', lambda e: e.tensor_tensor(out=m2a[:], in0=sel_all[:], in1=m1_all[:], op=ALU.subtract), RKS, [m2ak])
                tmp, tmpk = big.get()
                for (dst, dk, a_, ak, b_, bk) in ((slot1f, "slot1f", slot, slotk, m1_all, None), (slot2f, "slot2f", slot, slotk, m2a, m2ak),
                                                  (c1, "c1", comb_all, None, m1_all, None), (c2, "c2", comb_all, None, m2a, m2ak)):
                    rk = [k for k in (ak, bk) if k is not None]
                    M.op('dve', lambda e, a_=a_, b_=b_: e.tensor_tensor(out=tmp[:], in0=a_[:], in1=b_[:], op=ALU.mult), rk, [tmpk])
                    M.op('dve', lambda e, dst=dst: e.tensor_reduce(out=dst[:], in_=tmp[:], axis=AX.X, op=ALU.add), [tmpk], [dk])
                M.op('dve', lambda e: e.tensor_copy(out=slot1i[:], in_=slot1f[:]), ["slot1f"], ["slot1i"])
                M.op('dve', lambda e: e.tensor_copy(out=slot2i[:], in_=slot2f[:]), ["slot2f"], ["slot2i"])
                M.op('dve', lambda e: e.tensor_tensor(out=tx[:], in0=incl[:].unsqueeze(1).to_broadcast([128, TMAX, NE]),
                                                      in1=tv.unsqueeze(2).to_broadcast([128, TMAX, NE]), op=ALU.is_le), [inclk, "cst"], ["tx"])
                M.op('dve', lambda e: e.tensor_reduce(out=texf[:], in_=tx[:], axis=AX.X, op=ALU.add), ["tx"], ["texf"])
                M.op('dve', lambda e: e.tensor_scalar(out=texf[:], in0=texf[:], scalar1=float(NE - 1), scalar2=None, op0=ALU.min), ["texf"], ["texf"])
                M.op('dve', lambda e: e.tensor_scalar(out=texf[:], in0=texf[:], scalar1=float(NFG * 128), scalar2=cst32[:, C_PI:C_PI + 1], op0=ALU.mult, op1=ALU.add),
                     ["texf", "cst"], ["texf"])
                for fg in range(NFG):
                    M.op('dve', lambda e, fg=fg: e.tensor_scalar(out=widf[:, :, fg], in0=texf[:], scalar1=float(fg * 128), scalar2=None, op0=ALU.add), ["texf"], ["widf"])
                M.op('dve', lambda e: e.tensor_copy(out=widi[:], in_=widf[:]), ["widf"], ["widi"])
                with contextlib.ExitStack() as es2:
                  htk_r = Ring(es2, nc, "htk2", [128, D], BF16, 3)
                  HSZK = []
                  for t_ in range(NT):
                      ht, htk = htk_r.get()
                      M.dma('sp', ht[:], H2TOK[t_ * 128:(t_ + 1) * 128, :], writes=[htk])
                      for (si, sk) in ((slot1i, "slot1i"), (slot2i, "slot2i")):
                          M.dmai(HSLOT[:, :], bass.IndirectOffsetOnAxis(si[:, t_:t_ + 1], 0), ht[:], None, reads=[htk, sk] + HSZK, writes=[("HSLOT", t_, sk)])
                  M.barrier()
                with contextlib.ExitStack() as es2:
                  hs_r = Ring(es2, nc, "hs", [128, NSUB, D], BF16, 2)
                  h2_r = Ring(es2, nc, "h2s", [128, 8, TS], BF16, 2)
                  ya_r = Ring(es2, nc, "yacs", [128, 8, TS], F32, 2)
                  w1_r = Ring(es2, nc, "w1g", [128, 8, FW], BF16, 2)
                  w3_r = Ring(es2, nc, "w3g", [128, 8, FW], BF16, 2)
                  w2_r = Ring(es2, nc, "w2g", [128, FG, D], BF16, 2)
                  aT_r = Ring(es2, nc, "aT", [128, FG, 512], BF16, 2)
                  s_r = Ring(es2, nc, "sil", [128, 512], F32, 3)
                  ys_r = Ring(es2, nc, "ysb", [128, D], F32, 2)
                  ub = [0, 1, 2, 3]
                  ubi = [0]
                  yb = [4, 5, 6]
                  ybi = [0]
                  HSv = HSLOT.rearrange("(i u p) d -> i p u d", p=128, u=NSUB)
                  ps7b = ps[7][:].bitcast(BF16)

                  def load_hs(i):
                      hs, hsk = hs_r.get()
                      M.dma('sp', hs[:], HSv[i], writes=[hsk])
                      return hs, hsk

                  nxt_hs = load_hs(0)
                  pend = None
                  fin = None
                  for i in range(TMAX):
                      hs, hsk = nxt_hs
                      if i + 1 < TMAX:
                          nxt_hs = load_hs(i + 1)
                      h2, h2k = h2_r.get()
                      for u in range(NSUB):
                          for kc in range(8):
                              M.op('pe', lambda e, u=u, kc=kc: e.transpose(ps7b[:, kc * 128:(kc + 1) * 128], hs[:, u, kc * 128:(kc + 1) * 128], id16[:]),
                                   [hsk, "id16"], [PK[7]], signal=(kc == 7))
                          M.op('act', lambda e, u=u: e.copy(out=h2[:, :, u * 128:(u + 1) * 128], in_=ps7b[:, 0:1024].rearrange("p (k t) -> p k t", k=8)),
                               [PK[7]], [(h2k, u)])
                      H2K = [(h2k, u) for u in range(NSUB)]
                      yacc, yak = ya_r.get()
                      yfirst = [True] * 8
                      for fg in range(NFG):
                          w1g, w1k = w1_r.get()
                          w3g, w3k = w3_r.get()
                          w2g, w2k = w2_r.get()
                          fcs = slice(fg * FW, (fg + 1) * FW)
                          ioff = bass.IndirectOffsetOnAxis(widi[:, i, fg:fg + 1], 0)
                          M.dmai(w1g[:].rearrange("p a b -> p (a b)"), None, W1C[:, :], ioff, reads=["widi"], writes=[w1k])
                          M.dmai(w3g[:].rearrange("p a b -> p (a b)"), None, W3C[:, :], ioff, reads=["widi"], writes=[w3k])
                          M.dmai(w2g[:].rearrange("p a b -> p (a b)"), None, W2C[:, :], ioff, reads=["widi"], writes=[w2k])
                          for sbi in range(TS // 512):
                              sc_ = slice(sbi * 512, (sbi + 1) * 512)
                              aT, aTk = aT_r.get()
                              for fc in range(FG):
                                  b1 = ub[ubi[0] % 4]
                                  b3 = ub[(ubi[0] + 1) % 4]
                                  ubi[0] += 2
                                  for kc in range(8):
                                      M.op('pe', lambda e, kc=kc, fc=fc, b1=b1: e.matmul(ps[b1][:], w1g[:, kc, fc * 128:(fc + 1) * 128], h2[:, kc, sc_],
                                                                                       start=(kc == 0), stop=(kc == 7)), [w1k] + H2K, [PK[b1]], signal=(kc == 7))
                                  for kc in range(8):
                                      M.op('pe', lambda e, kc=kc, fc=fc, b3=b3: e.matmul(ps[b3][:], w3g[:, kc, fc * 128:(fc + 1) * 128], h2[:, kc, sc_],
                                                                                       start=(kc == 0), stop=(kc == 7)), [w3k] + H2K, [PK[b3]], signal=(kc == 7))
                                  s_, sk_ = s_r.get()
                                  M.op('act', lambda e, s_=s_, b1=b1: e.activation(out=s_[:], in_=ps[b1][:], func=AF.Silu), [PK[b1]], [sk_])
                                  M.op('dve', lambda e, s_=s_, b3=b3, fc=fc: e.tensor_tensor(out=aT[:, fc, :], in0=ps[b3][:], in1=s_[:], op=ALU.mult), [PK[b3], sk_], [(aTk, fc)])
                              if pend is not None:
                                  pend()
                              if fin is not None:
                                  fin()
                                  fin = None

                              def mk_pend(aT=aT, aTk=aTk, w2g=w2g, w2k=w2k, sc_=sc_, yacc=yacc, yak=yak, yfirst=yfirst):
                                  def f():
                                      for dc in range(8):
                                          b = yb[ybi[0] % 3]
                                          ybi[0] += 1
                                          for fc in range(FG):
                                              M.op('pe', lambda e, fc=fc, dc=dc, b=b: e.matmul(ps[b][:], w2g[:, fc, dc * 128:(dc + 1) * 128], aT[:, fc, :],
                                                                                             start=(fc == 0), stop=(fc == FG - 1)), [w2k, (aTk, fc)], [PK[b]], signal=(fc == FG - 1))
                                          yk = (yak, dc)
                                          if yfirst[dc]:
                                              yfirst[dc] = False
                                              M.op('dve', lambda e, dc=dc, b=b: e.tensor_copy(out=yacc[:, dc, sc_], in_=ps[b][:]), [PK[b]], [yk])
                                          else:
                                              M.op('dve', lambda e, dc=dc, b=b: e.tensor_tensor(out=yacc[:, dc, sc_], in0=ps[b][:], in1=yacc[:, dc, sc_], op=ALU.add),
                                                   [PK[b], yk], [yk])
                                  return f
                              pend = mk_pend()

                      def mk_fin(i=i, yacc=yacc, yak=yak):
                          def f():
                              for dc in range(8):
                                  M.op('act', lambda e, dc=dc: e.activation(out=yacc[:, dc, :], in_=yacc[:, dc, :], func=AF.Copy, scale=G2[:, dc:dc + 1]),
                                       [(yak, dc)] + PKEY, [(yak, dc)])
                              for u in range(NSUB):
                                  ysb, ysk = ys_r.get()
                                  for half in range(2):
                                      for q in range(4):
                                          dc = half * 4 + q
                                          M.op('pe', lambda e, q=q, dc=dc, u=u: e.transpose(ps[7][:, q * 128:(q + 1) * 128], yacc[:, dc, u * 128:(u + 1) * 128], id32),
                                               [(yak, dc), "cst"], [PK[7]], signal=(q == 3))
                                      M.op('act', lambda e, half=half, ysb=ysb: e.copy(out=ysb[:, half * 512:(half + 1) * 512], in_=ps[7][:]), [PK[7]], [ysk])
                                  r0 = i * TS + u * 128
                                  M.dma('sp', YSLOT[r0:r0 + 128, :], ysb[:], reads=[ysk], writes=[("YSLOT", i, u)])
                          return f
                      fin = mk_fin()
                  pend()
                  fin()
                  M.barrier()
                with contextlib.ExitStack() as es2:
                  xs_r = Ring(es2, nc, "xs", [128, 8, 512], F32, 2)
                  y1_r = Ring(es2, nc, "y1", [128, D], F32, 4)
                  y2_r = Ring(es2, nc, "y2", [128, D], F32, 4)
                  ot_r = Ring(es2, nc, "ot", [128, D], F32, 3)
                  for j in range(NB):
                      xs, xsk = xs_r.get()
                      M.dma('sp', xs[:], xTv[:, :, j * 512:(j + 1) * 512], writes=[xsk])
                      for tt in range(4):
                          t_ = j * 4 + tt
                          y1, y1k = y1_r.get()
                          y2, y2k = y2_r.get()
                          M.dmai(y1[:], None, YSLOT[:, :], bass.IndirectOffsetOnAxis(slot1i[:, t_:t_ + 1], 0), reads=["slot1i"], writes=[y1k])
                          M.dmai(y2[:], None, YSLOT[:, :], bass.IndirectOffsetOnAxis(slot2i[:, t_:t_ + 1], 0), reads=["slot2i"], writes=[y2k])
                          ot, otk = ot_r.get()
                          for half in range(2):
                              b = ub[ubi[0] % 4]
                              ubi[0] += 1
                              for q in range(4):
                                  dc = half * 4 + q
                                  M.op('pe', lambda e, b=b, q=q, dc=dc, tt=tt: e.transpose(ps[b][:, q * 128:(q + 1) * 128], xs[:, dc, tt * 128:(tt + 1) * 128], id32),
                                       [xsk, "cst"], [PK[b]], signal=(q == 3))
                              hc = slice(half * 512, (half + 1) * 512)
                              M.op('dve', lambda e, b=b, hc=hc: e.scalar_tensor_tensor(out=ot[:, hc], in0=y1[:, hc], scalar=c1[:, t_:t_ + 1], in1=ps[b][:], op0=ALU.mult, op1=ALU.add),
                                   [y1k, "c1", PK[b]], [(otk, half)])
                              M.op('dve', lambda e, hc=hc: e.scalar_tensor_tensor(out=ot[:, hc], in0=y2[:, hc], scalar=c2[:, t_:t_ + 1], in1=ot[:, hc], op0=ALU.mult, op1=ALU.add),
                                   [y2k, "c2", (otk, half)], [(otk, half)])
                          M.dma('sp', out[t_ * 128:(t_ + 1) * 128, :], ot[:], reads=[(otk, 0), (otk, 1)], writes=[("out", t_)])
                  M.barrier()
        M.finish()
    build.stats = (M.nops, M.nwaits)
    return nc


def make_consts():
    c = np.zeros((128, NCST), np.float32)
    p = np.arange(128)
    c[:, C_ID:C_ID + 128] = np.eye(128, dtype=np.float32)
    c[:, C_BLK:C_BLK + 128] = (p[:, None] // 64 == p[None, :] // 64).astype(np.float32)
    s = np.arange(32)
    m = (s[:, None] <= s[None, :]).astype(np.float32)
    c[0:32, C_M32:C_M32 + 128] = np.tile(m, (1, 4))
    c[:, C_TRI:C_TRI + 128] = (p[None, :] >= p[:, None]).astype(np.float32)
    rm = np.ones(512, np.float32)
    rm[0::32] = 0.0
    c[:, C_RM:C_RM + 512] = rm[None, :]
    for h in range(NH):
        for idx in range(32):
            c[:, C_KB + h * 32 + idx] = SLOPES[h] * (p + 128.0 * (idx - 28))
    c[:, C_US:C_US + 128] = (p[:, None] < p[None, :]).astype(np.float32)
    c[:, C_TH:C_TH + 8] = (np.arange(8) * TS).astype(np.float32)[None, :]
    c[:, C_TV:C_TV + 64] = (np.arange(64) * TS).astype(np.float32)[None, :]
    c[:, C_PI] = p.astype(np.float32)
    qi = np.arange(512)
    lo = (qi % 256).astype(np.float32)
    hi = (qi - qi % 256).astype(np.float32)
    qr = np.zeros((2, NH * 512), np.float32)
    for h in range(NH):
        qr[0, h * 512:(h + 1) * 512] = -SLOPES[h] * lo
        qr[1, h * 512:(h + 1) * 512] = -SLOPES[h] * hi
    return c, qr


def make_par(b, c, ada_b, norm_mix_g, norm_ffn_g, lb_logits, hgrn_norm_g, qn_g, kn_g, lam, subln_g):
    par = np.zeros((128, NPAR), np.float32)
    par[:, 0:8] = c[b].reshape(8, 128).T
    for l in range(2):
        o = 8 + l * PL
        par[:, o:o + 48] = ada_b[l].reshape(48, 128).T
        par[:, o + 48:o + 56] = norm_mix_g[l].reshape(8, 128).T
        par[:, o + 56:o + 64] = norm_ffn_g[l].reshape(8, 128).T
        par[:, o + 64:o + 68] = lb_logits[0].reshape(4, 128).T
        par[:, o + 68:o + 72] = lb_logits[1].reshape(4, 128).T
        par[:, o + 72] = hgrn_norm_g[l]
        par[:, o + 73] = np.tile(qn_g[l], 2)
        par[:, o + 74] = np.tile(kn_g[l], 2)
        par[:, o + 75] = subln_g[l]
        par[:, o + 76:o + 332] = lam[l].reshape(1, 256)
    return par


_NC_CACHE = {}


def kernel(x, c, ada_w, ada_b, norm_mix_g, norm_ffn_g, w_in, hgrn_lb_logits, hgrn_norm_g,
           da_qnorm_g, da_knorm_g, da_lambda, da_subln_g, w_branch_a, w_branch_b, w_out,
           ffn_w1, ffn_w3, ffn_w2, moe_router, moe_w1, moe_w3, moe_w2):
    f = lambda a: np.ascontiguousarray(np.asarray(a, dtype=np.float32))
    x = f(x)
    B, S, _ = x.shape
    cst, qr = make_consts()
    if S not in _NC_CACHE:
        _NC_CACHE[S] = build(S)
    nc = _NC_CACHE[S]
    shared = dict(cst=cst, qrows=qr, ada_w=f(ada_w), w_in=f(w_in), w_pa=f(w_branch_a), w_pb=f(w_branch_b), w_o=f(w_out),
                  ffn_w1=f(ffn_w1), ffn_w3=f(ffn_w3), ffn_w2=f(ffn_w2), router=f(moe_router),
                  moe_w1=f(moe_w1), moe_w3=f(moe_w3), moe_w2=f(moe_w2))
    args = [f(a) for a in (c, ada_b, norm_mix_g, norm_ffn_g, hgrn_lb_logits, hgrn_norm_g, da_qnorm_g, da_knorm_g, da_lambda, da_subln_g)]
    in_maps = []
    for b in range(B):
        m = dict(shared)
        m["x"] = x[b]
        m["par"] = make_par(b, *args)
        in_maps.append(m)
    res = run_bass_kernel_spmd(nc, in_maps, core_ids=list(range(B)))
    return np.stack([np.asarray(r["out"], dtype=np.float32) for r in res.results], axis=0)
```

```python
import math
import contextlib
import numpy as np
import concourse.bass as bass
import concourse.mybir as mybir
from concourse.bass_utils import run_bass_kernel_spmd

F32 = mybir.dt.float32
BF16 = mybir.dt.bfloat16
AF = mybir.ActivationFunctionType
ALU = mybir.AluOpType
AX = mybir.AxisListType

D = 1024
DIN = 5632
NH = 4
DFF = 2816
DFE = 3584
NE = 8
EPS = 1e-6
PL = 332
NPAR = 8 + 2 * PL
C_ID, C_BLK, C_M32, C_TRI, C_RM, C_KB = 0, 128, 256, 384, 512, 1024
C_US, C_TH, C_TV, C_PI = 1152, 1280, 1288, 1352
NCST = 1353
TS = 512
SLOPES = [2.0 ** (-8.0 * (i + 1) / NH) for i in range(NH)]


class MK:
    def __init__(self, nc, es, nds=48):
        self.nc = nc
        self.eng = dict(pe=nc.tensor, act=nc.scalar, dve=nc.vector, pool=nc.gpsimd, sp=nc.sync)
        self.esem = {k: es.enter_context(nc.semaphore("s_" + k)) for k in self.eng}
        self.ecnt = {k: 0 for k in self.eng}
        self.nds = nds
        self.dsem = [es.enter_context(nc.semaphore("d%d" % i)) for i in range(nds)]
        self.dcnt = [0] * nds
        self.dnext = {'sp': 0, 'pool': 0, 'act': 0}
        self.dpool = {'sp': list(range(0, nds - 16)), 'pool': list(range(nds - 16, nds)), 'act': []}
        self.seen = {k: {} for k in self.eng}
        self.lastw = {}
        self.readers = {}
        self.nwaits = 0
        self.nops = 0

    def _sem(self, sk):
        return self.esem[sk[1]] if sk[0] == 'e' else self.dsem[sk[1]]

    def _wait(self, en, ev):
        sk, val = ev
        if val <= 0:
            return
        if sk[0] == 'e' and sk[1] == en and en in ('pe', 'sp'):
            return
        if sk[0] == 'e':
            assert val <= self.ecnt[sk[1]], ("forward wait", en, ev, self.ecnt[sk[1]])
        if self.seen[en].get(sk, 0) >= val:
            return
        self.eng[en].wait_ge(self._sem(sk), val)
        self.seen[en][sk] = val
        self.nwaits += 1

    def _deps(self, en, reads, writes):
        for k in reads:
            ev = self.lastw.get(k)
            if ev is not None:
                self._wait(en, ev)
        for k in writes:
            ev = self.lastw.get(k)
            if ev is not None:
                self._wait(en, ev)
            for sk, val in self.readers.get(k, {}).items():
                self._wait(en, (sk, val))

    def _record(self, ev, reads, writes):
        sk, val = ev
        for k in writes:
            self.lastw[k] = ev
            self.readers[k] = {}
        for k in reads:
            d = self.readers.setdefault(k, {})
            if d.get(sk, 0) < val:
                d[sk] = val

    def op(self, en, fn, reads=(), writes=(), signal=True):
        self._deps(en, reads, writes)
        ins = fn(self.eng[en])
        self.nops += 1
        if signal:
            self.ecnt[en] += 1
            ins.then_inc(self.esem[en], 1)
            ev = (('e', en), self.ecnt[en])
        else:
            ev = (('e', en), self.ecnt[en] + 1)
        self._record(ev, reads, writes)
        return ins

    def dma(self, q, out, in_, reads=(), writes=(), **kw):
        pl = self.dpool[q]
        i = pl[self.dnext[q] % len(pl)]
        self.dnext[q] += 1
        self._wait(q, (('d', i), self.dcnt[i]))
        self._deps(q, reads, writes)
        ins = self.eng[q].dma_start(out=out, in_=in_, **kw)
        ins.then_inc(self.dsem[i], 16)
        self.dcnt[i] += 16
        ev = (('d', i), self.dcnt[i])
        self._record(ev, reads, writes)
        self.nops += 1
        return ins

    def dmai(self, out, out_off, in_, in_off, reads=(), writes=()):
        q = 'pool'
        pl = self.dpool[q]
        i = pl[self.dnext[q] % len(pl)]
        self.dnext[q] += 1
        self._wait(q, (('d', i), self.dcnt[i]))
        self._deps(q, reads, writes)
        ins = self.eng[q].indirect_dma_start(out, out_off, in_, in_off)
        ins.then_inc(self.dsem[i], 16)
        self.dcnt[i] += 16
        ev = (('d', i), self.dcnt[i])
        self._record(ev, reads, writes)
        self.nops += 1
        return ins

    def note_read(self, en, reads):
        self._deps(en, reads, ())

    def barrier(self):
        for en in self.eng:
            for fn in self.eng:
                if fn != en:
                    self._wait(en, (('e', fn), self.ecnt[fn]))
            for i in range(self.nds):
                self._wait(en, (('d', i), self.dcnt[i]))
        self.lastw = {}
        self.readers = {}

    def finish(self):
        for i in range(self.nds):
            self._wait('sp', (('d', i), self.dcnt[i]))
        for fn in self.eng:
            if fn != 'sp':
                self._wait('sp', (('e', fn), self.ecnt[fn]))


_UID = [0]


def _uname(name):
    _UID[0] += 1
    return "%s_u%d" % (name, _UID[0])


class Ring:
    def __init__(self, es, nc, name, shape, dt, n):
        self.t = [es.enter_context(nc.sbuf_tensor(_uname("%s%d" % (name, i)), shape, dt)) for i in range(n)]
        self.k = [(name, i) for i in range(n)]
        self.i = 0

    def get(self):
        i = self.i
        self.i = (i + 1) % len(self.t)
        return self.t[i], self.k[i]


def build(S=4096, dbg=(), nlayers=2, phases=None):
    NB = S // 512
    NT = S // 128
    NCH = S // 32
    nc = bass.Bass("TRN2", target_bir_lowering=False)

    def din(name, shape):
        return nc.dram_tensor(name, shape, F32, kind="ExternalInput").ap()

    x = din("x", [S, D])
    par = din("par", [128, NPAR])
    cst = din("cst", [128, NCST])
    qrows = din("qrows", [2, NH * 512])
    ada_w = din("ada_w", [2, D, 6 * D])
    w_in = din("w_in", [2, D, DIN])
    w_pa = din("w_pa", [2, 512, D])
    w_pb = din("w_pb", [2, 512, D])
    w_o = din("w_o", [2, D, D])
    f_w1 = din("ffn_w1", [1, D, DFF])
    f_w3 = din("ffn_w3", [1, D, DFF])
    f_w2 = din("ffn_w2", [1, DFF, D])
    router = din("router", [1, D, NE])
    m_w1 = din("moe_w1", [1, NE, D, DFE])
    m_w3 = din("moe_w3", [1, NE, D, DFE])
    m_w2 = din("moe_w2", [1, NE, DFE, D])
    out = nc.dram_tensor("out", [S, D], F32, kind="ExternalOutput").ap()

    def scr(name, shape, dt):
        kind = "ExternalOutput" if name in dbg else "Internal"
        return nc.dram_tensor(name, shape, dt, kind=kind).ap()

    xT = scr("xT", [D, S], F32)
    QH = scr("QH", [NH, 128, S], BF16)
    KD = scr("KD", [NH, 128, S], BF16)
    KDEC = scr("KDEC", [NH, 128, S], BF16)
    VHT = scr("VHT", [NH, 128, S], BF16)
    SG = scr("SG", [NH, 128, S], BF16)
    EBL = scr("EBL", [NH, 128, NCH], F32)
    QA = scr("QA", [NH, 2, 64, S], BF16)
    KA = scr("KA", [NH, 2, 64, S], BF16)
    VA = scr("VA", [S, 512], BF16)
    GA = scr("GA", [D, S], BF16)
    GB = scr("GB", [D, S], BF16)
    OHG = scr("OHG", [NH, 128, S], BF16)
    ODA = scr("ODA", [NH, 128, S], BF16)
    H2 = scr("H2", [D, S], BF16)
    TMAX = -(-(2 * S + NE * (TS - 1)) // TS)
    NSLOT = TMAX * TS
    H2TOK = scr("H2TOK", [S, D], BF16)
    HSLOT = scr("HSLOT", [NSLOT, D], BF16)
    YSLOT = scr("YSLOT", [NSLOT, D], F32)
    MFG = 4
    MNFG = DFE // (MFG * 128)
    W1C = scr("W1C", [NE * MNFG * 128, 8 * MFG * 128], BF16)
    W3C = scr("W3C", [NE * MNFG * 128, 8 * MFG * 128], BF16)
    W2C = scr("W2C", [NE * MNFG * 128, MFG * D], BF16)

    es0 = contextlib.ExitStack()
    with es0:
        M = MK(nc, es0)
        ps = [es0.enter_context(nc.psum_tensor("ps%d" % i, [128, 512], F32)) for i in range(8)]
        PK = [("ps", i) for i in range(8)]
        csem = es0.enter_context(nc.semaphore("csem"))
        cconv = [0]

        conv_list = [(e_, fg) for e_ in range(NE) for fg in range(MNFG)] if nlayers > 1 else []
        conv_pos = [0]

        def conv_step(n=1):
            FWc = MFG * 128
            for _ in range(n):
                if conv_pos[0] >= len(conv_list):
                    return
                e_, fg = conv_list[conv_pos[0]]
                conv_pos[0] += 1
                if True:
                    r0 = (e_ * MNFG + fg) * 128
                    for (dst, src) in ((W1C, m_w1), (W3C, m_w3)):
                        nc.gpsimd.dma_start(out=dst[r0:r0 + 128, :].rearrange("p (kc f) -> p kc f", kc=8),
                                            in_=src[0, e_].rearrange("(kc p) f -> p kc f", p=128)[:, :, fg * FWc:(fg + 1) * FWc]).then_inc(csem, 16)
                        cconv[0] += 16
                    nc.gpsimd.dma_start(out=W2C[r0:r0 + 128, :].rearrange("p (fc d) -> p fc d", fc=MFG),
                                        in_=m_w2[0, e_][fg * FWc:(fg + 1) * FWc, :].rearrange("(fc p) d -> p fc d", p=128)).then_inc(csem, 16)
                    cconv[0] += 16

        def sb0(name, shape, dt):
            return es0.enter_context(nc.sbuf_tensor(name, shape, dt))

        cst32 = sb0("cst32", [128, NCST], F32)
        par_sb = sb0("par_sb", [128, NPAR], F32)
        id16 = sb0("id16", [128, 128], BF16)
        blk16 = sb0("blk16", [128, 128], BF16)
        tri16 = sb0("tri16", [128, 128], BF16)
        ones16 = sb0("ones16", [128, 128], BF16)
        qrow16 = sb0("qrow16", [66, NH * 512], BF16)
        dv = sb0("dv", [128, 2, 64], F32)
        ada_sb = sb0("ada_sb", [128, 2, 48], F32)
        cs32 = sb0("cs32", [128, 8], F32)
        us16 = sb0("us16", [128, 128], BF16)
        rank_all = sb0("rank_all", [128, NT, NE], F32)
        sel_all = sb0("sel_all", [128, NT, NE], F32)
        m1_all = sb0("m1_all", [128, NT, NE], F32)
        comb_all = sb0("comb_all", [128, NT, NE], F32)
        rbase = sb0("rbase", [128, NE], F32)
        M.dma('sp', cst32[:], cst[:, :], writes=["cst"])
        M.dma('sp', par_sb[:], par[:, :], writes=["par"])
        M.dma('pool', id16[:], cst[:, C_ID:C_ID + 128], writes=["id16"])
        M.dma('pool', blk16[:], cst[:, C_BLK:C_BLK + 128], writes=["blk16"])
        M.dma('pool', tri16[:], cst[:, C_TRI:C_TRI + 128], writes=["tri16"])
        M.dma('pool', us16[:], cst[:, C_US:C_US + 128], writes=["us16"])
        M.dma('pool', qrow16[64:66, :], qrows[:, :], writes=["qrow16"])
        M.op('dve', lambda e: e.memset(ones16[:], 1.0), [], ["ones16"])
        id32 = cst32[:, C_ID:C_ID + 128]
        m32 = cst32[0:32, C_M32:C_M32 + 128]
        rmask = cst32[:, C_RM:C_RM + 512]
        kbias = cst32[:, C_KB:C_KB + 128]
        M.op('act', lambda e: e.activation(out=cs32[:], in_=par_sb[:, 0:8], func=AF.Silu), ["par"], ["cs32"])

        def pcol(l, off, n=1):
            b = 8 + l * PL + off
            return par_sb[:, b:b + n]

        DV_A1, DV_A2, DV_LB, DV_OML, DV_QSC, DV_GSUB, DV_NLAM, DV_T = 0, 8, 16, 20, 24, 25, 26, 27

        def ada_group(l, g, awr, bank):
            awv = ada_w[l].rearrange("(kc p) f -> p kc f", p=128)
            aw, awk = awr.get()
            M.dma('pool', aw[:], awv[:, :, g * 768:(g + 1) * 768], writes=[awk])
            for jj in range(6):
                j = g * 6 + jj
                for kc in range(8):
                    M.op('pe', lambda e, aw=aw, jj=jj, kc=kc, j=j: e.matmul(
                        ps[bank][:, j:j + 1], aw[:, kc, jj * 128:(jj + 1) * 128], cs32[:, kc:kc + 1],
                        start=(kc == 0), stop=(kc == 7), skip_group_check=True),
                        [awk, "cs32"], [PK[bank]], signal=(kc == 7))

        def ada_finish(l, lt, bank):
            M.op('dve', lambda e, l=l: e.tensor_tensor(out=ada_sb[:, l, :], in0=ps[bank][:, 0:48], in1=pcol(l, 0, 48), op=ALU.add),
                 [PK[bank], "par"], [("ada", l)])
            M.op('dve', lambda e, l=l: e.scalar_tensor_tensor(out=dv[:, l, DV_A1:DV_A1 + 8], in0=ada_sb[:, l, 8:16], scalar=1.0,
                                                              in1=pcol(l, 48, 8), op0=ALU.add, op1=ALU.mult), [("ada", l), "par"], [("dv", l)])
            M.op('dve', lambda e, l=l: e.scalar_tensor_tensor(out=dv[:, l, DV_A2:DV_A2 + 8], in0=ada_sb[:, l, 32:40], scalar=1.0,
                                                              in1=pcol(l, 56, 8), op0=ALU.add, op1=ALU.mult), [("ada", l), "par"], [("dv", l)])
            if l == 0:
                M.op('dve', lambda e, l=l: e.memset(dv[:, l, DV_LB:DV_LB + 4], 0.0), [], [("dv", l)])
                M.op('dve', lambda e, l=l: e.memset(dv[:, l, DV_OML:DV_OML + 4], 1.0), [], [("dv", l)])
            else:
                M.op('dve', lambda e, l=l: e.tensor_tensor(out=lt[:, 0:4], in0=pcol(l, 68, 4), in1=pcol(l, 64, 4), op=ALU.subtract), ["par"], ["lt"])
                M.op('act', lambda e, l=l: e.activation(out=dv[:, l, DV_LB:DV_LB + 4], in_=lt[:, 0:4], func=AF.Sigmoid), ["lt"], [("dv", l)])
                M.op('dve', lambda e, l=l: e.tensor_scalar(out=dv[:, l, DV_OML:DV_OML + 4], in0=dv[:, l, DV_LB:DV_LB + 4], scalar1=-1.0, scalar2=1.0,
                                                           op0=ALU.mult, op1=ALU.add), [("dv", l)], [("dv", l)])
            lam_init = 0.8 - 0.6 * math.exp(-0.3 * l)
            M.op('dve', lambda e, l=l: e.tensor_scalar(out=dv[:, l, DV_QSC:DV_QSC + 1], in0=pcol(l, 73), scalar1=0.125, scalar2=None, op0=ALU.mult), ["par"], [("dv", l)])
            M.op('dve', lambda e, l=l, li=lam_init: e.tensor_scalar(out=dv[:, l, DV_GSUB:DV_GSUB + 1], in0=pcol(l, 75), scalar1=1.0 - li, scalar2=None, op0=ALU.mult), ["par"], [("dv", l)])
            M.op('dve', lambda e, l=l: e.tensor_tensor(out=lt[:, 0:64], in0=pcol(l, 76, 64), in1=pcol(l, 140, 64), op=ALU.mult), ["par"], ["lt"])
            M.op('dve', lambda e, l=l: e.reduce_sum(out=dv[:, l, DV_T:DV_T + 1], in_=lt[:, 0:64], axis=AX.X), ["lt"], [("dv", l)])
            M.op('dve', lambda e, l=l: e.tensor_tensor(out=lt[:, 0:64], in0=pcol(l, 204, 64), in1=pcol(l, 268, 64), op=ALU.mult), ["par", ("dv", l)], ["lt"])
            M.op('dve', lambda e, l=l: e.reduce_sum(out=dv[:, l, DV_T + 1:DV_T + 2], in_=lt[:, 0:64], axis=AX.X), ["lt"], [("dv", l)])
            M.op('act', lambda e, l=l: e.activation(out=dv[:, l, DV_T:DV_T + 2], in_=dv[:, l, DV_T:DV_T + 2], func=AF.Exp), [("dv", l)], [("dv", l)])
            M.op('dve', lambda e, l=l: e.tensor_tensor(out=dv[:, l, DV_NLAM:DV_NLAM + 1], in0=dv[:, l, DV_T + 1:DV_T + 2], in1=dv[:, l, DV_T:DV_T + 1], op=ALU.subtract), [("dv", l)], [("dv", l)])
            M.op('dve', lambda e, l=l, li=lam_init: e.tensor_scalar(out=dv[:, l, DV_NLAM:DV_NLAM + 1], in0=dv[:, l, DV_NLAM:DV_NLAM + 1], scalar1=-li, scalar2=None, op0=ALU.add), [("dv", l)], [("dv", l)])

        win_state = {}

        def win_prefetch(l):
            wes = contextlib.ExitStack()
            wt = wes.enter_context(nc.sbuf_tensor(_uname("win"), [128, 8, DIN], BF16))
            wv = w_in[l].rearrange("(kc p) n -> p kc n", p=128)
            for kc in range(8):
                M.dma('pool', wt[:, kc, :], wv[:, kc, :], writes=[("win", kc)])
            win_state[l] = (wes, wt)

        if phases is None or "p1" in phases:
            win_prefetch(0)
        with contextlib.ExitStack() as es:
          if True:
            xin_r = Ring(es, nc, "xin", [128, D], F32, 2)
            xo_r = Ring(es, nc, "xo", [128, 8, 512], F32, 2)
            xTv = xT.rearrange("(kc p) s -> p kc s", p=128)
            for j in range(NB):
                xo, xok = xo_r.get()
                for tt in range(4):
                    t = j * 4 + tt
                    xin, xink = xin_r.get()
                    M.dma('sp', xin[:], x[t * 128:(t + 1) * 128, :], writes=[xink])
                    for half in range(2):
                        b = 4 + 2 * (tt % 2) + half
                        for q in range(4):
                            kc = half * 4 + q
                            M.op('pe', lambda e, b=b, q=q, kc=kc, xin=xin: e.transpose(ps[b][:, q * 128:(q + 1) * 128], xin[:, kc * 128:(kc + 1) * 128], id32),
                                 [xink, "cst"], [PK[b]], signal=(q == 3))
                        eng = 'act' if half == 0 else 'dve'
                        if eng == 'act':
                            M.op('act', lambda e, b=b, half=half, tt=tt, xo=xo: e.copy(
                                out=xo[:, half * 4:half * 4 + 4, tt * 128:(tt + 1) * 128], in_=ps[b][:].rearrange("p (q t) -> p q t", q=4)),
                                [PK[b]], [xok])
                        else:
                            M.op('dve', lambda e, b=b, half=half, tt=tt, xo=xo: e.tensor_copy(
                                out=xo[:, half * 4:half * 4 + 4, tt * 128:(tt + 1) * 128], in_=ps[b][:].rearrange("p (q t) -> p q t", q=4)),
                                [PK[b]], [xok])
                M.dma('sp', xTv[:, :, j * 512:(j + 1) * 512], xo[:], reads=[xok], writes=[("xT", j)])

          if True:
            awr = Ring(es, nc, "aw", [128, 8, 768], F32, 2)
            lt = es.enter_context(nc.sbuf_tensor(_uname("lt"), [128, 64], F32))
            for l in range(1):
                for g in range(8):
                    ada_group(l, g, awr, 0)
                ada_finish(l, lt, 0)
            M.barrier()

        def K8(k):
            return [(k, i) for i in range(8)]

        def norm_block(xs, xsk, A, B, Akey, hT, hTk, sq, sqk, r32, psb, rsr, h32=None):
            for kc in range(8):
                M.op('act', lambda e, kc=kc: e.activation(out=sq[:, kc, :], in_=xs[:, kc, :], func=AF.Square), [(xsk, kc)], [(sqk, kc)])
            for kc in range(8):
                M.op('pe', lambda e, kc=kc: e.matmul(ps[psb][:], ones16[:], sq[:, kc, :], start=(kc == 0), stop=(kc == 7)),
                     [(sqk, kc), "ones16"], [PK[psb]], signal=(kc == 7))
            rs, rsk = rsr.get()
            M.op('act', lambda e: e.activation(out=rs[:], in_=ps[psb][:], func=AF.Ln, bias=EPS, scale=1.0 / D), [PK[psb]], [rsk])
            M.op('act', lambda e: e.activation(out=rs[:], in_=rs[:], func=AF.Exp, scale=-0.5), [rsk], [rsk])
            for kc in range(8):
                t, tk = r32.get()
                M.op('dve', lambda e, kc=kc, t=t: e.scalar_tensor_tensor(out=t[:], in0=xs[:, kc, :], scalar=A[:, kc:kc + 1], in1=rs[:],
                                                                        op0=ALU.mult, op1=ALU.mult), [(xsk, kc), rsk, Akey], [tk])
                if h32 is None:
                    M.op('act', lambda e, kc=kc, t=t: e.activation(out=hT[:, kc, :], in_=t[:], func=AF.Identity, bias=B[:, kc:kc + 1], scale=1.0),
                         [tk, Akey], [(hTk, kc)])
                else:
                    M.op('act', lambda e, kc=kc, t=t: e.activation(out=h32[0][:, kc, :], in_=t[:], func=AF.Identity, bias=B[:, kc:kc + 1], scale=1.0),
                         [tk, Akey], [(h32[1], kc)])
                    M.op('pool', lambda e, kc=kc: e.tensor_copy(out=hT[:, kc, :], in_=h32[0][:, kc, :]), [(h32[1], kc)], [(hTk, kc)])

        QHv = QH.rearrange("h p s -> p h s")
        KDv = KD.rearrange("h p s -> p h s")
        KDECv = KDEC.rearrange("h p s -> p h s")
        VHTv = VHT.rearrange("h p s -> p h s")
        SGv = SG.rearrange("h p s -> p h s")
        EBLv = EBL.rearrange("h p c -> p h c")
        OHGv = OHG.rearrange("h p s -> p h s")
        ODAv = ODA.rearrange("h p s -> p h s")
        xTv = xT.rearrange("(kc p) s -> p kc s", p=128)
        GAv = GA.rearrange("(kc p) s -> p kc s", p=128)
        GBv = GB.rearrange("(kc p) s -> p kc s", p=128)
        H2v = H2.rearrange("(kc p) s -> p kc s", p=128)
        VAv = VA.rearrange("(t p) c -> p t c", p=128)

        def want(ph):
            return phases is None or ph in phases

        for l in range(nlayers):
            A1 = dv[:, l, DV_A1:DV_A1 + 8]
            B1 = ada_sb[:, l, 0:8]
            G1 = ada_sb[:, l, 16:24]
            A2 = dv[:, l, DV_A2:DV_A2 + 8]
            B2 = ada_sb[:, l, 24:32]
            G2 = ada_sb[:, l, 40:48]
            LB = dv[:, l, DV_LB:DV_LB + 4]
            OML = dv[:, l, DV_OML:DV_OML + 4]
            QSC = dv[:, l, DV_QSC:DV_QSC + 1]
            KSC = pcol(l, 74)
            GSUB = dv[:, l, DV_GSUB:DV_GSUB + 1]
            NLAM = dv[:, l, DV_NLAM:DV_NLAM + 1]
            HGN = pcol(l, 72)
            PKEY = [("dv", l), ("ada", l), "par"]

            if want("p1"):
              if l not in win_state:
                  win_prefetch(l)
              wes_, win = win_state.pop(l)
              with contextlib.ExitStack() as es:
                xs_r = Ring(es, nc, "xs", [128, 8, 512], F32, 2)
                sq = es.enter_context(nc.sbuf_tensor(_uname("sq"), [128, 8, 512], BF16))
                hT_r = Ring(es, nc, "hT", [128, 8, 512], BF16, 2)
                r32 = Ring(es, nc, "r32", [128, 512], F32, 8)
                r16 = Ring(es, nc, "r16", [128, 512], BF16, 8)
                rbl = Ring(es, nc, "rbl", [128, 16], F32, 4)
                rsr = Ring(es, nc, "rsr", [128, 512], F32, 2)
                WINK = [("win", kc) for kc in range(8)]
                pring = [2, 3, 4, 5, 6, 7]
                pri = [0]

                def nextbank():
                    b = pring[pri[0] % len(pring)]
                    pri[0] += 1
                    return b

                def p1_load(jn):
                    xs_, xsk_ = xs_r.get()
                    M.dma('sp', xs_[:], xTv[:, :, jn * 512:(jn + 1) * 512], reads=[("xT", jn)], writes=K8(xsk_))
                    return xs_, xsk_

                def p1_norm(ld):
                    hT_, hTk_ = hT_r.get()
                    norm_block(ld[0], ld[1], A1, B1, PKEY[0], hT_, hTk_, sq, "sq", r32, 0, rsr)
                    return hT_, hTk_

                ld_cur = p1_load(0)
                h_cur = p1_norm(ld_cur)
                for j in range(NB):
                    cols = slice(j * 512, (j + 1) * 512)
                    ld_nxt = p1_load(j + 1) if j + 1 < NB else None
                    if l == 1:
                        conv_step(2)
                    hT, hTk = h_cur

                    def proj_fm(oc):
                        b = nextbank()
                        for kc in range(8):
                            M.op('pe', lambda e, kc=kc, b=b, oc=oc: e.matmul(ps[b][:], win[:, kc, oc * 128:(oc + 1) * 128], hT[:, kc, :],
                                                                           start=(kc == 0), stop=(kc == 7)),
                                 [WINK[kc], (hTk, kc)], [PK[b]], signal=(kc == 7))
                        return b

                    for h in range(NH):
                        bz = proj_fm(4 + h)
                        bq = proj_fm(h)
                        sg, sgk = r32.get()
                        sn, snk = r32.get()
                        M.op('act', lambda e, sg=sg, bz=bz: e.activation(out=sg[:], in_=ps[bz][:], func=AF.Sigmoid), [PK[bz]], [sgk])
                        M.op('act', lambda e, sn=sn, bz=bz: e.activation(out=sn[:], in_=ps[bz][:], func=AF.Sigmoid, scale=-1.0), [PK[bz]], [snk])
                        M.op('dve', lambda e, sg=sg, h=h: e.tensor_scalar(out=sg[:], in0=sg[:], scalar1=OML[:, h:h + 1], scalar2=LB[:, h:h + 1],
                                                                        op0=ALU.mult, op1=ALU.add), [sgk, PKEY[0]], [sgk])
                        M.op('act', lambda e, sg=sg: e.activation(out=sg[:], in_=sg[:], func=AF.Ln), [sgk], [sgk])
                        bT, bTk = r32.get()
                        M.op('dve', lambda e, sg=sg, bT=bT: e.tensor_tensor_scan(out=bT[:], data0=rmask, data1=sg[:], initial=0.0, op0=ALU.mult, op1=ALU.add),
                             [sgk, "cst"], [bTk])
                        M.op('dve', lambda e, sn=sn, h=h: e.tensor_scalar(out=sn[:], in0=sn[:], scalar1=OML[:, h:h + 1], scalar2=None, op0=ALU.mult),
                             [snk, PKEY[0]], [snk])
                        e1, e1k = r32.get()
                        M.op('act', lambda e, e1=e1, bT=bT: e.activation(out=e1[:], in_=bT[:], func=AF.Exp, scale=-1.0), [bTk], [e1k])
                        kd, kdk = r16.get()
                        M.op('dve', lambda e, kd=kd, sn=sn, e1=e1: e.tensor_tensor(out=kd[:], in0=sn[:], in1=e1[:], op=ALU.mult), [snk, e1k], [kdk])
                        M.dma('sp', KDv[:, h, cols], kd[:], reads=[kdk], writes=[("KD", j)])
                        bT3 = bT[:].rearrange("p (c s) -> p c s", s=32)
                        M.op('dve', lambda e, e1=e1, bT3=bT3: e.tensor_tensor(out=e1[:].rearrange("p (c s) -> p c s", s=32),
                                                                               in0=bT3[:, :, 31:32].to_broadcast([128, 16, 32]), in1=bT3, op=ALU.subtract),
                             [bTk], [e1k])
                        M.op('act', lambda e, e1=e1: e.activation(out=e1[:], in_=e1[:], func=AF.Exp), [e1k], [e1k])
                        kdec, kdeck = r16.get()
                        M.op('dve', lambda e, kdec=kdec, sn=sn, e1=e1: e.tensor_tensor(out=kdec[:], in0=sn[:], in1=e1[:], op=ALU.mult), [snk, e1k], [kdeck])
                        M.dma('sp', KDECv[:, h, cols], kdec[:], reads=[kdeck], writes=[("KDEC", j)])
                        ebl, eblk = rbl.get()
                        M.op('act', lambda e, ebl=ebl, bT3=bT3: e.activation(out=ebl[:], in_=bT3[:, :, 31], func=AF.Exp), [bTk], [eblk])
                        M.dma('sp', EBLv[:, h, j * 16:(j + 1) * 16], ebl[:], reads=[eblk], writes=[("EBL", j)])
                        M.op('act', lambda e, bT=bT: e.activation(out=bT[:], in_=bT[:], func=AF.Exp), [bTk], [bTk])
                        qe, qek = r16.get()
                        M.op('dve', lambda e, qe=qe, bq=bq, bT=bT: e.tensor_tensor(out=qe[:], in0=ps[bq][:], in1=bT[:], op=ALU.mult), [PK[bq], bTk], [qek])
                        M.dma('sp', QHv[:, h, cols], qe[:], reads=[qek], writes=[("QH", j)])
                    for h in range(NH):
                        b = proj_fm(8 + h)
                        t, tk = r16.get()
                        M.op('act', lambda e, t=t, b=b: e.copy(out=t[:], in_=ps[b][:]), [PK[b]], [tk])
                        M.dma('sp', VHTv[:, h, cols], t[:], reads=[tk], writes=[("VHT", j)])
                    for h in range(NH):
                        b = proj_fm(12 + h)
                        t, tk = r16.get()
                        M.op('act', lambda e, t=t, b=b: e.activation(out=t[:], in_=ps[b][:], func=AF.Silu), [PK[b]], [tk])
                        M.dma('sp', SGv[:, h, cols], t[:], reads=[tk], writes=[("SG", j)])
                    for (base, scol, dst, dkey) in ((16, QSC, QA, "QA"), (20, KSC, KA, "KA")):
                        for h in range(NH):
                            b = proj_fm(base + h)
                            s2, s2k = r16.get()
                            M.op('act', lambda e, s2=s2, b=b: e.activation(out=s2[:], in_=ps[b][:], func=AF.Square), [PK[b]], [s2k])
                            M.op('pe', lambda e, s2=s2: e.matmul(ps[1][:], blk16[:], s2[:], start=True, stop=True), [s2k, "blk16"], [PK[1]])
                            rr, rrk = r32.get()
                            M.op('act', lambda e, rr=rr: e.activation(out=rr[:], in_=ps[1][:], func=AF.Ln, bias=EPS, scale=1.0 / 64), [PK[1]], [rrk])
                            M.op('act', lambda e, rr=rr: e.activation(out=rr[:], in_=rr[:], func=AF.Exp, scale=-0.5), [rrk], [rrk])
                            qn, qnk = r16.get()
                            M.op('dve', lambda e, qn=qn, b=b, rr=rr, scol=scol: e.scalar_tensor_tensor(out=qn[:], in0=ps[b][:], scalar=scol, in1=rr[:],
                                                                                                      op0=ALU.mult, op1=ALU.mult),
                                 [PK[b], rrk] + PKEY, [qnk])
                            M.dma('sp', dst[h].rearrange("c d s -> (c d) s")[:, cols], qn[:], reads=[qnk], writes=[(dkey, j)])
                    for tt in range(4):
                        b = nextbank()
                        for kc in range(8):
                            M.op('pe', lambda e, kc=kc, b=b, tt=tt: e.matmul(ps[b][:], hT[:, kc, tt * 128:(tt + 1) * 128], win[:, kc, 3072:3584],
                                                                           start=(kc == 0), stop=(kc == 7)),
                                 [WINK[kc], (hTk, kc)], [PK[b]], signal=(kc == 7))
                        t, tk = r16.get()
                        M.op('act', lambda e, t=t, b=b: e.copy(out=t[:], in_=ps[b][:]), [PK[b]], [tk])
                        r0 = j * 512 + tt * 128
                        M.dma('sp', VA[r0:r0 + 128, :], t[:], reads=[tk], writes=[("VA", j)])
                    if ld_nxt is not None:
                        h_cur = p1_norm(ld_nxt)
                    for (base, dstv, dkey) in ((28, GAv, "GA"), (36, GBv, "GB")):
                        for kc2 in range(8):
                            b = proj_fm(base + kc2)
                            t, tk = r16.get()
                            M.op('act', lambda e, t=t, b=b: e.activation(out=t[:], in_=ps[b][:], func=AF.Sigmoid), [PK[b]], [tk])
                            M.dma('sp', dstv[:, kc2, cols], t[:], reads=[tk], writes=[(dkey, j)])
                M.barrier()
              wes_.close()

            if want("p2a"):
              with contextlib.ExitStack() as es:
                qe_r = Ring(es, nc, "hq", [128, NH, 512], BF16, 2)
                kd_r = Ring(es, nc, "hkd", [128, NH, 512], BF16, 2)
                kc_r = Ring(es, nc, "hkc", [128, NH, 512], BF16, 2)
                vt_r = Ring(es, nc, "hvt", [128, NH, 512], BF16, 2)
                sg_r = Ring(es, nc, "hsg", [128, NH, 512], BF16, 2)
                eb_r = Ring(es, nc, "heb", [128, NH, 16], F32, 2)
                sm_r = Ring(es, nc, "hsm", [32, 128], BF16, 4)
                tk_r = Ring(es, nc, "htk", [32, 1024], BF16, 4)
                Sst = es.enter_context(nc.sbuf_tensor(_uname("Sst"), [128, NH, 128], F32))
                Sbf = [Ring(es, nc, "Sbf%d" % h, [128, 128], BF16, 2) for h in range(NH)]
                r32 = Ring(es, nc, "r32", [128, 512], F32, 4)
                r16 = Ring(es, nc, "r16", [128, 512], BF16, 4)
                M.op('dve', lambda e: e.memset(Sst[:], 0.0), [], [("S", h) for h in range(NH)])
                scur = []
                for h in range(NH):
                    s0, s0k = Sbf[h].get()
                    M.op('dve', lambda e, s0=s0: e.memset(s0[:], 0.0), [], [s0k])
                    scur.append((s0, s0k))
                psS, psE = 0, 7
                psTl = [1, 2]
                psUb = [3, 6]
                psOb = [4, 5]
                blk = {}

                def load_block(j):
                    cols = slice(j * 512, (j + 1) * 512)
                    d = {}
                    for nm, ring, src, key in (("qe", qe_r, QHv, "QH"), ("kd", kd_r, KDv, "KD"), ("kc", kc_r, KDECv, "KDEC"),
                                               ("vt", vt_r, VHTv, "VHT"), ("sg", sg_r, SGv, "SG")):
                        t, k = ring.get()
                        M.dma('sp', t[:], src[:, :, cols], reads=[(key, j)], writes=[k])
                        d[nm] = (t, k)
                    t, k = eb_r.get()
                    M.dma('sp', t[:], EBLv[:, :, j * 16:(j + 1) * 16], reads=[("EBL", j)], writes=[k])
                    d["eb"] = (t, k)
                    blk[j] = d

                def stage_a(g):
                    j, c = divmod(g, 16)
                    d = blk[j]
                    (qe, qek), (kd, kdk), (kc_, kck), (vt, vtk) = d["qe"], d["kd"], d["kc"], d["vt"]
                    cc = slice(c * 32, (c + 1) * 32)
                    for h in range(NH):
                        M.op('pe', lambda e, h=h: e.matmul(ps[psS][0:32, h * 32:(h + 1) * 32], kd[:, h, cc], qe[:, h, cc], start=True, stop=True),
                             [kdk, qek], [PK[psS]], signal=(h == NH - 1))
                    sm, smk = sm_r.get()
                    M.op('dve', lambda e: e.tensor_tensor(out=sm[:], in0=ps[psS][0:32, 0:128], in1=m32, op=ALU.mult), [PK[psS], "cst"], [smk])
                    pT = psTl[g % 2]
                    psTb = ps[pT][:].bitcast(BF16)
                    for h in range(NH):
                        M.op('pe', lambda e, h=h: e.transpose(psTb[0:32, h * 128:(h + 1) * 128], kc_[:, h, cc], id16[:]),
                             [kck, "id16"], [PK[pT]], signal=False)
                    for h in range(NH):
                        M.op('pe', lambda e, h=h: e.transpose(psTb[0:32, 512 + h * 128:512 + (h + 1) * 128], vt[:, h, cc], id16[:]),
                             [vtk, "id16"], [PK[pT]], signal=(h == NH - 1))
                    tk, tkk = tk_r.get()
                    M.op('act', lambda e: e.copy(out=tk[:, 0:512], in_=psTb[0:32, 0:512]), [PK[pT]], [(tkk, 0)])
                    M.op('dve', lambda e: e.tensor_copy(out=tk[:, 512:1024], in_=psTb[0:32, 512:1024]), [PK[pT], (tkk, 0)], [(tkk, 1)])
                    return sm, smk, tk, tkk

                def stage_b(g, a_out):
                    j, c = divmod(g, 16)
                    sm, smk, tk, tkk = a_out
                    d = blk[j]
                    (qe, qek), (eb, ebk) = d["qe"], d["eb"]
                    cc = slice(c * 32, (c + 1) * 32)
                    for h in range(NH):
                        s_bf, s_bfk = scur[h]
                        bo = psOb[h // 2]
                        oc = slice((h % 2) * 256 + (c % 8) * 32, (h % 2) * 256 + (c % 8) * 32 + 32)
                        firstw = (c % 8 == 0) and (h % 2 == 0)
                        M.op('pe', lambda e, h=h: e.matmul(ps[bo][:, oc], s_bf[:], qe[:, h, cc], start=firstw, stop=False, skip_group_check=True),
                             [s_bfk, qek], [PK[bo]], signal=False)
                        M.op('pe', lambda e, h=h: e.matmul(ps[bo][:, oc], tk[:, 512 + h * 128:512 + (h + 1) * 128], sm[:, h * 32:(h + 1) * 32],
                                                          start=False, stop=True, skip_group_check=True),
                             [(tkk, 1), smk], [PK[bo]], signal=False)
                        psU = psUb[h % 2]
                        M.op('pe', lambda e, h=h: e.matmul(ps[psU][:, 0:128], tk[:, h * 128:(h + 1) * 128], tk[:, 512 + h * 128:512 + (h + 1) * 128],
                                                          start=True, stop=True), [(tkk, 0), (tkk, 1)], [PK[psU]], signal=True)
                        M.op('dve', lambda e, h=h: e.scalar_tensor_tensor(out=Sst[:, h, :], in0=Sst[:, h, :], scalar=eb[:, h, c:c + 1],
                                                                        in1=ps[psU][:, 0:128], op0=ALU.mult, op1=ALU.add),
                             [("S", h), ebk, PK[psU]], [("S", h)])
                        s_n, s_nk = Sbf[h].get()
                        M.op('act', lambda e, h=h: e.copy(out=s_n[:], in_=Sst[:, h, :]), [("S", h)], [s_nk])
                        scur[h] = (s_n, s_nk)
                    if c % 8 == 7:
                        half = c // 8
                        (sg, sgk) = d["sg"]
                        tcols = slice(j * 512 + half * 256, j * 512 + half * 256 + 256)
                        for hp in range(2):
                            bo = psOb[hp]
                            oq, oqk = r16.get()
                            M.op('act', lambda e: e.activation(out=oq[:], in_=ps[bo][:], func=AF.Square), [PK[bo]], [oqk])
                            M.op('pe', lambda e: e.matmul(ps[psE][:], ones16[:], oq[:], start=True, stop=True), [oqk, "ones16"], [PK[psE]])
                            rr, rrk = r32.get()
                            M.op('act', lambda e: e.activation(out=rr[:], in_=ps[psE][:], func=AF.Ln, bias=EPS, scale=1.0 / 128), [PK[psE]], [rrk])
                            M.op('act', lambda e: e.activation(out=rr[:], in_=rr[:], func=AF.Exp, scale=-0.5), [rrk], [rrk])
                            t, tk2 = r32.get()
                            M.op('dve', lambda e: e.scalar_tensor_tensor(out=t[:], in0=ps[bo][:], scalar=HGN, in1=rr[:], op0=ALU.mult, op1=ALU.mult),
                                 [PK[bo], rrk, "par"], [tk2])
                            o16, o16k = r16.get()
                            M.op('dve', lambda e: e.tensor_tensor(out=o16[:].rearrange("p (h t) -> p h t", h=2), in0=t[:].rearrange("p (h t) -> p h t", h=2),
                                                                  in1=sg[:, 2 * hp:2 * hp + 2, half * 256:(half + 1) * 256], op=ALU.mult), [tk2, sgk], [o16k])
                            M.dma('sp', OHGv[:, 2 * hp:2 * hp + 2, tcols], o16[:].rearrange("p (h t) -> p h t", h=2), reads=[o16k], writes=[("OHG", j, half, hp)])

                NG = NB * 16
                load_block(0)
                a_cur = stage_a(0)
                for g in range(NG):
                    j, c = divmod(g, 16)
                    if c == 0 and j + 1 < NB:
                        load_block(j + 1)
                    a_nxt = stage_a(g + 1) if g + 1 < NG else None
                    stage_b(g, a_cur)
                    if g % 4 == 3 and l > 0:
                        conv_step(1)
                    a_cur = a_nxt
                M.barrier()

            if want("p2b"):
              with contextlib.ExitStack() as es:
                kp_r = Ring(es, nc, "kp", [66, 2, S], BF16, 2)
                qp_r = Ring(es, nc, "qp", [66, 2, 512], BF16, 3)
                vsb = es.enter_context(nc.sbuf_tensor(_uname("vsb"), [128, NT, 512], BF16))
                pT_r = Ring(es, nc, "pT", [128, 512], BF16, 4)
                rr_r = Ring(es, nc, "arr", [128, 512], F32, 3)
                tn_r = Ring(es, nc, "atn", [128, 512], F32, 4)
                o_r = Ring(es, nc, "ao", [128, 512], F32, 2)
                r16 = Ring(es, nc, "r16", [128, 512], BF16, 3)
                M.dma('sp', vsb[:], VAv[:, :, :], reads=[("VA", j) for j in range(NB)], writes=["vsb"])
                for t_, k_ in zip(kp_r.t, kp_r.k):
                    M.op('dve', lambda e, t_=t_: e.memset(t_[64:66, :, :], 1.0), [], [k_])
                scb = [0, 1, 2]
                sci = [0]
                pairs = [(3, 4), (5, 6)]
                defer = [None]
                for h in range(NH):
                    kp, kpk = kp_r.get()
                    M.dma('sp', kp[0:64, :, :], KA[h].rearrange("c d s -> d c s"), reads=[("KA", j) for j in range(NB)], writes=[kpk])
                    for j in range(NB):
                        cols = slice(j * 512, (j + 1) * 512)
                        qp, qpk = qp_r.get()
                        M.dma('sp', qp[0:64, :, :], QA[h].rearrange("c d s -> d c s")[:, :, cols], reads=[("QA", j)], writes=[qpk])
                        for c in range(2):
                            M.op('pool', lambda e, c=c, qp=qp, h=h: e.tensor_copy(out=qp[64:66, c, :], in_=qrow16[64:66, h * 512:(h + 1) * 512]),
                                 ["qrow16"], [qpk])
                        steps = [(c, kt) for c in range(2) for kt in range(4 * j + 4)]
                        tn = [tn_r.get(), tn_r.get()]
                        conv_step(1)

                        def emit_sc(st):
                            c, kt = st
                            m = kt - 4 * j
                            c0 = 128 * m if m > 0 else 0
                            b = scb[sci[0] % 3]
                            sci[0] += 1
                            M.op('pe', lambda e: e.matmul(ps[b][:, c0:512], kp[:, c, kt * 128:(kt + 1) * 128], qp[:, c, c0:512], start=True, stop=True),
                                 [kpk, qpk], [PK[b]])
                            return b, c0, m

                        def emit_rest(st, info):
                            c, kt = st
                            b, c0, m = info
                            bO_, bL_ = pairs[c]
                            pT, pTk = pT_r.get()
                            idx = kt - 4 * j + 28
                            M.op('act', lambda e: e.activation(out=pT[:, c0:512], in_=ps[b][:, c0:512], func=AF.Exp,
                                                               bias=kbias[:, h * 32 + idx:h * 32 + idx + 1], scale=1.0), [PK[b], "cst"], [pTk])
                            if m >= 0:
                                M.op('dve', lambda e: e.tensor_tensor(out=pT[:, c0:c0 + 128], in0=pT[:, c0:c0 + 128], in1=tri16[:], op=ALU.mult),
                                     [pTk, "tri16"], [pTk])
                            first = (kt == 0)
                            last = (kt == 4 * j + 3)
                            M.op('pe', lambda e: e.matmul(ps[bO_][:, c0:512], vsb[:, kt, h * 128:(h + 1) * 128], pT[:, c0:512], start=first, stop=last,
                                                          skip_group_check=True), ["vsb", pTk], [PK[bO_]], signal=False)
                            M.op('pe', lambda e: e.matmul(ps[bL_][:, c0:512], ones16[:], pT[:, c0:512], start=first, stop=last,
                                                          skip_group_check=True), ["ones16", pTk], [PK[bL_]], signal=True)
                            if last:
                                t_, tk_ = tn[c]
                                M.op('dve', lambda e: e.reciprocal(out=t_[:], in_=ps[bL_][:]), [PK[bL_]], [tk_])
                                M.op('dve', lambda e: e.tensor_tensor(out=t_[:], in0=ps[bO_][:], in1=t_[:], op=ALU.mult), [PK[bO_], tk_], [tk_])

                        def mk_epi(h=h, cols=cols, tn=tn, j=j):
                            def f():
                                (r0, r0k), (r1, r1k) = tn
                                o, ok_ = o_r.get()
                                M.op('dve', lambda e: e.scalar_tensor_tensor(out=o[:], in0=r1[:], scalar=NLAM, in1=r0[:], op0=ALU.mult, op1=ALU.add),
                                     [r0k, r1k] + PKEY, [ok_])
                                oq, oqk = r16.get()
                                M.op('act', lambda e: e.activation(out=oq[:], in_=o[:], func=AF.Square), [ok_], [oqk])
                                M.op('pe', lambda e: e.matmul(ps[7][:], ones16[:], oq[:], start=True, stop=True), [oqk, "ones16"], [PK[7]])
                                rr, rrk = rr_r.get()
                                M.op('act', lambda e: e.activation(out=rr[:], in_=ps[7][:], func=AF.Ln, bias=EPS, scale=1.0 / 128), [PK[7]], [rrk])
                                M.op('act', lambda e: e.activation(out=rr[:], in_=rr[:], func=AF.Exp, scale=-0.5), [rrk], [rrk])
                                o16, o16k = r16.get()
                                M.op('dve', lambda e: e.scalar_tensor_tensor(out=o16[:], in0=o[:], scalar=GSUB, in1=rr[:], op0=ALU.mult, op1=ALU.mult),
                                     [ok_, rrk] + PKEY, [o16k])
                                M.dma('sp', ODAv[:, h, cols], o16[:], reads=[o16k], writes=[("ODA", j)])
                            return f

                        LA = 2
                        infos = [emit_sc(steps[i]) for i in range(min(LA, len(steps)))]
                        for i, st in enumerate(steps):
                            if i + LA < len(steps):
                                infos.append(emit_sc(steps[i + LA]))
                            emit_rest(st, infos[i])
                            if i == 3 and defer[0] is not None:
                                defer[0]()
                                defer[0] = None
                        if defer[0] is not None:
                            defer[0]()
                        defer[0] = mk_epi()
                defer[0]()
                M.barrier()

            moe = (l % 2 == 1)
            if want("p3"):
              with contextlib.ExitStack() as es:
                wpa = es.enter_context(nc.sbuf_tensor(_uname("wpa"), [128, 4, D], BF16))
                wpb = es.enter_context(nc.sbuf_tensor(_uname("wpb"), [128, 4, D], BF16))
                wo = es.enter_context(nc.sbuf_tensor(_uname("wo"), [128, 8, D], BF16))
                M.dma('pool', wpa[:], w_pa[l].rearrange("(kc p) n -> p kc n", p=128), writes=["wpa"])
                M.dma('pool', wpb[:], w_pb[l].rearrange("(kc p) n -> p kc n", p=128), writes=["wpb"])
                M.dma('pool', wo[:], w_o[l].rearrange("(kc p) n -> p kc n", p=128), writes=["wo"])
                if moe:
                    rt32 = es.enter_context(nc.sbuf_tensor(_uname("rt32"), [128, 8, NE], F32))
                    M.dma('sp', rt32[:], router[l // 2].rearrange("(kc p) e -> p kc e", p=128), writes=["rt32"])
                    h32_r = Ring(es, nc, "h32", [128, 8, 512], F32, 1)
                    rs8 = Ring(es, nc, "rs8", [128, 8], F32, 8)
                    rs1 = Ring(es, nc, "rs1", [128, 1], F32, 8)
                    rs16 = Ring(es, nc, "rs16", [128, 8], BF16, 4)
                    zt = es.enter_context(nc.sbuf_tensor(_uname("zt"), [128, 2, D], BF16))
                    M.op('dve', lambda e: e.memset(zt[:], 0.0), [], ["zt"])
                    HSz = HSLOT.rearrange("(n p) d -> p n d", p=128)
                    for n0 in range(0, NSLOT // 128, 2):
                        M.dma('sp', HSz[:, n0:n0 + 2, :], zt[:], reads=["zt"], writes=[("HSZ", n0)])
                    htok_r = Ring(es, nc, "htok", [128, D], BF16, 2)
                    M.op('dve', lambda e: e.memset(rbase[:], 0.0), [], ["rbase"])
                fold_ada = (l == 0 and nlayers > 1)
                if fold_ada:
                    awr2 = Ring(es, nc, "aw2", [128, 8, 768], F32, 1)
                    lt2 = es.enter_context(nc.sbuf_tensor(_uname("lt2"), [128, 64], F32))
                oh_r = Ring(es, nc, "oh", [128, 4, 512], BF16, 2)
                od_r = Ring(es, nc, "od", [128, 4, 512], BF16, 2)
                ga_r = Ring(es, nc, "ga", [128, 8, 512], BF16, 2)
                gb_r = Ring(es, nc, "gb", [128, 8, 512], BF16, 2)
                xs_r = Ring(es, nc, "xs", [128, 8, 512], F32, 2)
                yT_r = Ring(es, nc, "yT", [128, 8, 512], BF16, 2)
                sq = es.enter_context(nc.sbuf_tensor(_uname("sq"), [128, 8, 512], BF16))
                hT_r = Ring(es, nc, "hT", [128, 8, 512], BF16, 2)
                r32 = Ring(es, nc, "r32", [128, 512], F32, 4)
                rsr = Ring(es, nc, "rsr", [128, 512], F32, 1)
                pb = [1, 2, 3, 4, 5, 6]
                pbi = [0]

                def nb_():
                    b = pb[pbi[0] % len(pb)]
                    pbi[0] += 1
                    return b

                def p3_load(j):
                    cols = slice(j * 512, (j + 1) * 512)
                    oh, ohk = oh_r.get()
                    od, odk = od_r.get()
                    ga, gak = ga_r.get()
                    gb, gbk = gb_r.get()
                    xs, xsk = xs_r.get()
                    M.dma('sp', oh[:], OHGv[:, :, cols], reads=[("OHG", j, hf_, hp_) for hf_ in range(2) for hp_ in range(2)], writes=[ohk])
                    M.dma('sp', od[:], ODAv[:, :, cols], reads=[("ODA", j)], writes=[odk])
                    M.dma('sp', ga[:], GAv[:, :, cols], reads=[("GA", j)], writes=[gak])
                    M.dma('sp', gb[:], GBv[:, :, cols], reads=[("GB", j)], writes=[gbk])
                    M.dma('sp', xs[:], xTv[:, :, cols], reads=[("xT", j)], writes=K8(xsk))
                    return (j, cols, oh, ohk, od, odk, ga, gak, gb, gbk, xs, xsk)
                def p3_mix(L):
                    j, cols, oh, ohk, od, odk, ga, gak, gb, gbk, xs, xsk = L
                    yT, yTk = yT_r.get()
                    for dc in range(8):
                        ba = nb_()
                        bb = nb_()
                        for kc in range(4):
                            M.op('pe', lambda e, kc=kc, dc=dc, ba=ba: e.matmul(ps[ba][:], wpa[:, kc, dc * 128:(dc + 1) * 128], oh[:, kc, :], start=(kc == 0), stop=(kc == 3)),
                                 ["wpa", ohk], [PK[ba]], signal=(kc == 3))
                        for kc in range(4):
                            M.op('pe', lambda e, kc=kc, dc=dc, bb=bb: e.matmul(ps[bb][:], wpb[:, kc, dc * 128:(dc + 1) * 128], od[:, kc, :], start=(kc == 0), stop=(kc == 3)),
                                 ["wpb", odk], [PK[bb]], signal=(kc == 3))
                        t1, t1k = r32.get()
                        t2, t2k = r32.get()
                        M.op('dve', lambda e, t1=t1, ba=ba, dc=dc: e.tensor_tensor(out=t1[:], in0=ps[ba][:], in1=ga[:, dc, :], op=ALU.mult), [PK[ba], gak], [t1k])
                        M.op('dve', lambda e, t2=t2, bb=bb, dc=dc: e.tensor_tensor(out=t2[:], in0=ps[bb][:], in1=gb[:, dc, :], op=ALU.mult), [PK[bb], gbk], [t2k])
                        M.op('pool', lambda e, t1=t1, t2=t2, dc=dc: e.tensor_tensor(out=yT[:, dc, :], in0=t1[:], in1=t2[:], op=ALU.add), [t1k, t2k], [(yTk, dc)])
                    return (yT, yTk)
                def p3_out(L, Y):
                    j, cols, oh, ohk, od, odk, ga, gak, gb, gbk, xs, xsk = L
                    yT, yTk = Y
                    for dc in range(8):
                        b = nb_()
                        for kc in range(8):
                            M.op('pe', lambda e, kc=kc, dc=dc, b=b: e.matmul(ps[b][:], wo[:, kc, dc * 128:(dc + 1) * 128], yT[:, kc, :], start=(kc == 0), stop=(kc == 7)),
                                 ["wo", (yTk, kc)], [PK[b]], signal=(kc == 7))
                        M.op('dve', lambda e, dc=dc, b=b: e.scalar_tensor_tensor(out=xs[:, dc, :], in0=ps[b][:], scalar=G1[:, dc:dc + 1], in1=xs[:, dc, :],
                                                                              op0=ALU.mult, op1=ALU.add), [PK[b], (xsk, dc)] + PKEY, [(xsk, dc)])
                    M.dma('sp', xTv[:, :, cols], xs[:], reads=K8(xsk), writes=[("xT", j)])
                def p3_norm(L):
                    j, cols, oh, ohk, od, odk, ga, gak, gb, gbk, xs, xsk = L
                    hT, hTk = hT_r.get()
                    if moe:
                        h32, h32k = h32_r.get()
                        norm_block(xs, xsk, A2, B2, PKEY[0], hT, hTk, sq, "sq", r32, 0, rsr, h32=(h32, h32k))
                    else:
                        norm_block(xs, xsk, A2, B2, PKEY[0], hT, hTk, sq, "sq", r32, 0, rsr)
                    if not moe:
                        M.dma('sp', H2v[:, :, cols], hT[:], reads=K8(hTk), writes=[("H2", j)])
                    if moe:
                        for tt in range(4):
                            t_ = j * 4 + tt
                            for kc in range(8):
                                M.op('pe', lambda e, kc=kc, tt=tt: e.matmul(ps[7][:, 0:NE], h32[:, kc, tt * 128:(tt + 1) * 128], rt32[:, kc, :],
                                                                          start=(kc == 0), stop=(kc == 7)), [(h32k, kc), "rt32"], [PK[7]], signal=(kc == 7))
                            lg, lgk = rs8.get()
                            M.op('dve', lambda e, lg=lg: e.tensor_copy(out=lg[:], in_=ps[7][:, 0:NE]), [PK[7]], [lgk])
                            m1, m1k = rs1.get()
                            M.op('dve', lambda e, lg=lg, m1=m1: e.reduce_max(out=m1[:], in_=lg[:], axis=AX.X), [lgk], [m1k])
                            RK = ("route", t_)
                            M.op('dve', lambda e, lg=lg, m1=m1: e.tensor_scalar(out=m1_all[:, t_, :], in0=lg[:], scalar1=m1[:, 0:1], scalar2=None, op0=ALU.is_equal), [lgk, m1k], [RK])
                            eq, eqk = rs8.get()
                            M.op('dve', lambda e, lg=lg, eq=eq: e.scalar_tensor_tensor(out=eq[:], in0=m1_all[:, t_, :], scalar=-1e30, in1=lg[:], op0=ALU.mult, op1=ALU.add), [lgk, RK], [eqk])
                            m2, m2k = rs1.get()
                            M.op('dve', lambda e, eq=eq, m2=m2: e.reduce_max(out=m2[:], in_=eq[:], axis=AX.X), [eqk], [m2k])
                            M.op('dve', lambda e, lg=lg, m2=m2: e.tensor_scalar(out=sel_all[:, t_, :], in0=lg[:], scalar1=m2[:, 0:1], scalar2=None, op0=ALU.is_ge), [lgk, m2k], [RK])
                            M.op('dve', lambda e, m1=m1: e.tensor_scalar(out=m1[:], in0=m1[:], scalar1=-1.0, scalar2=None, op0=ALU.mult), [m1k], [m1k])
                            ex, exk = rs8.get()
                            M.op('act', lambda e, lg=lg, m1=m1, ex=ex: e.activation(out=ex[:], in_=lg[:], func=AF.Exp, bias=m1[:, 0:1], scale=1.0), [lgk, m1k], [exk])
                            M.op('dve', lambda e, ex=ex: e.tensor_tensor(out=ex[:], in0=ex[:], in1=sel_all[:, t_, :], op=ALU.mult), [exk, RK], [exk])
                            M.op('dve', lambda e, ex=ex, m2=m2: e.reduce_sum(out=m2[:], in_=ex[:], axis=AX.X), [exk, m2k], [m2k])
                            M.op('dve', lambda e, m2=m2: e.reciprocal(out=m2[:], in_=m2[:]), [m2k], [m2k])
                            M.op('dve', lambda e, ex=ex, m2=m2: e.tensor_scalar(out=comb_all[:, t_, :], in0=ex[:], scalar1=m2[:, 0:1], scalar2=None, op0=ALU.mult), [exk, m2k], [RK])
                            s16, s16k = rs16.get()
                            M.op('dve', lambda e, s16=s16: e.tensor_copy(out=s16[:], in_=sel_all[:, t_, :]), [RK], [s16k])
                            M.op('pe', lambda e, s16=s16: e.matmul(ps[7][:, 16:16 + NE], us16[:], s16[:], start=True, stop=True), [s16k, "us16"], [PK[7]], signal=False)
                            M.op('pe', lambda e, s16=s16: e.matmul(ps[7][:, 32:32 + NE], ones16[:], s16[:], start=True, stop=True), [s16k, "ones16"], [PK[7]])
                            M.op('dve', lambda e: e.tensor_tensor(out=rank_all[:, t_, :], in0=ps[7][:, 16:16 + NE], in1=rbase[:], op=ALU.add), [PK[7], "rbase"], [RK])
                            M.op('dve', lambda e: e.tensor_tensor(out=rbase[:], in0=ps[7][:, 32:32 + NE], in1=rbase[:], op=ALU.add), [PK[7], "rbase"], ["rbase"])
                            ht, htk = htok_r.get()
                            psb16 = ps[7][:].bitcast(BF16)
                            for kc in range(8):
                                M.op('pe', lambda e, kc=kc, tt=tt: e.transpose(psb16[:, kc * 128:(kc + 1) * 128], hT[:, kc, tt * 128:(tt + 1) * 128], id16[:]),
                                     [(hTk, kc), "id16"], [PK[7]], signal=(kc == 7))
                            M.op('act', lambda e, ht=ht: e.copy(out=ht[:], in_=psb16[:, 0:1024]), [PK[7]], [htk])
                            M.dma('sp', H2TOK[t_ * 128:(t_ + 1) * 128, :], ht[:], reads=[htk], writes=[("H2TOK", t_)])
                L_cur = p3_load(0)
                Y_cur = p3_mix(L_cur)
                for j in range(NB):
                    if l == 0:
                        conv_step(2)
                    if fold_ada:
                        gl = ([j] if j < 8 else []) if NB >= 8 else [g_ for g_ in range(8) if g_ % NB == j]
                        for g_ in gl:
                            ada_group(1, g_, awr2, 7)
                        if j == NB - 1:
                            ada_finish(1, lt2, 7)
                    L_nxt = p3_load(j + 1) if j + 1 < NB else None
                    p3_out(L_cur, Y_cur)
                    Y_nxt = p3_mix(L_nxt) if L_nxt is not None else None
                    p3_norm(L_cur)
                    L_cur, Y_cur = L_nxt, Y_nxt
                M.barrier()

            if want("p4") and not moe:
              last = (l == nlayers - 1)
              if not last and want("p1"):
                  win_prefetch(l + 1)
              with contextlib.ExitStack() as es:
                TBF = min(S, 1024)
                NSB = TBF // 512
                if moe:
                    nexp, dff, FG = NE, DFE, 4
                    W1 = lambda e_: m_w1[l // 2, e_]
                    W3 = lambda e_: m_w3[l // 2, e_]
                    W2 = lambda e_: m_w2[l // 2, e_]
                else:
                    nexp, dff, FG = 1, DFF, 2
                    W1 = lambda e_: f_w1[l // 2]
                    W3 = lambda e_: f_w3[l // 2]
                    W2 = lambda e_: f_w2[l // 2]
                NFG = dff // (FG * 128)
                FW = FG * 128
                h2 = es.enter_context(nc.sbuf_tensor(_uname("h2"), [128, 8, TBF], BF16))
                yacc = es.enter_context(nc.sbuf_tensor(_uname("yacc"), [128, 8, TBF], F32))
                w1_r = Ring(es, nc, "w1g", [128, 8, FW], BF16, 2)
                w3_r = Ring(es, nc, "w3g", [128, 8, FW], BF16, 2)
                w2_r = Ring(es, nc, "w2g", [128, FG, D], BF16, 2)
                aT_r = Ring(es, nc, "aT", [128, FG, 512], BF16, 2)
                s_r = Ring(es, nc, "sil", [128, 512], F32, 2)
                if moe:
                    t_r = Ring(es, nc, "tt", [128, 512], F32, 3)
                    cb_r = Ring(es, nc, "cb", [128, TBF], F32, 2)
                if last:
                    xs_r = Ring(es, nc, "xs", [128, 8, 512], F32, 1)
                    ot_r = Ring(es, nc, "ot", [128, D], F32, 2)
                else:
                    xc_r = Ring(es, nc, "xc", [128, 512], F32, 3)
                ub = [0, 1, 2, 3]
                ubi = [0]
                yb = [4, 5, 6]
                ybi = [0]
                for p in range(S // TBF):
                    tc0 = p * TBF
                    M.dma('sp', h2[:], H2v[:, :, tc0:tc0 + TBF], reads=[("H2", jj) for jj in range(NB)], writes=["h2"])
                    yfirst = [True] * (NSB * 8)
                    groups = [(e_, fg) for e_ in range(nexp) for fg in range(NFG)]
                    pend = None
                    cb = cbk = None
                    for gi, (e_, fg) in enumerate(groups):
                        conv_step(1)
                        if moe and fg == 0:
                            cb, cbk = cb_r.get()
                            M.dma('sp', cb[:], COMBT[e_:e_ + 1, tc0:tc0 + TBF].partition_broadcast(128), reads=[("COMBT", jj) for jj in range(NB)], writes=[cbk])
                        w1g, w1k = w1_r.get()
                        w3g, w3k = w3_r.get()
                        w2g, w2k = w2_r.get()
                        fcs = slice(fg * FW, (fg + 1) * FW)
                        M.dma('pool', w1g[:], W1(e_).rearrange("(kc p) f -> p kc f", p=128)[:, :, fcs], writes=[w1k])
                        M.dma('pool', w3g[:], W3(e_).rearrange("(kc p) f -> p kc f", p=128)[:, :, fcs], writes=[w3k])
                        M.dma('pool', w2g[:], W2(e_)[fg * FW:(fg + 1) * FW, :].rearrange("(fc p) d -> p fc d", p=128), writes=[w2k])
                        for sbi in range(NSB):
                            sc_ = slice(sbi * 512, (sbi + 1) * 512)
                            aT, aTk = aT_r.get()
                            for fc in range(FG):
                                b1 = ub[ubi[0] % 4]
                                b3 = ub[(ubi[0] + 1) % 4]
                                ubi[0] += 2
                                for kc in range(8):
                                    M.op('pe', lambda e, kc=kc, fc=fc, b1=b1, w1g=w1g: e.matmul(ps[b1][:], w1g[:, kc, fc * 128:(fc + 1) * 128], h2[:, kc, sc_],
                                                                                           start=(kc == 0), stop=(kc == 7)), [w1k, "h2"], [PK[b1]], signal=(kc == 7))
                                for kc in range(8):
                                    M.op('pe', lambda e, kc=kc, fc=fc, b3=b3, w3g=w3g: e.matmul(ps[b3][:], w3g[:, kc, fc * 128:(fc + 1) * 128], h2[:, kc, sc_],
                                                                                           start=(kc == 0), stop=(kc == 7)), [w3k, "h2"], [PK[b3]], signal=(kc == 7))
                                s, sk = s_r.get()
                                M.op('act', lambda e, s=s, b1=b1: e.activation(out=s[:], in_=ps[b1][:], func=AF.Silu), [PK[b1]], [sk])
                                if moe:
                                    t, tk = t_r.get()
                                    M.op('dve', lambda e, s=s, b3=b3, t=t: e.tensor_tensor(out=t[:], in0=ps[b3][:], in1=s[:], op=ALU.mult), [PK[b3], sk], [tk])
                                    M.op('pool', lambda e, t=t, fc=fc, aT=aT, cb=cb: e.tensor_tensor(out=aT[:, fc, :], in0=t[:], in1=cb[:, sc_], op=ALU.mult), [tk, cbk], [aTk])
                                else:
                                    M.op('dve', lambda e, s=s, b3=b3, fc=fc, aT=aT: e.tensor_tensor(out=aT[:, fc, :], in0=ps[b3][:], in1=s[:], op=ALU.mult), [PK[b3], sk], [aTk])
                            if pend is not None:
                                pend()

                            def mk_pend(aT=aT, aTk=aTk, w2g=w2g, w2k=w2k, sbi=sbi, sc_=sc_):
                                def f():
                                    for dc in range(8):
                                        b = yb[ybi[0] % 3]
                                        ybi[0] += 1
                                        for fc in range(FG):
                                            M.op('pe', lambda e, fc=fc, dc=dc, b=b: e.matmul(ps[b][:], w2g[:, fc, dc * 128:(dc + 1) * 128], aT[:, fc, :],
                                                                                           start=(fc == 0), stop=(fc == FG - 1)), [w2k, aTk], [PK[b]], signal=(fc == FG - 1))
                                        yk = ("yacc", sbi, dc)
                                        if yfirst[sbi * 8 + dc]:
                                            yfirst[sbi * 8 + dc] = False
                                            M.op('dve', lambda e, dc=dc, b=b: e.tensor_copy(out=yacc[:, dc, sc_], in_=ps[b][:]), [PK[b]], [yk])
                                        else:
                                            M.op('dve', lambda e, dc=dc, b=b: e.tensor_tensor(out=yacc[:, dc, sc_], in0=ps[b][:], in1=yacc[:, dc, sc_], op=ALU.add),
                                                 [PK[b], yk], [yk])
                                return f
                            pend = mk_pend()
                    pend()
                    for sbi in range(NSB):
                        sc_ = slice(sbi * 512, (sbi + 1) * 512)
                        jb = (tc0 // 512) + sbi
                        gcols = slice(tc0 + sbi * 512, tc0 + (sbi + 1) * 512)
                        if not last:
                            for dc in range(8):
                                xc, xck = xc_r.get()
                                M.dma('sp', xc[:], xTv[:, dc, gcols], reads=[("xT", jb)], writes=[xck])
                                M.op('dve', lambda e, dc=dc, xc=xc: e.scalar_tensor_tensor(out=yacc[:, dc, sc_], in0=yacc[:, dc, sc_], scalar=G2[:, dc:dc + 1], in1=xc[:],
                                                                                        op0=ALU.mult, op1=ALU.add), [("yacc", sbi, dc), xck] + PKEY, [("yacc", sbi, dc)])
                            M.dma('sp', xTv[:, :, gcols], yacc[:, :, sc_], reads=[("yacc", sbi, dc) for dc in range(8)], writes=[("xT", jb)])
                            continue
                        xs, xsk = xs_r.get()
                        M.dma('sp', xs[:], xTv[:, :, gcols], reads=[("xT", jb)], writes=K8(xsk))
                        for dc in range(8):
                            M.op('dve', lambda e, dc=dc: e.scalar_tensor_tensor(out=xs[:, dc, :], in0=yacc[:, dc, sc_], scalar=G2[:, dc:dc + 1], in1=xs[:, dc, :],
                                                                             op0=ALU.mult, op1=ALU.add), [("yacc", sbi, dc), (xsk, dc)] + PKEY, [(xsk, dc)])
                        if True:
                            for tt in range(4):
                                ot, otk = ot_r.get()
                                for half in range(2):
                                    b = ub[ubi[0] % 4]
                                    ubi[0] += 1
                                    for q in range(4):
                                        dc = half * 4 + q
                                        M.op('pe', lambda e, b=b, q=q, dc=dc, tt=tt: e.transpose(ps[b][:, q * 128:(q + 1) * 128], xs[:, dc, tt * 128:(tt + 1) * 128], id32),
                                             [(xsk, dc), "cst"], [PK[b]], signal=(q == 3))
                                    if half == 0:
                                        M.op('act', lambda e, b=b, ot=ot: e.copy(out=ot[:, 0:512], in_=ps[b][:]), [PK[b]], [otk])
                                    else:
                                        M.op('dve', lambda e, b=b, ot=ot: e.tensor_copy(out=ot[:, 512:1024], in_=ps[b][:]), [PK[b]], [otk])
                                r0 = tc0 + sbi * 512 + tt * 128
                                M.dma('sp', out[r0:r0 + 128, :], ot[:], reads=[otk], writes=[("out", r0)])
                M.barrier()
            if want("p4") and moe:
              assert l == nlayers - 1
              with contextlib.ExitStack() as es:
                nexp, dff, FG = NE, DFE, 4
                NFG = dff // (FG * 128)
                FW = FG * 128
                NSUB = TS // 128
                ei = l // 2
                conv_step(10 ** 6)
                assert FG == MFG and cconv[0] == NE * MNFG * 3 * 16, cconv[0]
                for en_ in ('pool',):
                    nc.gpsimd.wait_ge(csem, cconv[0])
                sm8 = Ring(es, nc, "sm8", [128, NE], F32, 6)
                big = Ring(es, nc, "big", [128, NT, NE], F32, 3)
                t88 = es.enter_context(nc.sbuf_tensor(_uname("t88"), [128, NE, 8], F32))
                tx = es.enter_context(nc.sbuf_tensor(_uname("tx"), [128, TMAX, NE], F32))
                slot1f = es.enter_context(nc.sbuf_tensor(_uname("slot1f"), [128, NT], F32))
                slot2f = es.enter_context(nc.sbuf_tensor(_uname("slot2f"), [128, NT], F32))
                slot1i = es.enter_context(nc.sbuf_tensor(_uname("slot1i"), [128, NT], mybir.dt.int32))
                slot2i = es.enter_context(nc.sbuf_tensor(_uname("slot2i"), [128, NT], mybir.dt.int32))
                c1 = es.enter_context(nc.sbuf_tensor(_uname("c1"), [128, NT], F32))
                c2 = es.enter_context(nc.sbuf_tensor(_uname("c2"), [128, NT], F32))
                texf = es.enter_context(nc.sbuf_tensor(_uname("texf"), [128, TMAX], F32))
                widf = es.enter_context(nc.sbuf_tensor(_uname("widf"), [128, TMAX, NFG], F32))
                widi = es.enter_context(nc.sbuf_tensor(_uname("widi"), [128, TMAX, NFG], mybir.dt.int32))
                th = cst32[:, C_TH:C_TH + 8]
                tv = cst32[:, C_TV:C_TV + TMAX]
                M.op('dve', lambda e: e.tensor_tensor(out=t88[:], in0=rbase[:].unsqueeze(2).to_broadcast([128, NE, 8]),
                                                      in1=th.unsqueeze(1).to_broadcast([128, NE, 8]), op=ALU.is_gt), ["rbase", "cst"], ["t88"])
                pe_, pek = sm8.get()
                M.op('dve', lambda e: e.tensor_reduce(out=pe_[:], in_=t88[:], axis=AX.X, op=ALU.add), ["t88"], [pek])
                M.op('dve', lambda e: e.tensor_scalar(out=pe_[:], in0=pe_[:], scalar1=float(TS), scalar2=None, op0=ALU.mult), [pek], [pek])
                on8, on8k = sm8.get()
                M.op('dve', lambda e: e.memset(on8[:], 1.0), [], [on8k])
                incl, inclk = sm8.get()
                M.op('dve', lambda e: e.tensor_tensor_scan(out=incl[:], data0=on8[:], data1=pe_[:], initial=0.0, op0=ALU.mult, op1=ALU.add), [on8k, pek], [inclk])
                off, offk = sm8.get()
                M.op('dve', lambda e: e.tensor_tensor(out=off[:], in0=incl[:], in1=pe_[:], op=ALU.subtract), [inclk, pek], [offk])
                RKS = [("route", t_) for t_ in range(NT)]
                slot, slotk = big.get()
                M.op('dve', lambda e: e.tensor_tensor(out=slot[:], in0=rank_all[:], in1=off[:].unsqueeze(1).to_broadcast([128, NT, NE]), op=ALU.add), RKS + [offk], [slotk])
                m2a, m2ak = big.get()
                M.op('dve', lambda e: e.tensor_tensor(out=m2a[:], in0=sel_all[:], in1=m1_all[:], op=ALU.subtract), RKS, [m2ak])
                tmp, tmpk = big.get()
                for (dst, dk, a_, ak, b_, bk) in ((slot1f, "slot1f", slot, slotk, m1_all, None), (slot2f, "slot2f", slot, slotk, m2a, m2ak),
                                                  (c1, "c1", comb_all, None, m1_all, None), (c2, "c2", comb_all, None, m2a, m2ak)):
                    rk = [k for k in (ak, bk) if k is not None]
                    M.op('dve', lambda e, a_=a_, b_=b_: e.tensor_tensor(out=tmp[:], in0=a_[:], in1=b_[:], op=ALU.mult), rk, [tmpk])
                    M.op('dve', lambda e, dst=dst: e.tensor_reduce(out=dst[:], in_=tmp[:], axis=AX.X, op=ALU.add), [tmpk], [dk])
                M.op('dve', lambda e: e.tensor_copy(out=slot1i[:], in_=slot1f[:]), ["slot1f"], ["slot1i"])
                M.op('dve', lambda e: e.tensor_copy(out=slot2i[:], in_=slot2f[:]), ["slot2f"], ["slot2i"])
                M.op('dve', lambda e: e.tensor_tensor(out=tx[:], in0=incl[:].unsqueeze(1).to_broadcast([128, TMAX, NE]),
                                                      in1=tv.unsqueeze(2).to_broadcast([128, TMAX, NE]), op=ALU.is_le), [inclk, "cst"], ["tx"])
                M.op('dve', lambda e: e.tensor_reduce(out=texf[:], in_=tx[:], axis=AX.X, op=ALU.add), ["tx"], ["texf"])
                M.op('dve', lambda e: e.tensor_scalar(out=texf[:], in0=texf[:], scalar1=float(NE - 1), scalar2=None, op0=ALU.min), ["texf"], ["texf"])
                M.op('dve', lambda e: e.tensor_scalar(out=texf[:], in0=texf[:], scalar1=float(NFG * 128), scalar2=cst32[:, C_PI:C_PI + 1], op0=ALU.mult, op1=ALU.add),
                     ["texf", "cst"], ["texf"])
                for fg in range(NFG):
                    M.op('dve', lambda e, fg=fg: e.tensor_scalar(out=widf[:, :, fg], in0=texf[:], scalar1=float(fg * 128), scalar2=None, op0=ALU.add), ["texf"], ["widf"])
                M.op('dve', lambda e: e.tensor_copy(out=widi[:], in_=widf[:]), ["widf"], ["widi"])
                with contextlib.ExitStack() as es2:
                  htk_r = Ring(es2, nc, "htk2", [128, D], BF16, 3)
                  HSZK = []
                  for t_ in range(NT):
                      ht, htk = htk_r.get()
                      M.dma('sp', ht[:], H2TOK[t_ * 128:(t_ + 1) * 128, :], writes=[htk])
                      for (si, sk) in ((slot1i, "slot1i"), (slot2i, "slot2i")):
                          M.dmai(HSLOT[:, :], bass.IndirectOffsetOnAxis(si[:, t_:t_ + 1], 0), ht[:], None, reads=[htk, sk] + HSZK, writes=[("HSLOT", t_, sk)])
                  M.barrier()
                with contextlib.ExitStack() as es2:
                  hs_r = Ring(es2, nc, "hs", [128, NSUB, D], BF16, 2)
                  h2_r = Ring(es2, nc, "h2s", [128, 8, TS], BF16, 2)
                  ya_r = Ring(es2, nc, "yacs", [128, 8, TS], F32, 2)
                  w1_r = Ring(es2, nc, "w1g", [128, 8, FW], BF16, 2)
                  w3_r = Ring(es2, nc, "w3g", [128, 8, FW], BF16, 2)
                  w2_r = Ring(es2, nc, "w2g", [128, FG, D], BF16, 2)
                  aT_r = Ring(es2, nc, "aT", [128, FG, 512], BF16, 2)
                  s_r = Ring(es2, nc, "sil", [128, 512], F32, 3)
                  ys_r = Ring(es2, nc, "ysb", [128, D], F32, 2)
                  ub = [0, 1, 2, 3]
                  ubi = [0]
                  yb = [4, 5]
                  ybi = [0]
                  HSv = HSLOT.rearrange("(i u p) d -> i p u d", p=128, u=NSUB)
                  tb = [6, 7]
                  tbi = [0]

                  def load_hs(i):
                      hs, hsk = hs_r.get()
                      M.dma('sp', hs[:], HSv[i], writes=[hsk])
                      return hs, hsk

                  nxt_hs = load_hs(0)
                  pend = None
                  fin = None
                  for i in range(TMAX):
                      hs, hsk = nxt_hs
                      if i + 1 < TMAX:
                          nxt_hs = load_hs(i + 1)
                      h2, h2k = h2_r.get()
                      for u in range(NSUB):
                          bt = tb[tbi[0] % 2]
                          tbi[0] += 1
                          psTb_ = ps[bt][:].bitcast(BF16)
                          for kc in range(8):
                              M.op('pe', lambda e, u=u, kc=kc: e.transpose(psTb_[:, kc * 128:(kc + 1) * 128], hs[:, u, kc * 128:(kc + 1) * 128], id16[:]),
                                   [hsk, "id16"], [PK[bt]], signal=(kc == 7))
                          M.op('act', lambda e, u=u: e.copy(out=h2[:, :, u * 128:(u + 1) * 128], in_=psTb_[:, 0:1024].rearrange("p (k t) -> p k t", k=8)),
                               [PK[bt]], [(h2k, u)])
                      H2K = [(h2k, u) for u in range(NSUB)]
                      yacc, yak = ya_r.get()
                      yfirst = [True] * 8
                      for fg in range(NFG):
                          w1g, w1k = w1_r.get()
                          w3g, w3k = w3_r.get()
                          w2g, w2k = w2_r.get()
                          fcs = slice(fg * FW, (fg + 1) * FW)
                          ioff = bass.IndirectOffsetOnAxis(widi[:, i, fg:fg + 1], 0)
                          M.dmai(w1g[:].rearrange("p a b -> p (a b)"), None, W1C[:, :], ioff, reads=["widi"], writes=[w1k])
                          M.dmai(w3g[:].rearrange("p a b -> p (a b)"), None, W3C[:, :], ioff, reads=["widi"], writes=[w3k])
                          M.dmai(w2g[:].rearrange("p a b -> p (a b)"), None, W2C[:, :], ioff, reads=["widi"], writes=[w2k])
                          for sbi in range(TS // 512):
                              sc_ = slice(sbi * 512, (sbi + 1) * 512)
                              aT, aTk = aT_r.get()
                              for fc in range(FG):
                                  b1 = ub[ubi[0] % 4]
                                  b3 = ub[(ubi[0] + 1) % 4]
                                  ubi[0] += 2
                                  for kc in range(8):
                                      M.op('pe', lambda e, kc=kc, fc=fc, b1=b1: e.matmul(ps[b1][:], w1g[:, kc, fc * 128:(fc + 1) * 128], h2[:, kc, sc_],
                                                                                       start=(kc == 0), stop=(kc == 7)), [w1k] + H2K, [PK[b1]], signal=(kc == 7))
                                  for kc in range(8):
                                      M.op('pe', lambda e, kc=kc, fc=fc, b3=b3: e.matmul(ps[b3][:], w3g[:, kc, fc * 128:(fc + 1) * 128], h2[:, kc, sc_],
                                                                                       start=(kc == 0), stop=(kc == 7)), [w3k] + H2K, [PK[b3]], signal=(kc == 7))
                                  s_, sk_ = s_r.get()
                                  M.op('act', lambda e, s_=s_, b1=b1: e.activation(out=s_[:], in_=ps[b1][:], func=AF.Silu), [PK[b1]], [sk_])
                                  M.op('dve', lambda e, s_=s_, b3=b3, fc=fc: e.tensor_tensor(out=aT[:, fc, :], in0=ps[b3][:], in1=s_[:], op=ALU.mult), [PK[b3], sk_], [(aTk, fc)])
                              if pend is not None:
                                  pend()
                              if fin is not None:
                                  fin()
                                  fin = None

                              def mk_pend(aT=aT, aTk=aTk, w2g=w2g, w2k=w2k, sc_=sc_, yacc=yacc, yak=yak, yfirst=yfirst):
                                  def f():
                                      for dc in range(8):
                                          b = yb[ybi[0] % 2]
                                          ybi[0] += 1
                                          for fc in range(FG):
                                              M.op('pe', lambda e, fc=fc, dc=dc, b=b: e.matmul(ps[b][:], w2g[:, fc, dc * 128:(dc + 1) * 128], aT[:, fc, :],
                                                                                             start=(fc == 0), stop=(fc == FG - 1)), [w2k, (aTk, fc)], [PK[b]], signal=(fc == FG - 1))
                                          yk = (yak, dc)
                                          if yfirst[dc]:
                                              yfirst[dc] = False
                                              M.op('dve', lambda e, dc=dc, b=b: e.tensor_copy(out=yacc[:, dc, sc_], in_=ps[b][:]), [PK[b]], [yk])
                                          else:
                                              M.op('dve', lambda e, dc=dc, b=b: e.tensor_tensor(out=yacc[:, dc, sc_], in0=ps[b][:], in1=yacc[:, dc, sc_], op=ALU.add),
                                                   [PK[b], yk], [yk])
                                  return f
                              pend = mk_pend()

                      def mk_fin(i=i, yacc=yacc, yak=yak):
                          def f():
                              for dc in range(8):
                                  M.op('act', lambda e, dc=dc: e.activation(out=yacc[:, dc, :], in_=yacc[:, dc, :], func=AF.Copy, scale=G2[:, dc:dc + 1]),
                                       [(yak, dc)] + PKEY, [(yak, dc)])
                              for u in range(NSUB):
                                  ysb, ysk = ys_r.get()
                                  for half in range(2):
                                      bt = tb[tbi[0] % 2]
                                      tbi[0] += 1
                                      for q in range(4):
                                          dc = half * 4 + q
                                          M.op('pe', lambda e, q=q, dc=dc, u=u: e.transpose(ps[bt][:, q * 128:(q + 1) * 128], yacc[:, dc, u * 128:(u + 1) * 128], id32),
                                               [(yak, dc), "cst"], [PK[bt]], signal=(q == 3))
                                      M.op('act', lambda e, half=half, ysb=ysb: e.copy(out=ysb[:, half * 512:(half + 1) * 512], in_=ps[bt][:]), [PK[bt]], [ysk])
                                  r0 = i * TS + u * 128
                                  M.dma('sp', YSLOT[r0:r0 + 128, :], ysb[:], reads=[ysk], writes=[("YSLOT", i, u)])
                          return f
                      fin = mk_fin()
                  pend()
                  fin()
                  M.barrier()
                with contextlib.ExitStack() as es2:
                  xs_r = Ring(es2, nc, "xs", [128, 8, 512], F32, 2)
                  y1_r = Ring(es2, nc, "y1", [128, D], F32, 4)
                  y2_r = Ring(es2, nc, "y2", [128, D], F32, 4)
                  ot_r = Ring(es2, nc, "ot", [128, D], F32, 3)
                  for j in range(NB):
                      xs, xsk = xs_r.get()
                      M.dma('sp', xs[:], xTv[:, :, j * 512:(j + 1) * 512], writes=[xsk])
                      for tt in range(4):
                          t_ = j * 4 + tt
                          y1, y1k = y1_r.get()
                          y2, y2k = y2_r.get()
                          M.dmai(y1[:], None, YSLOT[:, :], bass.IndirectOffsetOnAxis(slot1i[:, t_:t_ + 1], 0), reads=["slot1i"], writes=[y1k])
                          M.dmai(y2[:], None, YSLOT[:, :], bass.IndirectOffsetOnAxis(slot2i[:, t_:t_ + 1], 0), reads=["slot2i"], writes=[y2k])
                          ot, otk = ot_r.get()
                          for half in range(2):
                              b = ub[ubi[0] % 4]
                              ubi[0] += 1
                              for q in range(4):
                                  dc = half * 4 + q
                                  M.op('pe', lambda e, b=b, q=q, dc=dc, tt=tt: e.transpose(ps[b][:, q * 128:(q + 1) * 128], xs[:, dc, tt * 128:(tt + 1) * 128], id32),
                                       [xsk, "cst"], [PK[b]], signal=(q == 3))
                              hc = slice(half * 512, (half + 1) * 512)
                              M.op('dve', lambda e, b=b, hc=hc: e.scalar_tensor_tensor(out=ot[:, hc], in0=y1[:, hc], scalar=c1[:, t_:t_ + 1], in1=ps[b][:], op0=ALU.mult, op1=ALU.add),
                                   [y1k, "c1", PK[b]], [(otk, half)])
                              M.op('dve', lambda e, hc=hc: e.scalar_tensor_tensor(out=ot[:, hc], in0=y2[:, hc], scalar=c2[:, t_:t_ + 1], in1=ot[:, hc], op0=ALU.mult, op1=ALU.add),
                                   [y2k, "c2", (otk, half)], [(otk, half)])
                          M.dma('sp', out[t_ * 128:(t_ + 1) * 128, :], ot[:], reads=[(otk, 0), (otk, 1)], writes=[("out", t_)])
                  M.barrier()
        M.finish()
    build.stats = (M.nops, M.nwaits)
    return nc


def make_consts():
    c = np.zeros((128, NCST), np.float32)
    p = np.arange(128)
    c[:, C_ID:C_ID + 128] = np.eye(128, dtype=np.float32)
    c[:, C_BLK:C_BLK + 128] = (p[:, None] // 64 == p[None, :] // 64).astype(np.float32)
    s = np.arange(32)
    m = (s[:, None] <= s[None, :]).astype(np.float32)
    c[0:32, C_M32:C_M32 + 128] = np.tile(m, (1, 4))
    c[:, C_TRI:C_TRI + 128] = (p[None, :] >= p[:, None]).astype(np.float32)
    rm = np.ones(512, np.float32)
    rm[0::32] = 0.0
    c[:, C_RM:C_RM + 512] = rm[None, :]
    for h in range(NH):
        for idx in range(32):
            c[:, C_KB + h * 32 + idx] = SLOPES[h] * (p + 128.0 * (idx - 28))
    c[:, C_US:C_US + 128] = (p[:, None] < p[None, :]).astype(np.float32)
    c[:, C_TH:C_TH + 8] = (np.arange(8) * TS).astype(np.float32)[None, :]
    c[:, C_TV:C_TV + 64] = (np.arange(64) * TS).astype(np.float32)[None, :]
    c[:, C_PI] = p.astype(np.float32)
    qi = np.arange(512)
    lo = (qi % 256).astype(np.float32)
    hi = (qi - qi % 256).astype(np.float32)
    qr = np.zeros((2, NH * 512), np.float32)
    for h in range(NH):
        qr[0, h * 512:(h + 1) * 512] = -SLOPES[h] * lo
        qr[1, h * 512:(h + 1) * 512] = -SLOPES[h] * hi
    return c, qr


def make_par(b, c, ada_b, norm_mix_g, norm_ffn_g, lb_logits, hgrn_norm_g, qn_g, kn_g, lam, subln_g):
    par = np.zeros((128, NPAR), np.float32)
    par[:, 0:8] = c[b].reshape(8, 128).T
    for l in range(2):
        o = 8 + l * PL
        par[:, o:o + 48] = ada_b[l].reshape(48, 128).T
        par[:, o + 48:o + 56] = norm_mix_g[l].reshape(8, 128).T
        par[:, o + 56:o + 64] = norm_ffn_g[l].reshape(8, 128).T
        par[:, o + 64:o + 68] = lb_logits[0].reshape(4, 128).T
        par[:, o + 68:o + 72] = lb_logits[1].reshape(4, 128).T
        par[:, o + 72] = hgrn_norm_g[l]
        par[:, o + 73] = np.tile(qn_g[l], 2)
        par[:, o + 74] = np.tile(kn_g[l], 2)
        par[:, o + 75] = subln_g[l]
        par[:, o + 76:o + 332] = lam[l].reshape(1, 256)
    return par


_NC_CACHE = {}


def kernel(x, c, ada_w, ada_b, norm_mix_g, norm_ffn_g, w_in, hgrn_lb_logits, hgrn_norm_g,
           da_qnorm_g, da_knorm_g, da_lambda, da_subln_g, w_branch_a, w_branch_b, w_out,
           ffn_w1, ffn_w3, ffn_w2, moe_router, moe_w1, moe_w3, moe_w2):
    f = lambda a: np.ascontiguousarray(np.asarray(a, dtype=np.float32))
    x = f(x)
    B, S, _ = x.shape
    cst, qr = make_consts()
    if S not in _NC_CACHE:
        _NC_CACHE[S] = build(S)
    nc = _NC_CACHE[S]
    shared = dict(cst=cst, qrows=qr, ada_w=f(ada_w), w_in=f(w_in), w_pa=f(w_branch_a), w_pb=f(w_branch_b), w_o=f(w_out),
                  ffn_w1=f(ffn_w1), ffn_w3=f(ffn_w3), ffn_w2=f(ffn_w2), router=f(moe_router),
                  moe_w1=f(moe_w1), moe_w3=f(moe_w3), moe_w2=f(moe_w2))
    args = [f(a) for a in (c, ada_b, norm_mix_g, norm_ffn_g, hgrn_lb_logits, hgrn_norm_g, da_qnorm_g, da_knorm_g, da_lambda, da_subln_g)]
    in_maps = []
    for b in range(B):
        m = dict(shared)
        m["x"] = x[b]
        m["par"] = make_par(b, *args)
        in_maps.append(m)
    res = run_bass_kernel_spmd(nc, in_maps, core_ids=list(range(B)))
    return np.stack([np.asarray(r["out"], dtype=np.float32) for r in res.results], axis=0)
```

```python
import math
import contextlib
import numpy as np
import concourse.bass as bass
import concourse.mybir as mybir
from concourse.bass_utils import run_bass_kernel_spmd

F32 = mybir.dt.float32
BF16 = mybir.dt.bfloat16
AF = mybir.ActivationFunctionType
ALU = mybir.AluOpType
AX = mybir.AxisListType

D = 1024
DIN = 5632
NH = 4
DFF = 2816
DFE = 3584
NE = 8
EPS = 1e-6
PL = 332
NPAR = 8 + 2 * PL
C_ID, C_BLK, C_M32, C_TRI, C_RM, C_KB = 0, 128, 256, 384, 512, 1024
C_US, C_TH, C_TV, C_PI = 1152, 1280, 1288, 1352
NCST = 1353
TS = 512
SLOPES = [2.0 ** (-8.0 * (i + 1) / NH) for i in range(NH)]


class MK:
    def __init__(self, nc, es, nds=48):
        self.nc = nc
        self.eng = dict(pe=nc.tensor, act=nc.scalar, dve=nc.vector, pool=nc.gpsimd, sp=nc.sync)
        self.esem = {k: es.enter_context(nc.semaphore("s_" + k)) for k in self.eng}
        self.ecnt = {k: 0 for k in self.eng}
        self.nds = nds
        self.dsem = [es.enter_context(nc.semaphore("d%d" % i)) for i in range(nds)]
        self.dcnt = [0] * nds
        self.dnext = {'sp': 0, 'pool': 0, 'act': 0}
        self.dpool = {'sp': list(range(0, nds - 16)), 'pool': list(range(nds - 16, nds)), 'act': []}
        self.seen = {k: {} for k in self.eng}
        self.lastw = {}
        self.readers = {}
        self.nwaits = 0
        self.nops = 0

    def _sem(self, sk):
        return self.esem[sk[1]] if sk[0] == 'e' else self.dsem[sk[1]]

    def _wait(self, en, ev):
        sk, val = ev
        if val <= 0:
            return
        if sk[0] == 'e' and sk[1] == en and en in ('pe', 'sp'):
            return
        if sk[0] == 'e':
            assert val <= self.ecnt[sk[1]], ("forward wait", en, ev, self.ecnt[sk[1]])
        if self.seen[en].get(sk, 0) >= val:
            return
        self.eng[en].wait_ge(self._sem(sk), val)
        self.seen[en][sk] = val
        self.nwaits += 1

    def _deps(self, en, reads, writes):
        for k in reads:
            ev = self.lastw.get(k)
            if ev is not None:
                self._wait(en, ev)
        for k in writes:
            ev = self.lastw.get(k)
            if ev is not None:
                self._wait(en, ev)
            for sk, val in self.readers.get(k, {}).items():
                self._wait(en, (sk, val))

    def _record(self, ev, reads, writes):
        sk, val = ev
        for k in writes:
            self.lastw[k] = ev
            self.readers[k] = {}
        for k in reads:
            d = self.readers.setdefault(k, {})
            if d.get(sk, 0) < val:
                d[sk] = val

    def op(self, en, fn, reads=(), writes=(), signal=True):
        self._deps(en, reads, writes)
        ins = fn(self.eng[en])
        self.nops += 1
        if signal:
            self.ecnt[en] += 1
            ins.then_inc(self.esem[en], 1)
            ev = (('e', en), self.ecnt[en])
        else:
            ev = (('e', en), self.ecnt[en] + 1)
        self._record(ev, reads, writes)
        return ins

    def dma(self, q, out, in_, reads=(), writes=(), **kw):
        pl = self.dpool[q]
        i = pl[self.dnext[q] % len(pl)]
        self.dnext[q] += 1
        self._wait(q, (('d', i), self.dcnt[i]))
        self._deps(q, reads, writes)
        ins = self.eng[q].dma_start(out=out, in_=in_, **kw)
        ins.then_inc(self.dsem[i], 16)
        self.dcnt[i] += 16
        ev = (('d', i), self.dcnt[i])
        self._record(ev, reads, writes)
        self.nops += 1
        return ins

    def dmai(self, out, out_off, in_, in_off, reads=(), writes=()):
        q = 'pool'
        pl = self.dpool[q]
        i = pl[self.dnext[q] % len(pl)]
        self.dnext[q] += 1
        self._wait(q, (('d', i), self.dcnt[i]))
        self._deps(q, reads, writes)
        ins = self.eng[q].indirect_dma_start(out, out_off, in_, in_off)
        ins.then_inc(self.dsem[i], 16)
        self.dcnt[i] += 16
        ev = (('d', i), self.dcnt[i])
        self._record(ev, reads, writes)
        self.nops += 1
        return ins

    def note_read(self, en, reads):
        self._deps(en, reads, ())

    def barrier(self):
        for en in self.eng:
            for fn in self.eng:
                if fn != en:
                    self._wait(en, (('e', fn), self.ecnt[fn]))
            for i in range(self.nds):
                self._wait(en, (('d', i), self.dcnt[i]))
        self.lastw = {}
        self.readers = {}

    def finish(self):
        for i in range(self.nds):
            self._wait('sp', (('d', i), self.dcnt[i]))
        for fn in self.eng:
            if fn != 'sp':
                self._wait('sp', (('e', fn), self.ecnt[fn]))


_UID = [0]


def _uname(name):
    _UID[0] += 1
    return "%s_u%d" % (name, _UID[0])


class Ring:
    def __init__(self, es, nc, name, shape, dt, n):
        self.t = [es.enter_context(nc.sbuf_tensor(_uname("%s%d" % (name, i)), shape, dt)) for i in range(n)]
        self.k = [(name, i) for i in range(n)]
        self.i = 0

    def get(self):
        i = self.i
        self.i = (i + 1) % len(self.t)
        return self.t[i], self.k[i]


def build(S=4096, dbg=(), nlayers=2, phases=None):
    NB = S // 512
    NT = S // 128
    NCH = S // 32
    nc = bass.Bass("TRN2", target_bir_lowering=False)

    def din(name, shape):
        return nc.dram_tensor(name, shape, F32, kind="ExternalInput").ap()

    x = din("x", [S, D])
    par = din("par", [128, NPAR])
    cst = din("cst", [128, NCST])
    qrows = din("qrows", [2, NH * 512])
    ada_w = din("ada_w", [2, D, 6 * D])
    w_in = din("w_in", [2, D, DIN])
    w_pa = din("w_pa", [2, 512, D])
    w_pb = din("w_pb", [2, 512, D])
    w_o = din("w_o", [2, D, D])
    f_w1 = din("ffn_w1", [1, D, DFF])
    f_w3 = din("ffn_w3", [1, D, DFF])
    f_w2 = din("ffn_w2", [1, DFF, D])
    router = din("router", [1, D, NE])
    m_w1 = din("moe_w1", [1, NE, D, DFE])
    m_w3 = din("moe_w3", [1, NE, D, DFE])
    m_w2 = din("moe_w2", [1, NE, DFE, D])
    out = nc.dram_tensor("out", [S, D], F32, kind="ExternalOutput").ap()

    def scr(name, shape, dt):
        kind = "ExternalOutput" if name in dbg else "Internal"
        return nc.dram_tensor(name, shape, dt, kind=kind).ap()

    xT = scr("xT", [D, S], F32)
    QH = scr("QH", [NH, 128, S], BF16)
    KD = scr("KD", [NH, 128, S], BF16)
    KDEC = scr("KDEC", [NH, 128, S], BF16)
    VHT = scr("VHT", [NH, 128, S], BF16)
    SG = scr("SG", [NH, 128, S], BF16)
    EBL = scr("EBL", [NH, 128, NCH], F32)
    QA = scr("QA", [NH, 2, 64, S], BF16)
    KA = scr("KA", [NH, 2, 64, S], BF16)
    VA = scr("VA", [S, 512], BF16)
    GA = scr("GA", [D, S], BF16)
    GB = scr("GB", [D, S], BF16)
    OHG = scr("OHG", [NH, 128, S], BF16)
    ODA = scr("ODA", [NH, 128, S], BF16)
    H2 = scr("H2", [D, S], BF16)
    TMAX = -(-(2 * S + NE * (TS - 1)) // TS)
    NSLOT = TMAX * TS
    H2TOK = scr("H2TOK", [S, D], BF16)
    HSLOT = scr("HSLOT", [NSLOT, D], BF16)
    YSLOT = scr("YSLOT", [NSLOT, D], F32)
    MFG = 4
    MNFG = DFE // (MFG * 128)
    W1C = scr("W1C", [NE * MNFG * 128, 8 * MFG * 128], BF16)
    W3C = scr("W3C", [NE * MNFG * 128, 8 * MFG * 128], BF16)
    W2C = scr("W2C", [NE * MNFG * 128, MFG * D], BF16)

    es0 = contextlib.ExitStack()
    with es0:
        M = MK(nc, es0)
        ps = [es0.enter_context(nc.psum_tensor("ps%d" % i, [128, 512], F32)) for i in range(8)]
        PK = [("ps", i) for i in range(8)]
        csem = es0.enter_context(nc.semaphore("csem"))
        cconv = [0]

        conv_list = [(e_, fg) for e_ in range(NE) for fg in range(MNFG)] if nlayers > 1 else []
        conv_pos = [0]

        def conv_step(n=1):
            FWc = MFG * 128
            for _ in range(n):
                if conv_pos[0] >= len(conv_list):
                    return
                e_, fg = conv_list[conv_pos[0]]
                conv_pos[0] += 1
                if True:
                    r0 = (e_ * MNFG + fg) * 128
                    for (dst, src) in ((W1C, m_w1), (W3C, m_w3)):
                        nc.gpsimd.dma_start(out=dst[r0:r0 + 128, :].rearrange("p (kc f) -> p kc f", kc=8),
                                            in_=src[0, e_].rearrange("(kc p) f -> p kc f", p=128)[:, :, fg * FWc:(fg + 1) * FWc]).then_inc(csem, 16)
                        cconv[0] += 16
                    nc.gpsimd.dma_start(out=W2C[r0:r0 + 128, :].rearrange("p (fc d) -> p fc d", fc=MFG),
                                        in_=m_w2[0, e_][fg * FWc:(fg + 1) * FWc, :].rearrange("(fc p) d -> p fc d", p=128)).then_inc(csem, 16)
                    cconv[0] += 16

        def sb0(name, shape, dt):
            return es0.enter_context(nc.sbuf_tensor(name, shape, dt))

        cst32 = sb0("cst32", [128, NCST], F32)
        par_sb = sb0("par_sb", [128, NPAR], F32)
        id16 = sb0("id16", [128, 128], BF16)
        blk16 = sb0("blk16", [128, 128], BF16)
        tri16 = sb0("tri16", [128, 128], BF16)
        ones16 = sb0("ones16", [128, 128], BF16)
        qrow16 = sb0("qrow16", [66, NH * 512], BF16)
        dv = sb0("dv", [128, 2, 64], F32)
        ada_sb = sb0("ada_sb", [128, 2, 48], F32)
        cs32 = sb0("cs32", [128, 8], F32)
        us16 = sb0("us16", [128, 128], BF16)
        rank_all = sb0("rank_all", [128, NT, NE], F32)
        sel_all = sb0("sel_all", [128, NT, NE], F32)
        m1_all = sb0("m1_all", [128, NT, NE], F32)
        comb_all = sb0("comb_all", [128, NT, NE], F32)
        rbase = sb0("rbase", [128, NE], F32)
        M.dma('sp', cst32[:], cst[:, :], writes=["cst"])
        M.dma('sp', par_sb[:], par[:, :], writes=["par"])
        M.dma('pool', id16[:], cst[:, C_ID:C_ID + 128], writes=["id16"])
        M.dma('pool', blk16[:], cst[:, C_BLK:C_BLK + 128], writes=["blk16"])
        M.dma('pool', tri16[:], cst[:, C_TRI:C_TRI + 128], writes=["tri16"])
        M.dma('pool', us16[:], cst[:, C_US:C_US + 128], writes=["us16"])
        M.dma('pool', qrow16[64:66, :], qrows[:, :], writes=["qrow16"])
        M.op('dve', lambda e: e.memset(ones16[:], 1.0), [], ["ones16"])
        id32 = cst32[:, C_ID:C_ID + 128]
        m32 = cst32[0:32, C_M32:C_M32 + 128]
        rmask = cst32[:, C_RM:C_RM + 512]
        kbias = cst32[:, C_KB:C_KB + 128]
        M.op('act', lambda e: e.activation(out=cs32[:], in_=par_sb[:, 0:8], func=AF.Silu), ["par"], ["cs32"])

        def pcol(l, off, n=1):
            b = 8 + l * PL + off
            return par_sb[:, b:b + n]

        DV_A1, DV_A2, DV_LB, DV_OML, DV_QSC, DV_GSUB, DV_NLAM, DV_T = 0, 8, 16, 20, 24, 25, 26, 27

        def ada_group(l, g, awr, bank):
            awv = ada_w[l].rearrange("(kc p) f -> p kc f", p=128)
            aw, awk = awr.get()
            M.dma('pool', aw[:], awv[:, :, g * 768:(g + 1) * 768], writes=[awk])
            for jj in range(6):
                j = g * 6 + jj
                for kc in range(8):
                    M.op('pe', lambda e, aw=aw, jj=jj, kc=kc, j=j: e.matmul(
                        ps[bank][:, j:j + 1], aw[:, kc, jj * 128:(jj + 1) * 128], cs32[:, kc:kc + 1],
                        start=(kc == 0), stop=(kc == 7), skip_group_check=True),
                        [awk, "cs32"], [PK[bank]], signal=(kc == 7))

        def ada_finish(l, lt, bank):
            M.op('dve', lambda e, l=l: e.tensor_tensor(out=ada_sb[:, l, :], in0=ps[bank][:, 0:48], in1=pcol(l, 0, 48), op=ALU.add),
                 [PK[bank], "par"], [("ada", l)])
            M.op('dve', lambda e, l=l: e.scalar_tensor_tensor(out=dv[:, l, DV_A1:DV_A1 + 8], in0=ada_sb[:, l, 8:16], scalar=1.0,
                                                              in1=pcol(l, 48, 8), op0=ALU.add, op1=ALU.mult), [("ada", l), "par"], [("dv", l)])
            M.op('dve', lambda e, l=l: e.scalar_tensor_tensor(out=dv[:, l, DV_A2:DV_A2 + 8], in0=ada_sb[:, l, 32:40], scalar=1.0,
                                                              in1=pcol(l, 56, 8), op0=ALU.add, op1=ALU.mult), [("ada", l), "par"], [("dv", l)])
            if l == 0:
                M.op('dve', lambda e, l=l: e.memset(dv[:, l, DV_LB:DV_LB + 4], 0.0), [], [("dv", l)])
                M.op('dve', lambda e, l=l: e.memset(dv[:, l, DV_OML:DV_OML + 4], 1.0), [], [("dv", l)])
            else:
                M.op('dve', lambda e, l=l: e.tensor_tensor(out=lt[:, 0:4], in0=pcol(l, 68, 4), in1=pcol(l, 64, 4), op=ALU.subtract), ["par"], ["lt"])
                M.op('act', lambda e, l=l: e.activation(out=dv[:, l, DV_LB:DV_LB + 4], in_=lt[:, 0:4], func=AF.Sigmoid), ["lt"], [("dv", l)])
                M.op('dve', lambda e, l=l: e.tensor_scalar(out=dv[:, l, DV_OML:DV_OML + 4], in0=dv[:, l, DV_LB:DV_LB + 4], scalar1=-1.0, scalar2=1.0,
                                                           op0=ALU.mult, op1=ALU.add), [("dv", l)], [("dv", l)])
            lam_init = 0.8 - 0.6 * math.exp(-0.3 * l)
            M.op('dve', lambda e, l=l: e.tensor_scalar(out=dv[:, l, DV_QSC:DV_QSC + 1], in0=pcol(l, 73), scalar1=0.125, scalar2=None, op0=ALU.mult), ["par"], [("dv", l)])
            M.op('dve', lambda e, l=l, li=lam_init: e.tensor_scalar(out=dv[:, l, DV_GSUB:DV_GSUB + 1], in0=pcol(l, 75), scalar1=1.0 - li, scalar2=None, op0=ALU.mult), ["par"], [("dv", l)])
            M.op('dve', lambda e, l=l: e.tensor_tensor(out=lt[:, 0:64], in0=pcol(l, 76, 64), in1=pcol(l, 140, 64), op=ALU.mult), ["par"], ["lt"])
            M.op('dve', lambda e, l=l: e.reduce_sum(out=dv[:, l, DV_T:DV_T + 1], in_=lt[:, 0:64], axis=AX.X), ["lt"], [("dv", l)])
            M.op('dve', lambda e, l=l: e.tensor_tensor(out=lt[:, 0:64], in0=pcol(l, 204, 64), in1=pcol(l, 268, 64), op=ALU.mult), ["par", ("dv", l)], ["lt"])
            M.op('dve', lambda e, l=l: e.reduce_sum(out=dv[:, l, DV_T + 1:DV_T + 2], in_=lt[:, 0:64], axis=AX.X), ["lt"], [("dv", l)])
            M.op('act', lambda e, l=l: e.activation(out=dv[:, l, DV_T:DV_T + 2], in_=dv[:, l, DV_T:DV_T + 2], func=AF.Exp), [("dv", l)], [("dv", l)])
            M.op('dve', lambda e, l=l: e.tensor_tensor(out=dv[:, l, DV_NLAM:DV_NLAM + 1], in0=dv[:, l, DV_T + 1:DV_T + 2], in1=dv[:, l, DV_T:DV_T + 1], op=ALU.subtract), [("dv", l)], [("dv", l)])
            M.op('dve', lambda e, l=l, li=lam_init: e.tensor_scalar(out=dv[:, l, DV_NLAM:DV_NLAM + 1], in0=dv[:, l, DV_NLAM:DV_NLAM + 1], scalar1=-li, scalar2=None, op0=ALU.add), [("dv", l)], [("dv", l)])

        win_state = {}

        def win_prefetch(l):
            wes = contextlib.ExitStack()
            wt = wes.enter_context(nc.sbuf_tensor(_uname("win"), [128, 8, DIN], BF16))
            wv = w_in[l].rearrange("(kc p) n -> p kc n", p=128)
            for kc in range(8):
                M.dma('pool', wt[:, kc, :], wv[:, kc, :], writes=[("win", kc)])
            win_state[l] = (wes, wt)

        if phases is None or "p1" in phases:
            win_prefetch(0)
        with contextlib.ExitStack() as es:
          if True:
            xin_r = Ring(es, nc, "xin", [128, D], F32, 2)
            xo_r = Ring(es, nc, "xo", [128, 8, 512], F32, 2)
            xTv = xT.rearrange("(kc p) s -> p kc s", p=128)
            for j in range(NB):
                xo, xok = xo_r.get()
                for tt in range(4):
                    t = j * 4 + tt
                    xin, xink = xin_r.get()
                    M.dma('sp', xin[:], x[t * 128:(t + 1) * 128, :], writes=[xink])
                    for half in range(2):
                        b = 4 + 2 * (tt % 2) + half
                        for q in range(4):
                            kc = half * 4 + q
                            M.op('pe', lambda e, b=b, q=q, kc=kc, xin=xin: e.transpose(ps[b][:, q * 128:(q + 1) * 128], xin[:, kc * 128:(kc + 1) * 128], id32),
                                 [xink, "cst"], [PK[b]], signal=(q == 3))
                        eng = 'act' if half == 0 else 'dve'
                        if eng == 'act':
                            M.op('act', lambda e, b=b, half=half, tt=tt, xo=xo: e.copy(
                                out=xo[:, half * 4:half * 4 + 4, tt * 128:(tt + 1) * 128], in_=ps[b][:].rearrange("p (q t) -> p q t", q=4)),
                                [PK[b]], [xok])
                        else:
                            M.op('dve', lambda e, b=b, half=half, tt=tt, xo=xo: e.tensor_copy(
                                out=xo[:, half * 4:half * 4 + 4, tt * 128:(tt + 1) * 128], in_=ps[b][:].rearrange("p (q t) -> p q t", q=4)),
                                [PK[b]], [xok])
                M.dma('sp', xTv[:, :, j * 512:(j + 1) * 512], xo[:], reads=[xok], writes=[("xT", j)])

          if True:
            awr = Ring(es, nc, "aw", [128, 8, 768], F32, 2)
            lt = es.enter_context(nc.sbuf_tensor(_uname("lt"), [128, 64], F32))
            for l in range(1):
                for g in range(8):
                    ada_group(l, g, awr, 0)
                ada_finish(l, lt, 0)
            M.barrier()

        def K8(k):
            return [(k, i) for i in range(8)]

        def norm_block(xs, xsk, A, B, Akey, hT, hTk, sq, sqk, r32, psb, rsr, h32=None):
            for kc in range(8):
                M.op('act', lambda e, kc=kc: e.activation(out=sq[:, kc, :], in_=xs[:, kc, :], func=AF.Square), [(xsk, kc)], [(sqk, kc)])
            for kc in range(8):
                M.op('pe', lambda e, kc=kc: e.matmul(ps[psb][:], ones16[:], sq[:, kc, :], start=(kc == 0), stop=(kc == 7)),
                     [(sqk, kc), "ones16"], [PK[psb]], signal=(kc == 7))
            rs, rsk = rsr.get()
            M.op('act', lambda e: e.activation(out=rs[:], in_=ps[psb][:], func=AF.Ln, bias=EPS, scale=1.0 / D), [PK[psb]], [rsk])
            M.op('act', lambda e: e.activation(out=rs[:], in_=rs[:], func=AF.Exp, scale=-0.5), [rsk], [rsk])
            for kc in range(8):
                t, tk = r32.get()
                M.op('dve', lambda e, kc=kc, t=t: e.scalar_tensor_tensor(out=t[:], in0=xs[:, kc, :], scalar=A[:, kc:kc + 1], in1=rs[:],
                                                                        op0=ALU.mult, op1=ALU.mult), [(xsk, kc), rsk, Akey], [tk])
                if h32 is None:
                    M.op('act', lambda e, kc=kc, t=t: e.activation(out=hT[:, kc, :], in_=t[:], func=AF.Identity, bias=B[:, kc:kc + 1], scale=1.0),
                         [tk, Akey], [(hTk, kc)])
                else:
                    M.op('act', lambda e, kc=kc, t=t: e.activation(out=h32[0][:, kc, :], in_=t[:], func=AF.Identity, bias=B[:, kc:kc + 1], scale=1.0),
                         [tk, Akey], [(h32[1], kc)])
                    M.op('pool', lambda e, kc=kc: e.tensor_copy(out=hT[:, kc, :], in_=h32[0][:, kc, :]), [(h32[1], kc)], [(hTk, kc)])

        QHv = QH.rearrange("h p s -> p h s")
        KDv = KD.rearrange("h p s -> p h s")
        KDECv = KDEC.rearrange("h p s -> p h s")
        VHTv = VHT.rearrange("h p s -> p h s")
        SGv = SG.rearrange("h p s -> p h s")
        EBLv = EBL.rearrange("h p c -> p h c")
        OHGv = OHG.rearrange("h p s -> p h s")
        ODAv = ODA.rearrange("h p s -> p h s")
        xTv = xT.rearrange("(kc p) s -> p kc s", p=128)
        GAv = GA.rearrange("(kc p) s -> p kc s", p=128)
        GBv = GB.rearrange("(kc p) s -> p kc s", p=128)
        H2v = H2.rearrange("(kc p) s -> p kc s", p=128)
        VAv = VA.rearrange("(t p) c -> p t c", p=128)

        def want(ph):
            return phases is None or ph in phases

        for l in range(nlayers):
            A1 = dv[:, l, DV_A1:DV_A1 + 8]
            B1 = ada_sb[:, l, 0:8]
            G1 = ada_sb[:, l, 16:24]
            A2 = dv[:, l, DV_A2:DV_A2 + 8]
            B2 = ada_sb[:, l, 24:32]
            G2 = ada_sb[:, l, 40:48]
            LB = dv[:, l, DV_LB:DV_LB + 4]
            OML = dv[:, l, DV_OML:DV_OML + 4]
            QSC = dv[:, l, DV_QSC:DV_QSC + 1]
            KSC = pcol(l, 74)
            GSUB = dv[:, l, DV_GSUB:DV_GSUB + 1]
            NLAM = dv[:, l, DV_NLAM:DV_NLAM + 1]
            HGN = pcol(l, 72)
            PKEY = [("dv", l), ("ada", l), "par"]

            if want("p1"):
              if l not in win_state:
                  win_prefetch(l)
              wes_, win = win_state.pop(l)
              with contextlib.ExitStack() as es:
                xs_r = Ring(es, nc, "xs", [128, 8, 512], F32, 2)
                sq = es.enter_context(nc.sbuf_tensor(_uname("sq"), [128, 8, 512], BF16))
                hT_r = Ring(es, nc, "hT", [128, 8, 512], BF16, 2)
                r32 = Ring(es, nc, "r32", [128, 512], F32, 8)
                r16 = Ring(es, nc, "r16", [128, 512], BF16, 8)
                rbl = Ring(es, nc, "rbl", [128, 16], F32, 4)
                rsr = Ring(es, nc, "rsr", [128, 512], F32, 2)
                WINK = [("win", kc) for kc in range(8)]
                pring = [2, 3, 4, 5, 6, 7]
                pri = [0]

                def nextbank():
                    b = pring[pri[0] % len(pring)]
                    pri[0] += 1
                    return b

                def p1_load(jn):
                    xs_, xsk_ = xs_r.get()
                    M.dma('sp', xs_[:], xTv[:, :, jn * 512:(jn + 1) * 512], reads=[("xT", jn)], writes=K8(xsk_))
                    return xs_, xsk_

                def p1_norm(ld):
                    hT_, hTk_ = hT_r.get()
                    norm_block(ld[0], ld[1], A1, B1, PKEY[0], hT_, hTk_, sq, "sq", r32, 0, rsr)
                    return hT_, hTk_

                ld_cur = p1_load(0)
                h_cur = p1_norm(ld_cur)
                for j in range(NB):
                    cols = slice(j * 512, (j + 1) * 512)
                    ld_nxt = p1_load(j + 1) if j + 1 < NB else None
                    if l == 1:
                        conv_step(2)
                    hT, hTk = h_cur

                    def proj_fm(oc):
                        b = nextbank()
                        for kc in range(8):
                            M.op('pe', lambda e, kc=kc, b=b, oc=oc: e.matmul(ps[b][:], win[:, kc, oc * 128:(oc + 1) * 128], hT[:, kc, :],
                                                                           start=(kc == 0), stop=(kc == 7)),
                                 [WINK[kc], (hTk, kc)], [PK[b]], signal=(kc == 7))
                        return b

                    for h in range(NH):
                        bz = proj_fm(4 + h)
                        bq = proj_fm(h)
                        sg, sgk = r32.get()
                        sn, snk = r32.get()
                        M.op('act', lambda e, sg=sg, bz=bz: e.activation(out=sg[:], in_=ps[bz][:], func=AF.Sigmoid), [PK[bz]], [sgk])
                        M.op('act', lambda e, sn=sn, bz=bz: e.activation(out=sn[:], in_=ps[bz][:], func=AF.Sigmoid, scale=-1.0), [PK[bz]], [snk])
                        M.op('dve', lambda e, sg=sg, h=h: e.tensor_scalar(out=sg[:], in0=sg[:], scalar1=OML[:, h:h + 1], scalar2=LB[:, h:h + 1],
                                                                        op0=ALU.mult, op1=ALU.add), [sgk, PKEY[0]], [sgk])
                        M.op('act', lambda e, sg=sg: e.activation(out=sg[:], in_=sg[:], func=AF.Ln), [sgk], [sgk])
                        bT, bTk = r32.get()
                        M.op('dve', lambda e, sg=sg, bT=bT: e.tensor_tensor_scan(out=bT[:], data0=rmask, data1=sg[:], initial=0.0, op0=ALU.mult, op1=ALU.add),
                             [sgk, "cst"], [bTk])
                        M.op('dve', lambda e, sn=sn, h=h: e.tensor_scalar(out=sn[:], in0=sn[:], scalar1=OML[:, h:h + 1], scalar2=None, op0=ALU.mult),
                             [snk, PKEY[0]], [snk])
                        e1, e1k = r32.get()
                        M.op('act', lambda e, e1=e1, bT=bT: e.activation(out=e1[:], in_=bT[:], func=AF.Exp, scale=-1.0), [bTk], [e1k])
                        kd, kdk = r16.get()
                        M.op('dve', lambda e, kd=kd, sn=sn, e1=e1: e.tensor_tensor(out=kd[:], in0=sn[:], in1=e1[:], op=ALU.mult), [snk, e1k], [kdk])
                        M.dma('sp', KDv[:, h, cols], kd[:], reads=[kdk], writes=[("KD", j)])
                        bT3 = bT[:].rearrange("p (c s) -> p c s", s=32)
                        M.op('dve', lambda e, e1=e1, bT3=bT3: e.tensor_tensor(out=e1[:].rearrange("p (c s) -> p c s", s=32),
                                                                               in0=bT3[:, :, 31:32].to_broadcast([128, 16, 32]), in1=bT3, op=ALU.subtract),
                             [bTk], [e1k])
                        M.op('act', lambda e, e1=e1: e.activation(out=e1[:], in_=e1[:], func=AF.Exp), [e1k], [e1k])
                        kdec, kdeck = r16.get()
                        M.op('dve', lambda e, kdec=kdec, sn=sn, e1=e1: e.tensor_tensor(out=kdec[:], in0=sn[:], in1=e1[:], op=ALU.mult), [snk, e1k], [kdeck])
                        M.dma('sp', KDECv[:, h, cols], kdec[:], reads=[kdeck], writes=[("KDEC", j)])
                        ebl, eblk = rbl.get()
                        M.op('act', lambda e, ebl=ebl, bT3=bT3: e.activation(out=ebl[:], in_=bT3[:, :, 31], func=AF.Exp), [bTk], [eblk])
                        M.dma('sp', EBLv[:, h, j * 16:(j + 1) * 16], ebl[:], reads=[eblk], writes=[("EBL", j)])
                        M.op('act', lambda e, bT=bT: e.activation(out=bT[:], in_=bT[:], func=AF.Exp), [bTk], [bTk])
                        qe, qek = r16.get()
                        M.op('dve', lambda e, qe=qe, bq=bq, bT=bT: e.tensor_tensor(out=qe[:], in0=ps[bq][:], in1=bT[:], op=ALU.mult), [PK[bq], bTk], [qek])
                        M.dma('sp', QHv[:, h, cols], qe[:], reads=[qek], writes=[("QH", j)])
                    for h in range(NH):
                        b = proj_fm(8 + h)
                        t, tk = r16.get()
                        M.op('act', lambda e, t=t, b=b: e.copy(out=t[:], in_=ps[b][:]), [PK[b]], [tk])
                        M.dma('sp', VHTv[:, h, cols], t[:], reads=[tk], writes=[("VHT", j)])
                    for h in range(NH):
                        b = proj_fm(12 + h)
                        t, tk = r16.get()
                        M.op('act', lambda e, t=t, b=b: e.activation(out=t[:], in_=ps[b][:], func=AF.Silu), [PK[b]], [tk])
                        M.dma('sp', SGv[:, h, cols], t[:], reads=[tk], writes=[("SG", j)])
                    for (base, scol, dst, dkey) in ((16, QSC, QA, "QA"), (20, KSC, KA, "KA")):
                        for h in range(NH):
                            b = proj_fm(base + h)
                            s2, s2k = r16.get()
                            M.op('act', lambda e, s2=s2, b=b: e.activation(out=s2[:], in_=ps[b][:], func=AF.Square), [PK[b]], [s2k])
                            M.op('pe', lambda e, s2=s2: e.matmul(ps[1][:], blk16[:], s2[:], start=True, stop=True), [s2k, "blk16"], [PK[1]])
                            rr, rrk = r32.get()
                            M.op('act', lambda e, rr=rr: e.activation(out=rr[:], in_=ps[1][:], func=AF.Ln, bias=EPS, scale=1.0 / 64), [PK[1]], [rrk])
                            M.op('act', lambda e, rr=rr: e.activation(out=rr[:], in_=rr[:], func=AF.Exp, scale=-0.5), [rrk], [rrk])
                            qn, qnk = r16.get()
                            M.op('dve', lambda e, qn=qn, b=b, rr=rr, scol=scol: e.scalar_tensor_tensor(out=qn[:], in0=ps[b][:], scalar=scol, in1=rr[:],
                                                                                                      op0=ALU.mult, op1=ALU.mult),
                                 [PK[b], rrk] + PKEY, [qnk])
                            M.dma('sp', dst[h].rearrange("c d s -> (c d) s")[:, cols], qn[:], reads=[qnk], writes=[(dkey, j)])
                    for tt in range(4):
                        b = nextbank()
                        for kc in range(8):
                            M.op('pe', lambda e, kc=kc, b=b, tt=tt: e.matmul(ps[b][:], hT[:, kc, tt * 128:(tt + 1) * 128], win[:, kc, 3072:3584],
                                                                           start=(kc == 0), stop=(kc == 7)),
                                 [WINK[kc], (hTk, kc)], [PK[b]], signal=(kc == 7))
                        t, tk = r16.get()
                        M.op('act', lambda e, t=t, b=b: e.copy(out=t[:], in_=ps[b][:]), [PK[b]], [tk])
                        r0 = j * 512 + tt * 128
                        M.dma('sp', VA[r0:r0 + 128, :], t[:], reads=[tk], writes=[("VA", j)])
                    if ld_nxt is not None:
                        h_cur = p1_norm(ld_nxt)
                    for (base, dstv, dkey) in ((28, GAv, "GA"), (36, GBv, "GB")):
                        for kc2 in range(8):
                            b = proj_fm(base + kc2)
                            t, tk = r16.get()
                            M.op('act', lambda e, t=t, b=b: e.activation(out=t[:], in_=ps[b][:], func=AF.Sigmoid), [PK[b]], [tk])
                            M.dma('sp', dstv[:, kc2, cols], t[:], reads=[tk], writes=[(dkey, j)])
                M.barrier()
              wes_.close()

            if want("p2a"):
              with contextlib.ExitStack() as es:
                qe_r = Ring(es, nc, "hq", [128, NH, 512], BF16, 2)
                kd_r = Ring(es, nc, "hkd", [128, NH, 512], BF16, 2)
                kc_r = Ring(es, nc, "hkc", [128, NH, 512], BF16, 2)
                vt_r = Ring(es, nc, "hvt", [128, NH, 512], BF16, 2)
                sg_r = Ring(es, nc, "hsg", [128, NH, 512], BF16, 2)
                eb_r = Ring(es, nc, "heb", [128, NH, 16], F32, 2)
                sm_r = Ring(es, nc, "hsm", [32, 128], BF16, 4)
                tk_r = Ring(es, nc, "htk", [32, 1024], BF16, 4)
                Sst = es.enter_context(nc.sbuf_tensor(_uname("Sst"), [128, NH, 128], F32))
                Sbf = [Ring(es, nc, "Sbf%d" % h, [128, 128], BF16, 2) for h in range(NH)]
                r32 = Ring(es, nc, "r32", [128, 512], F32, 4)
                r16 = Ring(es, nc, "r16", [128, 512], BF16, 4)
                M.op('dve', lambda e: e.memset(Sst[:], 0.0), [], [("S", h) for h in range(NH)])
                scur = []
                for h in range(NH):
                    s0, s0k = Sbf[h].get()
                    M.op('dve', lambda e, s0=s0: e.memset(s0[:], 0.0), [], [s0k])
                    scur.append((s0, s0k))
                psS, psE = 0, 7
                psTl = [1, 2]
                psUb = [3, 6]
                psOb = [4, 5]
                blk = {}

                def load_block(j):
                    cols = slice(j * 512, (j + 1) * 512)
                    d = {}
                    for nm, ring, src, key in (("qe", qe_r, QHv, "QH"), ("kd", kd_r, KDv, "KD"), ("kc", kc_r, KDECv, "KDEC"),
                                               ("vt", vt_r, VHTv, "VHT"), ("sg", sg_r, SGv, "SG")):
                        t, k = ring.get()
                        M.dma('sp', t[:], src[:, :, cols], reads=[(key, j)], writes=[k])
                        d[nm] = (t, k)
                    t, k = eb_r.get()
                    M.dma('sp', t[:], EBLv[:, :, j * 16:(j + 1) * 16], reads=[("EBL", j)], writes=[k])
                    d["eb"] = (t, k)
                    blk[j] = d

                def stage_a(g):
                    j, c = divmod(g, 16)
                    d = blk[j]
                    (qe, qek), (kd, kdk), (kc_, kck), (vt, vtk) = d["qe"], d["kd"], d["kc"], d["vt"]
                    cc = slice(c * 32, (c + 1) * 32)
                    for h in range(NH):
                        M.op('pe', lambda e, h=h: e.matmul(ps[psS][0:32, h * 32:(h + 1) * 32], kd[:, h, cc], qe[:, h, cc], start=True, stop=True),
                             [kdk, qek], [PK[psS]], signal=(h == NH - 1))
                    sm, smk = sm_r.get()
                    M.op('dve', lambda e: e.tensor_tensor(out=sm[:], in0=ps[psS][0:32, 0:128], in1=m32, op=ALU.mult), [PK[psS], "cst"], [smk])
                    pT = psTl[g % 2]
                    psTb = ps[pT][:].bitcast(BF16)
                    for h in range(NH):
                        M.op('pe', lambda e, h=h: e.transpose(psTb[0:32, h * 128:(h + 1) * 128], kc_[:, h, cc], id16[:]),
                             [kck, "id16"], [PK[pT]], signal=False)
                    for h in range(NH):
                        M.op('pe', lambda e, h=h: e.transpose(psTb[0:32, 512 + h * 128:512 + (h + 1) * 128], vt[:, h, cc], id16[:]),
                             [vtk, "id16"], [PK[pT]], signal=(h == NH - 1))
                    tk, tkk = tk_r.get()
                    M.op('act', lambda e: e.copy(out=tk[:, 0:512], in_=psTb[0:32, 0:512]), [PK[pT]], [(tkk, 0)])
                    M.op('dve', lambda e: e.tensor_copy(out=tk[:, 512:1024], in_=psTb[0:32, 512:1024]), [PK[pT], (tkk, 0)], [(tkk, 1)])
                    return sm, smk, tk, tkk

                def stage_b(g, a_out):
                    j, c = divmod(g, 16)
                    sm, smk, tk, tkk = a_out
                    d = blk[j]
                    (qe, qek), (eb, ebk) = d["qe"], d["eb"]
                    cc = slice(c * 32, (c + 1) * 32)
                    for h in range(NH):
                        s_bf, s_bfk = scur[h]
                        bo = psOb[h // 2]
                        oc = slice((h % 2) * 256 + (c % 8) * 32, (h % 2) * 256 + (c % 8) * 32 + 32)
                        firstw = (c % 8 == 0) and (h % 2 == 0)
                        M.op('pe', lambda e, h=h: e.matmul(ps[bo][:, oc], s_bf[:], qe[:, h, cc], start=firstw, stop=False, skip_group_check=True),
                             [s_bfk, qek], [PK[bo]], signal=False)
                        M.op('pe', lambda e, h=h: e.matmul(ps[bo][:, oc], tk[:, 512 + h * 128:512 + (h + 1) * 128], sm[:, h * 32:(h + 1) * 32],
                                                          start=False, stop=True, skip_group_check=True),
                             [(tkk, 1), smk], [PK[bo]], signal=False)
                        psU = psUb[h % 2]
                        M.op('pe', lambda e, h=h: e.matmul(ps[psU][:, 0:128], tk[:, h * 128:(h + 1) * 128], tk[:, 512 + h * 128:512 + (h + 1) * 128],
                                                          start=True, stop=True), [(tkk, 0), (tkk, 1)], [PK[psU]], signal=True)
                        M.op('dve', lambda e, h=h: e.scalar_tensor_tensor(out=Sst[:, h, :], in0=Sst[:, h, :], scalar=eb[:, h, c:c + 1],
                                                                        in1=ps[psU][:, 0:128], op0=ALU.mult, op1=ALU.add),
                             [("S", h), ebk, PK[psU]], [("S", h)])
                        s_n, s_nk = Sbf[h].get()
                        M.op('act', lambda e, h=h: e.copy(out=s_n[:], in_=Sst[:, h, :]), [("S", h)], [s_nk])
                        scur[h] = (s_n, s_nk)
                    if c % 8 == 7:
                        half = c // 8
                        (sg, sgk) = d["sg"]
                        tcols = slice(j * 512 + half * 256, j * 512 + half * 256 + 256)
                        for hp in range(2):
                            bo = psOb[hp]
                            oq, oqk = r16.get()
                            M.op('act', lambda e: e.activation(out=oq[:], in_=ps[bo][:], func=AF.Square), [PK[bo]], [oqk])
                            M.op('pe', lambda e: e.matmul(ps[psE][:], ones16[:], oq[:], start=True, stop=True), [oqk, "ones16"], [PK[psE]])
                            rr, rrk = r32.get()
                            M.op('act', lambda e: e.activation(out=rr[:], in_=ps[psE][:], func=AF.Ln, bias=EPS, scale=1.0 / 128), [PK[psE]], [rrk])
                            M.op('act', lambda e: e.activation(out=rr[:], in_=rr[:], func=AF.Exp, scale=-0.5), [rrk], [rrk])
                            t, tk2 = r32.get()
                            M.op('dve', lambda e: e.scalar_tensor_tensor(out=t[:], in0=ps[bo][:], scalar=HGN, in1=rr[:], op0=ALU.mult, op1=ALU.mult),
                                 [PK[bo], rrk, "par"], [tk2])
                            o16, o16k = r16.get()
                            M.op('dve', lambda e: e.tensor_tensor(out=o16[:].rearrange("p (h t) -> p h t", h=2), in0=t[:].rearrange("p (h t) -> p h t", h=2),
                                                                  in1=sg[:, 2 * hp:2 * hp + 2, half * 256:(half + 1) * 256], op=ALU.mult), [tk2, sgk], [o16k])
                            M.dma('sp', OHGv[:, 2 * hp:2 * hp + 2, tcols], o16[:].rearrange("p (h t) -> p h t", h=2), reads=[o16k], writes=[("OHG", j, half, hp)])

                NG = NB * 16
                load_block(0)
                a_cur = stage_a(0)
                for g in range(NG):
                    j, c = divmod(g, 16)
                    if c == 0 and j + 1 < NB:
                        load_block(j + 1)
                    a_nxt = stage_a(g + 1) if g + 1 < NG else None
                    stage_b(g, a_cur)
                    if g % 4 == 3:
                        conv_step(1)
                    a_cur = a_nxt
                M.barrier()

            if want("p2b"):
              with contextlib.ExitStack() as es:
                kp_r = Ring(es, nc, "kp", [66, 2, S], BF16, 2)
                qp_r = Ring(es, nc, "qp", [66, 2, 512], BF16, 3)
                vsb = es.enter_context(nc.sbuf_tensor(_uname("vsb"), [128, NT, 512], BF16))
                pT_r = Ring(es, nc, "pT", [128, 512], BF16, 4)
                rr_r = Ring(es, nc, "arr", [128, 512], F32, 3)
                tn_r = Ring(es, nc, "atn", [128, 512], F32, 4)
                o_r = Ring(es, nc, "ao", [128, 512], F32, 2)
                r16 = Ring(es, nc, "r16", [128, 512], BF16, 3)
                M.dma('sp', vsb[:], VAv[:, :, :], reads=[("VA", j) for j in range(NB)], writes=["vsb"])
                for t_, k_ in zip(kp_r.t, kp_r.k):
                    M.op('dve', lambda e, t_=t_: e.memset(t_[64:66, :, :], 1.0), [], [k_])
                scb = [0, 1, 2]
                sci = [0]
                pairs = [(3, 4), (5, 6)]
                defer = [None]
                for h in range(NH):
                    kp, kpk = kp_r.get()
                    M.dma('sp', kp[0:64, :, :], KA[h].rearrange("c d s -> d c s"), reads=[("KA", j) for j in range(NB)], writes=[kpk])
                    for j in range(NB):
                        cols = slice(j * 512, (j + 1) * 512)
                        qp, qpk = qp_r.get()
                        M.dma('sp', qp[0:64, :, :], QA[h].rearrange("c d s -> d c s")[:, :, cols], reads=[("QA", j)], writes=[qpk])
                        for c in range(2):
                            M.op('pool', lambda e, c=c, qp=qp, h=h: e.tensor_copy(out=qp[64:66, c, :], in_=qrow16[64:66, h * 512:(h + 1) * 512]),
                                 ["qrow16"], [qpk])
                        steps = [(c, kt) for c in range(2) for kt in range(4 * j + 4)]
                        tn = [tn_r.get(), tn_r.get()]
                        conv_step(1)

                        def emit_sc(st):
                            c, kt = st
                            m = kt - 4 * j
                            c0 = 128 * m if m > 0 else 0
                            b = scb[sci[0] % 3]
                            sci[0] += 1
                            M.op('pe', lambda e: e.matmul(ps[b][:, c0:512], kp[:, c, kt * 128:(kt + 1) * 128], qp[:, c, c0:512], start=True, stop=True),
                                 [kpk, qpk], [PK[b]])
                            return b, c0, m

                        def emit_rest(st, info):
                            c, kt = st
                            b, c0, m = info
                            bO_, bL_ = pairs[c]
                            pT, pTk = pT_r.get()
                            idx = kt - 4 * j + 28
                            M.op('act', lambda e: e.activation(out=pT[:, c0:512], in_=ps[b][:, c0:512], func=AF.Exp,
                                                               bias=kbias[:, h * 32 + idx:h * 32 + idx + 1], scale=1.0), [PK[b], "cst"], [pTk])
                            if m >= 0:
                                M.op('dve', lambda e: e.tensor_tensor(out=pT[:, c0:c0 + 128], in0=pT[:, c0:c0 + 128], in1=tri16[:], op=ALU.mult),
                                     [pTk, "tri16"], [pTk])
                            first = (kt == 0)
                            last = (kt == 4 * j + 3)
                            M.op('pe', lambda e: e.matmul(ps[bO_][:, c0:512], vsb[:, kt, h * 128:(h + 1) * 128], pT[:, c0:512], start=first, stop=last,
                                                          skip_group_check=True), ["vsb", pTk], [PK[bO_]], signal=False)
                            M.op('pe', lambda e: e.matmul(ps[bL_][:, c0:512], ones16[:], pT[:, c0:512], start=first, stop=last,
                                                          skip_group_check=True), ["ones16", pTk], [PK[bL_]], signal=True)
                            if last:
                                t_, tk_ = tn[c]
                                M.op('dve', lambda e: e.reciprocal(out=t_[:], in_=ps[bL_][:]), [PK[bL_]], [tk_])
                                M.op('dve', lambda e: e.tensor_tensor(out=t_[:], in0=ps[bO_][:], in1=t_[:], op=ALU.mult), [PK[bO_], tk_], [tk_])

                        def mk_epi(h=h, cols=cols, tn=tn, j=j):
                            def f():
                                (r0, r0k), (r1, r1k) = tn
                                o, ok_ = o_r.get()
                                M.op('dve', lambda e: e.scalar_tensor_tensor(out=o[:], in0=r1[:], scalar=NLAM, in1=r0[:], op0=ALU.mult, op1=ALU.add),
                                     [r0k, r1k] + PKEY, [ok_])
                                oq, oqk = r16.get()
                                M.op('act', lambda e: e.activation(out=oq[:], in_=o[:], func=AF.Square), [ok_], [oqk])
                                M.op('pe', lambda e: e.matmul(ps[7][:], ones16[:], oq[:], start=True, stop=True), [oqk, "ones16"], [PK[7]])
                                rr, rrk = rr_r.get()
                                M.op('act', lambda e: e.activation(out=rr[:], in_=ps[7][:], func=AF.Ln, bias=EPS, scale=1.0 / 128), [PK[7]], [rrk])
                                M.op('act', lambda e: e.activation(out=rr[:], in_=rr[:], func=AF.Exp, scale=-0.5), [rrk], [rrk])
                                o16, o16k = r16.get()
                                M.op('dve', lambda e: e.scalar_tensor_tensor(out=o16[:], in0=o[:], scalar=GSUB, in1=rr[:], op0=ALU.mult, op1=ALU.mult),
                                     [ok_, rrk] + PKEY, [o16k])
                                M.dma('sp', ODAv[:, h, cols], o16[:], reads=[o16k], writes=[("ODA", j)])
                            return f

                        LA = 2
                        infos = [emit_sc(steps[i]) for i in range(min(LA, len(steps)))]
                        for i, st in enumerate(steps):
                            if i + LA < len(steps):
                                infos.append(emit_sc(steps[i + LA]))
                            emit_rest(st, infos[i])
                            if i == 3 and defer[0] is not None:
                                defer[0]()
                                defer[0] = None
                        if defer[0] is not None:
                            defer[0]()
                        defer[0] = mk_epi()
                defer[0]()
                M.barrier()

            moe = (l % 2 == 1)
            if want("p3"):
              with contextlib.ExitStack() as es:
                wpa = es.enter_context(nc.sbuf_tensor(_uname("wpa"), [128, 4, D], BF16))
                wpb = es.enter_context(nc.sbuf_tensor(_uname("wpb"), [128, 4, D], BF16))
                wo = es.enter_context(nc.sbuf_tensor(_uname("wo"), [128, 8, D], BF16))
                M.dma('pool', wpa[:], w_pa[l].rearrange("(kc p) n -> p kc n", p=128), writes=["wpa"])
                M.dma('pool', wpb[:], w_pb[l].rearrange("(kc p) n -> p kc n", p=128), writes=["wpb"])
                M.dma('pool', wo[:], w_o[l].rearrange("(kc p) n -> p kc n", p=128), writes=["wo"])
                if moe:
                    rt32 = es.enter_context(nc.sbuf_tensor(_uname("rt32"), [128, 8, NE], F32))
                    M.dma('sp', rt32[:], router[l // 2].rearrange("(kc p) e -> p kc e", p=128), writes=["rt32"])
                    h32_r = Ring(es, nc, "h32", [128, 8, 512], F32, 1)
                    rs8 = Ring(es, nc, "rs8", [128, 8], F32, 8)
                    rs1 = Ring(es, nc, "rs1", [128, 1], F32, 8)
                    rs16 = Ring(es, nc, "rs16", [128, 8], BF16, 4)
                    zt = es.enter_context(nc.sbuf_tensor(_uname("zt"), [128, 2, D], BF16))
                    M.op('dve', lambda e: e.memset(zt[:], 0.0), [], ["zt"])
                    HSz = HSLOT.rearrange("(n p) d -> p n d", p=128)
                    for n0 in range(0, NSLOT // 128, 2):
                        M.dma('sp', HSz[:, n0:n0 + 2, :], zt[:], reads=["zt"], writes=[("HSZ", n0)])
                    htok_r = Ring(es, nc, "htok", [128, D], BF16, 2)
                    M.op('dve', lambda e: e.memset(rbase[:], 0.0), [], ["rbase"])
                fold_ada = (l == 0 and nlayers > 1)
                if fold_ada:
                    awr2 = Ring(es, nc, "aw2", [128, 8, 768], F32, 1)
                    lt2 = es.enter_context(nc.sbuf_tensor(_uname("lt2"), [128, 64], F32))
                oh_r = Ring(es, nc, "oh", [128, 4, 512], BF16, 2)
                od_r = Ring(es, nc, "od", [128, 4, 512], BF16, 2)
                ga_r = Ring(es, nc, "ga", [128, 8, 512], BF16, 2)
                gb_r = Ring(es, nc, "gb", [128, 8, 512], BF16, 2)
                xs_r = Ring(es, nc, "xs", [128, 8, 512], F32, 2)
                yT_r = Ring(es, nc, "yT", [128, 8, 512], BF16, 2)
                sq = es.enter_context(nc.sbuf_tensor(_uname("sq"), [128, 8, 512], BF16))
                hT_r = Ring(es, nc, "hT", [128, 8, 512], BF16, 2)
                r32 = Ring(es, nc, "r32", [128, 512], F32, 4)
                rsr = Ring(es, nc, "rsr", [128, 512], F32, 1)
                pb = [1, 2, 3, 4, 5, 6]
                pbi = [0]

                def nb_():
                    b = pb[pbi[0] % len(pb)]
                    pbi[0] += 1
                    return b

                def p3_load(j):
                    cols = slice(j * 512, (j + 1) * 512)
                    oh, ohk = oh_r.get()
                    od, odk = od_r.get()
                    ga, gak = ga_r.get()
                    gb, gbk = gb_r.get()
                    xs, xsk = xs_r.get()
                    M.dma('sp', oh[:], OHGv[:, :, cols], reads=[("OHG", j, hf_, hp_) for hf_ in range(2) for hp_ in range(2)], writes=[ohk])
                    M.dma('sp', od[:], ODAv[:, :, cols], reads=[("ODA", j)], writes=[odk])
                    M.dma('sp', ga[:], GAv[:, :, cols], reads=[("GA", j)], writes=[gak])
                    M.dma('sp', gb[:], GBv[:, :, cols], reads=[("GB", j)], writes=[gbk])
                    M.dma('sp', xs[:], xTv[:, :, cols], reads=[("xT", j)], writes=K8(xsk))
                    return (j, cols, oh, ohk, od, odk, ga, gak, gb, gbk, xs, xsk)
                def p3_mix(L):
                    j, cols, oh, ohk, od, odk, ga, gak, gb, gbk, xs, xsk = L
                    yT, yTk = yT_r.get()
                    for dc in range(8):
                        ba = nb_()
                        bb = nb_()
                        for kc in range(4):
                            M.op('pe', lambda e, kc=kc, dc=dc, ba=ba: e.matmul(ps[ba][:], wpa[:, kc, dc * 128:(dc + 1) * 128], oh[:, kc, :], start=(kc == 0), stop=(kc == 3)),
                                 ["wpa", ohk], [PK[ba]], signal=(kc == 3))
                        for kc in range(4):
                            M.op('pe', lambda e, kc=kc, dc=dc, bb=bb: e.matmul(ps[bb][:], wpb[:, kc, dc * 128:(dc + 1) * 128], od[:, kc, :], start=(kc == 0), stop=(kc == 3)),
                                 ["wpb", odk], [PK[bb]], signal=(kc == 3))
                        t1, t1k = r32.get()
                        t2, t2k = r32.get()
                        M.op('dve', lambda e, t1=t1, ba=ba, dc=dc: e.tensor_tensor(out=t1[:], in0=ps[ba][:], in1=ga[:, dc, :], op=ALU.mult), [PK[ba], gak], [t1k])
                        M.op('dve', lambda e, t2=t2, bb=bb, dc=dc: e.tensor_tensor(out=t2[:], in0=ps[bb][:], in1=gb[:, dc, :], op=ALU.mult), [PK[bb], gbk], [t2k])
                        M.op('pool', lambda e, t1=t1, t2=t2, dc=dc: e.tensor_tensor(out=yT[:, dc, :], in0=t1[:], in1=t2[:], op=ALU.add), [t1k, t2k], [(yTk, dc)])
                    return (yT, yTk)
                def p3_out(L, Y):
                    j, cols, oh, ohk, od, odk, ga, gak, gb, gbk, xs, xsk = L
                    yT, yTk = Y
                    for dc in range(8):
                        b = nb_()
                        for kc in range(8):
                            M.op('pe', lambda e, kc=kc, dc=dc, b=b: e.matmul(ps[b][:], wo[:, kc, dc * 128:(dc + 1) * 128], yT[:, kc, :], start=(kc == 0), stop=(kc == 7)),
                                 ["wo", (yTk, kc)], [PK[b]], signal=(kc == 7))
                        M.op('dve', lambda e, dc=dc, b=b: e.scalar_tensor_tensor(out=xs[:, dc, :], in0=ps[b][:], scalar=G1[:, dc:dc + 1], in1=xs[:, dc, :],
                                                                              op0=ALU.mult, op1=ALU.add), [PK[b], (xsk, dc)] + PKEY, [(xsk, dc)])
                    M.dma('sp', xTv[:, :, cols], xs[:], reads=K8(xsk), writes=[("xT", j)])
                def p3_norm(L):
                    j, cols, oh, ohk, od, odk, ga, gak, gb, gbk, xs, xsk = L
                    hT, hTk = hT_r.get()
                    if moe:
                        h32, h32k = h32_r.get()
                        norm_block(xs, xsk, A2, B2, PKEY[0], hT, hTk, sq, "sq", r32, 0, rsr, h32=(h32, h32k))
                    else:
                        norm_block(xs, xsk, A2, B2, PKEY[0], hT, hTk, sq, "sq", r32, 0, rsr)
                    if not moe:
                        M.dma('sp', H2v[:, :, cols], hT[:], reads=K8(hTk), writes=[("H2", j)])
                    if moe:
                        for tt in range(4):
                            t_ = j * 4 + tt
                            for kc in range(8):
                                M.op('pe', lambda e, kc=kc, tt=tt: e.matmul(ps[7][:, 0:NE], h32[:, kc, tt * 128:(tt + 1) * 128], rt32[:, kc, :],
                                                                          start=(kc == 0), stop=(kc == 7)), [(h32k, kc), "rt32"], [PK[7]], signal=(kc == 7))
                            lg, lgk = rs8.get()
                            M.op('dve', lambda e, lg=lg: e.tensor_copy(out=lg[:], in_=ps[7][:, 0:NE]), [PK[7]], [lgk])
                            m1, m1k = rs1.get()
                            M.op('dve', lambda e, lg=lg, m1=m1: e.reduce_max(out=m1[:], in_=lg[:], axis=AX.X), [lgk], [m1k])
                            RK = ("route", t_)
                            M.op('dve', lambda e, lg=lg, m1=m1: e.tensor_scalar(out=m1_all[:, t_, :], in0=lg[:], scalar1=m1[:, 0:1], scalar2=None, op0=ALU.is_equal), [lgk, m1k], [RK])
                            eq, eqk = rs8.get()
                            M.op('dve', lambda e, lg=lg, eq=eq: e.scalar_tensor_tensor(out=eq[:], in0=m1_all[:, t_, :], scalar=-1e30, in1=lg[:], op0=ALU.mult, op1=ALU.add), [lgk, RK], [eqk])
                            m2, m2k = rs1.get()
                            M.op('dve', lambda e, eq=eq, m2=m2: e.reduce_max(out=m2[:], in_=eq[:], axis=AX.X), [eqk], [m2k])
                            M.op('dve', lambda e, lg=lg, m2=m2: e.tensor_scalar(out=sel_all[:, t_, :], in0=lg[:], scalar1=m2[:, 0:1], scalar2=None, op0=ALU.is_ge), [lgk, m2k], [RK])
                            M.op('dve', lambda e, m1=m1: e.tensor_scalar(out=m1[:], in0=m1[:], scalar1=-1.0, scalar2=None, op0=ALU.mult), [m1k], [m1k])
                            ex, exk = rs8.get()
                            M.op('act', lambda e, lg=lg, m1=m1, ex=ex: e.activation(out=ex[:], in_=lg[:], func=AF.Exp, bias=m1[:, 0:1], scale=1.0), [lgk, m1k], [exk])
                            M.op('dve', lambda e, ex=ex: e.tensor_tensor(out=ex[:], in0=ex[:], in1=sel_all[:, t_, :], op=ALU.mult), [exk, RK], [exk])
                            M.op('dve', lambda e, ex=ex, m2=m2: e.reduce_sum(out=m2[:], in_=ex[:], axis=AX.X), [exk, m2k], [m2k])
                            M.op('dve', lambda e, m2=m2: e.reciprocal(out=m2[:], in_=m2[:]), [m2k], [m2k])
                            M.op('dve', lambda e, ex=ex, m2=m2: e.tensor_scalar(out=comb_all[:, t_, :], in0=ex[:], scalar1=m2[:, 0:1], scalar2=None, op0=ALU.mult), [exk, m2k], [RK])
                            s16, s16k = rs16.get()
                            M.op('dve', lambda e, s16=s16: e.tensor_copy(out=s16[:], in_=sel_all[:, t_, :]), [RK], [s16k])
                            M.op('pe', lambda e, s16=s16: e.matmul(ps[7][:, 16:16 + NE], us16[:], s16[:], start=True, stop=True), [s16k, "us16"], [PK[7]], signal=False)
                            M.op('pe', lambda e, s16=s16: e.matmul(ps[7][:, 32:32 + NE], ones16[:], s16[:], start=True, stop=True), [s16k, "ones16"], [PK[7]])
                            M.op('dve', lambda e: e.tensor_tensor(out=rank_all[:, t_, :], in0=ps[7][:, 16:16 + NE], in1=rbase[:], op=ALU.add), [PK[7], "rbase"], [RK])
                            M.op('dve', lambda e: e.tensor_tensor(out=rbase[:], in0=ps[7][:, 32:32 + NE], in1=rbase[:], op=ALU.add), [PK[7], "rbase"], ["rbase"])
                            ht, htk = htok_r.get()
                            psb16 = ps[7][:].bitcast(BF16)
                            for kc in range(8):
                                M.op('pe', lambda e, kc=kc, tt=tt: e.transpose(psb16[:, kc * 128:(kc + 1) * 128], hT[:, kc, tt * 128:(tt + 1) * 128], id16[:]),
                                     [(hTk, kc), "id16"], [PK[7]], signal=(kc == 7))
                            M.op('act', lambda e, ht=ht: e.copy(out=ht[:], in_=psb16[:, 0:1024]), [PK[7]], [htk])
                            M.dma('sp', H2TOK[t_ * 128:(t_ + 1) * 128, :], ht[:], reads=[htk], writes=[("H2TOK", t_)])
                L_cur = p3_load(0)
                Y_cur = p3_mix(L_cur)
                for j in range(NB):
                    if l == 0:
                        conv_step(2)
                    if fold_ada:
                        gl = ([j] if j < 8 else []) if NB >= 8 else [g_ for g_ in range(8) if g_ % NB == j]
                        for g_ in gl:
                            ada_group(1, g_, awr2, 7)
                        if j == NB - 1:
                            ada_finish(1, lt2, 7)
                    L_nxt = p3_load(j + 1) if j + 1 < NB else None
                    p3_out(L_cur, Y_cur)
                    Y_nxt = p3_mix(L_nxt) if L_nxt is not None else None
                    p3_norm(L_cur)
                    L_cur, Y_cur = L_nxt, Y_nxt
                M.barrier()

            if want("p4") and not moe:
              last = (l == nlayers - 1)
              if not last and want("p1"):
                  win_prefetch(l + 1)
              with contextlib.ExitStack() as es:
                TBF = min(S, 1024)
                NSB = TBF // 512
                if moe:
                    nexp, dff, FG = NE, DFE, 4
                    W1 = lambda e_: m_w1[l // 2, e_]
                    W3 = lambda e_: m_w3[l // 2, e_]
                    W2 = lambda e_: m_w2[l // 2, e_]
                else:
                    nexp, dff, FG = 1, DFF, 2
                    W1 = lambda e_: f_w1[l // 2]
                    W3 = lambda e_: f_w3[l // 2]
                    W2 = lambda e_: f_w2[l // 2]
                NFG = dff // (FG * 128)
                FW = FG * 128
                h2 = es.enter_context(nc.sbuf_tensor(_uname("h2"), [128, 8, TBF], BF16))
                yacc = es.enter_context(nc.sbuf_tensor(_uname("yacc"), [128, 8, TBF], F32))
                w1_r = Ring(es, nc, "w1g", [128, 8, FW], BF16, 2)
                w3_r = Ring(es, nc, "w3g", [128, 8, FW], BF16, 2)
                w2_r = Ring(es, nc, "w2g", [128, FG, D], BF16, 2)
                aT_r = Ring(es, nc, "aT", [128, FG, 512], BF16, 2)
                s_r = Ring(es, nc, "sil", [128, 512], F32, 2)
                if moe:
                    t_r = Ring(es, nc, "tt", [128, 512], F32, 3)
                    cb_r = Ring(es, nc, "cb", [128, TBF], F32, 2)
                if last:
                    xs_r = Ring(es, nc, "xs", [128, 8, 512], F32, 1)
                    ot_r = Ring(es, nc, "ot", [128, D], F32, 2)
                else:
                    xc_r = Ring(es, nc, "xc", [128, 512], F32, 3)
                ub = [0, 1, 2, 3]
                ubi = [0]
                yb = [4, 5, 6]
                ybi = [0]
                for p in range(S // TBF):
                    tc0 = p * TBF
                    M.dma('sp', h2[:], H2v[:, :, tc0:tc0 + TBF], reads=[("H2", jj) for jj in range(NB)], writes=["h2"])
                    yfirst = [True] * (NSB * 8)
                    groups = [(e_, fg) for e_ in range(nexp) for fg in range(NFG)]
                    pend = None
                    cb = cbk = None
                    for gi, (e_, fg) in enumerate(groups):
                        if gi % 2 == 0:
                            conv_step(1)
                        if moe and fg == 0:
                            cb, cbk = cb_r.get()
                            M.dma('sp', cb[:], COMBT[e_:e_ + 1, tc0:tc0 + TBF].partition_broadcast(128), reads=[("COMBT", jj) for jj in range(NB)], writes=[cbk])
                        w1g, w1k = w1_r.get()
                        w3g, w3k = w3_r.get()
                        w2g, w2k = w2_r.get()
                        fcs = slice(fg * FW, (fg + 1) * FW)
                        M.dma('pool', w1g[:], W1(e_).rearrange("(kc p) f -> p kc f", p=128)[:, :, fcs], writes=[w1k])
                        M.dma('pool', w3g[:], W3(e_).rearrange("(kc p) f -> p kc f", p=128)[:, :, fcs], writes=[w3k])
                        M.dma('pool', w2g[:], W2(e_)[fg * FW:(fg + 1) * FW, :].rearrange("(fc p) d -> p fc d", p=128), writes=[w2k])
                        for sbi in range(NSB):
                            sc_ = slice(sbi * 512, (sbi + 1) * 512)
                            aT, aTk = aT_r.get()
                            for fc in range(FG):
                                b1 = ub[ubi[0] % 4]
                                b3 = ub[(ubi[0] + 1) % 4]
                                ubi[0] += 2
                                for kc in range(8):
                                    M.op('pe', lambda e, kc=kc, fc=fc, b1=b1, w1g=w1g: e.matmul(ps[b1][:], w1g[:, kc, fc * 128:(fc + 1) * 128], h2[:, kc, sc_],
                                                                                           start=(kc == 0), stop=(kc == 7)), [w1k, "h2"], [PK[b1]], signal=(kc == 7))
                                for kc in range(8):
                                    M.op('pe', lambda e, kc=kc, fc=fc, b3=b3, w3g=w3g: e.matmul(ps[b3][:], w3g[:, kc, fc * 128:(fc + 1) * 128], h2[:, kc, sc_],
                                                                                           start=(kc == 0), stop=(kc == 7)), [w3k, "h2"], [PK[b3]], signal=(kc == 7))
                                s, sk = s_r.get()
                                M.op('act', lambda e, s=s, b1=b1: e.activation(out=s[:], in_=ps[b1][:], func=AF.Silu), [PK[b1]], [sk])
                                if moe:
                                    t, tk = t_r.get()
                                    M.op('dve', lambda e, s=s, b3=b3, t=t: e.tensor_tensor(out=t[:], in0=ps[b3][:], in1=s[:], op=ALU.mult), [PK[b3], sk], [tk])
                                    M.op('pool', lambda e, t=t, fc=fc, aT=aT, cb=cb: e.tensor_tensor(out=aT[:, fc, :], in0=t[:], in1=cb[:, sc_], op=ALU.mult), [tk, cbk], [aTk])
                                else:
                                    M.op('dve', lambda e, s=s, b3=b3, fc=fc, aT=aT: e.tensor_tensor(out=aT[:, fc, :], in0=ps[b3][:], in1=s[:], op=ALU.mult), [PK[b3], sk], [aTk])
                            if pend is not None:
                                pend()

                            def mk_pend(aT=aT, aTk=aTk, w2g=w2g, w2k=w2k, sbi=sbi, sc_=sc_):
                                def f():
                                    for dc in range(8):
                                        b = yb[ybi[0] % 3]
                                        ybi[0] += 1
                                        for fc in range(FG):
                                            M.op('pe', lambda e, fc=fc, dc=dc, b=b: e.matmul(ps[b][:], w2g[:, fc, dc * 128:(dc + 1) * 128], aT[:, fc, :],
                                                                                           start=(fc == 0), stop=(fc == FG - 1)), [w2k, aTk], [PK[b]], signal=(fc == FG - 1))
                                        yk = ("yacc", sbi, dc)
                                        if yfirst[sbi * 8 + dc]:
                                            yfirst[sbi * 8 + dc] = False
                                            M.op('dve', lambda e, dc=dc, b=b: e.tensor_copy(out=yacc[:, dc, sc_], in_=ps[b][:]), [PK[b]], [yk])
                                        else:
                                            M.op('dve', lambda e, dc=dc, b=b: e.tensor_tensor(out=yacc[:, dc, sc_], in0=ps[b][:], in1=yacc[:, dc, sc_], op=ALU.add),
                                                 [PK[b], yk], [yk])
                                return f
                            pend = mk_pend()
                    pend()
                    for sbi in range(NSB):
                        sc_ = slice(sbi * 512, (sbi + 1) * 512)
                        jb = (tc0 // 512) + sbi
                        gcols = slice(tc0 + sbi * 512, tc0 + (sbi + 1) * 512)
                        if not last:
                            for dc in range(8):
                                xc, xck = xc_r.get()
                                M.dma('sp', xc[:], xTv[:, dc, gcols], reads=[("xT", jb)], writes=[xck])
                                M.op('dve', lambda e, dc=dc, xc=xc: e.scalar_tensor_tensor(out=yacc[:, dc, sc_], in0=yacc[:, dc, sc_], scalar=G2[:, dc:dc + 1], in1=xc[:],
                                                                                        op0=ALU.mult, op1=ALU.add), [("yacc", sbi, dc), xck] + PKEY, [("yacc", sbi, dc)])
                            M.dma('sp', xTv[:, :, gcols], yacc[:, :, sc_], reads=[("yacc", sbi, dc) for dc in range(8)], writes=[("xT", jb)])
                            continue
                        xs, xsk = xs_r.get()
                        M.dma('sp', xs[:], xTv[:, :, gcols], reads=[("xT", jb)], writes=K8(xsk))
                        for dc in range(8):
                            M.op('dve', lambda e, dc=dc: e.scalar_tensor_tensor(out=xs[:, dc, :], in0=yacc[:, dc, sc_], scalar=G2[:, dc:dc + 1], in1=xs[:, dc, :],
                                                                             op0=ALU.mult, op1=ALU.add), [("yacc", sbi, dc), (xsk, dc)] + PKEY, [(xsk, dc)])
                        if True:
                            for tt in range(4):
                                ot, otk = ot_r.get()
                                for half in range(2):
                                    b = ub[ubi[0] % 4]
                                    ubi[0] += 1
                                    for q in range(4):
                                        dc = half * 4 + q
                                        M.op('pe', lambda e, b=b, q=q, dc=dc, tt=tt: e.transpose(ps[b][:, q * 128:(q + 1) * 128], xs[:, dc, tt * 128:(tt + 1) * 128], id32),
                                             [(xsk, dc), "cst"], [PK[b]], signal=(q == 3))
                                    if half == 0:
                                        M.op('act', lambda e, b=b, ot=ot: e.copy(out=ot[:, 0:512], in_=ps[b][:]), [PK[b]], [otk])
                                    else:
                                        M.op('dve', lambda e, b=b, ot=ot: e.tensor_copy(out=ot[:, 512:1024], in_=ps[b][:]), [PK[b]], [otk])
                                r0 = tc0 + sbi * 512 + tt * 128
                                M.dma('sp', out[r0:r0 + 128, :], ot[:], reads=[otk], writes=[("out", r0)])
                M.barrier()
            if want("p4") and moe:
              assert l == nlayers - 1
              with contextlib.ExitStack() as es:
                nexp, dff, FG = NE, DFE, 4
                NFG = dff // (FG * 128)
                FW = FG * 128
                NSUB = TS // 128
                ei = l // 2
                conv_step(10 ** 6)
                assert FG == MFG and cconv[0] == NE * MNFG * 3 * 16, cconv[0]
                for en_ in ('pool',):
                    nc.gpsimd.wait_ge(csem, cconv[0])
                sm8 = Ring(es, nc, "sm8", [128, NE], F32, 6)
                big = Ring(es, nc, "big", [128, NT, NE], F32, 3)
                t88 = es.enter_context(nc.sbuf_tensor(_uname("t88"), [128, NE, 8], F32))
                tx = es.enter_context(nc.sbuf_tensor(_uname("tx"), [128, TMAX, NE], F32))
                slot1f = es.enter_context(nc.sbuf_tensor(_uname("slot1f"), [128, NT], F32))
                slot2f = es.enter_context(nc.sbuf_tensor(_uname("slot2f"), [128, NT], F32))
                slot1i = es.enter_context(nc.sbuf_tensor(_uname("slot1i"), [128, NT], mybir.dt.int32))
                slot2i = es.enter_context(nc.sbuf_tensor(_uname("slot2i"), [128, NT], mybir.dt.int32))
                c1 = es.enter_context(nc.sbuf_tensor(_uname("c1"), [128, NT], F32))
                c2 = es.enter_context(nc.sbuf_tensor(_uname("c2"), [128, NT], F32))
                texf = es.enter_context(nc.sbuf_tensor(_uname("texf"), [128, TMAX], F32))
                widf = es.enter_context(nc.sbuf_tensor(_uname("widf"), [128, TMAX, NFG], F32))
                widi = es.enter_context(nc.sbuf_tensor(_uname("widi"), [128, TMAX, NFG], mybir.dt.int32))
                th = cst32[:, C_TH:C_TH + 8]
                tv = cst32[:, C_TV:C_TV + TMAX]
                M.op('dve', lambda e: e.tensor_tensor(out=t88[:], in0=rbase[:].unsqueeze(2).to_broadcast([128, NE, 8]),
                                                      in1=th.unsqueeze(1).to_broadcast([128, NE, 8]), op=ALU.is_gt), ["rbase", "cst"], ["t88"])
                pe_, pek = sm8.get()
                M.op('dve', lambda e: e.tensor_reduce(out=pe_[:], in_=t88[:], axis=AX.X, op=ALU.add), ["t88"], [pek])
                M.op('dve', lambda e: e.tensor_scalar(out=pe_[:], in0=pe_[:], scalar1=float(TS), scalar2=None, op0=ALU.mult), [pek], [pek])
                on8, on8k = sm8.get()
                M.op('dve', lambda e: e.memset(on8[:], 1.0), [], [on8k])
                incl, inclk = sm8.get()
                M.op('dve', lambda e: e.tensor_tensor_scan(out=incl[:], data0=on8[:], data1=pe_[:], initial=0.0, op0=ALU.mult, op1=ALU.add), [on8k, pek], [inclk])
                off, offk = sm8.get()
                M.op('dve', lambda e: e.tensor_tensor(out=off[:], in0=incl[:], in1=pe_[:], op=ALU.subtract), [inclk, pek], [offk])
                RKS = [("route", t_) for t_ in range(NT)]
                slot, slotk = big.get()
                M.op('dve', lambda e: e.tensor_tensor(out=slot[:], in0=rank_all[:], in1=off[:].unsqueeze(1).to_broadcast([128, NT, NE]), op=ALU.add), RKS + [offk], [slotk])
                m2a, m2ak = big.get()
                M.op('dve', lambda e: e.tensor_tensor(out=m2a[:], in0=sel_all[:], in1=m1_all[:], op=ALU.subtract), RKS, [m2ak])
                tmp, tmpk = big.get()
                for (dst, dk, a_, ak, b_, bk) in ((slot1f, "slot1f", slot, slotk, m1_all, None), (slot2f, "slot2f", slot, slotk, m2a, m2ak),
                                                  (c1, "c1", comb_all, None, m1_all, None), (c2, "c2", comb_all, None, m2a, m2ak)):
                    rk = [k for k in (ak, bk) if k is not None]
                    M.op('dve', lambda e, a_=a_, b_=b_: e.tensor_tensor(out=tmp[:], in0=a_[:], in1=b_[:], op=ALU.mult), rk, [tmpk])
                    M.op('dve', lambda e, dst=dst: e.tensor_reduce(out=dst[:], in_=tmp[:], axis=AX.X, op=ALU.add), [tmpk], [dk])
                M.op('dve', lambda e: e.tensor_copy(out=slot1i[:], in_=slot1f[:]), ["slot1f"], ["slot1i"])
                M.op('dve', lambda e: e.tensor_copy(out=slot2i[:], in_=slot2f[:]), ["slot2f"], ["slot2i"])
                M.op('dve', lambda e: e.tensor_tensor(out=tx[:], in0=incl[:].unsqueeze(1).to_broadcast([128, TMAX, NE]),
                                                      in1=tv.unsqueeze(2).to_broadcast([128, TMAX, NE]), op=ALU.is_le), [inclk, "cst"], ["tx"])
                M.op('dve', lambda e: e.tensor_reduce(out=texf[:], in_=tx[:], axis=AX.X, op=ALU.add), ["tx"], ["texf"])
                M.op('dve', lambda e: e.tensor_scalar(out=texf[:], in0=texf[:], scalar1=float(NE - 1), scalar2=None, op0=ALU.min), ["texf"], ["texf"])
                M.op('dve', lambda e: e.tensor_scalar(out=texf[:], in0=texf[:], scalar1=float(NFG * 128), scalar2=cst32[:, C_PI:C_PI + 1], op0=ALU.mult, op1=ALU.add),
                     ["texf", "cst"], ["texf"])
                for fg in range(NFG):
                    M.op('dve', lambda e, fg=fg: e.tensor_scalar(out=widf[:, :, fg], in0=texf[:], scalar1=float(fg * 128), scalar2=None, op0=ALU.add), ["texf"], ["widf"])
                M.op('dve', lambda e: e.tensor_copy(out=widi[:], in_=widf[:]), ["widf"], ["widi"])
                with contextlib.ExitStack() as es2:
                  htk_r = Ring(es2, nc, "htk2", [128, D], BF16, 3)
                  HSZK = []
                  for t_ in range(NT):
                      ht, htk = htk_r.get()
                      M.dma('sp', ht[:], H2TOK[t_ * 128:(t_ + 1) * 128, :], writes=[htk])
                      for (si, sk) in ((slot1i, "slot1i"), (slot2i, "slot2i")):
                          M.dmai(HSLOT[:, :], bass.IndirectOffsetOnAxis(si[:, t_:t_ + 1], 0), ht[:], None, reads=[htk, sk] + HSZK, writes=[("HSLOT", t_, sk)])
                  M.barrier()
                with contextlib.ExitStack() as es2:
                  hs_r = Ring(es2, nc, "hs", [128, NSUB, D], BF16, 2)
                  h2_r = Ring(es2, nc, "h2s", [128, 8, TS], BF16, 2)
                  ya_r = Ring(es2, nc, "yacs", [128, 8, TS], F32, 2)
                  w1_r = Ring(es2, nc, "w1g", [128, 8, FW], BF16, 2)
                  w3_r = Ring(es2, nc, "w3g", [128, 8, FW], BF16, 2)
                  w2_r = Ring(es2, nc, "w2g", [128, FG, D], BF16, 2)
                  aT_r = Ring(es2, nc, "aT", [128, FG, 512], BF16, 2)
                  s_r = Ring(es2, nc, "sil", [128, 512], F32, 3)
                  ys_r = Ring(es2, nc, "ysb", [128, D], F32, 2)
                  ub = [0, 1, 2, 3]
                  ubi = [0]
                  yb = [4, 5]
                  ybi = [0]
                  HSv = HSLOT.rearrange("(i u p) d -> i p u d", p=128, u=NSUB)
                  tb = [6, 7]
                  tbi = [0]

                  def load_hs(i):
                      hs, hsk = hs_r.get()
                      M.dma('sp', hs[:], HSv[i], writes=[hsk])
                      return hs, hsk

                  nxt_hs = load_hs(0)
                  pend = None
                  fin = None
                  for i in range(TMAX):
                      hs, hsk = nxt_hs
                      if i + 1 < TMAX:
                          nxt_hs = load_hs(i + 1)
                      h2, h2k = h2_r.get()
                      for u in range(NSUB):
                          bt = tb[tbi[0] % 2]
                          tbi[0] += 1
                          psTb_ = ps[bt][:].bitcast(BF16)
                          for kc in range(8):
                              M.op('pe', lambda e, u=u, kc=kc: e.transpose(psTb_[:, kc * 128:(kc + 1) * 128], hs[:, u, kc * 128:(kc + 1) * 128], id16[:]),
                                   [hsk, "id16"], [PK[bt]], signal=(kc == 7))
                          M.op('act', lambda e, u=u: e.copy(out=h2[:, :, u * 128:(u + 1) * 128], in_=psTb_[:, 0:1024].rearrange("p (k t) -> p k t", k=8)),
                               [PK[bt]], [(h2k, u)])
                      H2K = [(h2k, u) for u in range(NSUB)]
                      yacc, yak = ya_r.get()
                      yfirst = [True] * 8
                      for fg in range(NFG):
                          w1g, w1k = w1_r.get()
                          w3g, w3k = w3_r.get()
                          w2g, w2k = w2_r.get()
                          fcs = slice(fg * FW, (fg + 1) * FW)
                          ioff = bass.IndirectOffsetOnAxis(widi[:, i, fg:fg + 1], 0)
                          M.dmai(w1g[:].rearrange("p a b -> p (a b)"), None, W1C[:, :], ioff, reads=["widi"], writes=[w1k])
                          M.dmai(w3g[:].rearrange("p a b -> p (a b)"), None, W3C[:, :], ioff, reads=["widi"], writes=[w3k])
                          M.dmai(w2g[:].rearrange("p a b -> p (a b)"), None, W2C[:, :], ioff, reads=["widi"], writes=[w2k])
                          for sbi in range(TS // 512):
                              sc_ = slice(sbi * 512, (sbi + 1) * 512)
                              aT, aTk = aT_r.get()
                              for fc in range(FG):
                                  b1 = ub[ubi[0] % 4]
                                  b3 = ub[(ubi[0] + 1) % 4]
                                  ubi[0] += 2
                                  for kc in range(8):
                                      M.op('pe', lambda e, kc=kc, fc=fc, b1=b1: e.matmul(ps[b1][:], w1g[:, kc, fc * 128:(fc + 1) * 128], h2[:, kc, sc_],
                                                                                       start=(kc == 0), stop=(kc == 7)), [w1k] + H2K, [PK[b1]], signal=(kc == 7))
                                  for kc in range(8):
                                      M.op('pe', lambda e, kc=kc, fc=fc, b3=b3: e.matmul(ps[b3][:], w3g[:, kc, fc * 128:(fc + 1) * 128], h2[:, kc, sc_],
                                                                                       start=(kc == 0), stop=(kc == 7)), [w3k] + H2K, [PK[b3]], signal=(kc == 7))
                                  s_, sk_ = s_r.get()
                                  M.op('act', lambda e, s_=s_, b1=b1: e.activation(out=s_[:], in_=ps[b1][:], func=AF.Silu), [PK[b1]], [sk_])
                                  M.op('dve', lambda e, s_=s_, b3=b3, fc=fc: e.tensor_tensor(out=aT[:, fc, :], in0=ps[b3][:], in1=s_[:], op=ALU.mult), [PK[b3], sk_], [(aTk, fc)])
                              if pend is not None:
                                  pend()
                              if fin is not None:
                                  fin()
                                  fin = None

                              def mk_pend(aT=aT, aTk=aTk, w2g=w2g, w2k=w2k, sc_=sc_, yacc=yacc, yak=yak, yfirst=yfirst):
                                  def f():
                                      for dc in range(8):
                                          b = yb[ybi[0] % 2]
                                          ybi[0] += 1
                                          for fc in range(FG):
                                              M.op('pe', lambda e, fc=fc, dc=dc, b=b: e.matmul(ps[b][:], w2g[:, fc, dc * 128:(dc + 1) * 128], aT[:, fc, :],
                                                                                             start=(fc == 0), stop=(fc == FG - 1)), [w2k, (aTk, fc)], [PK[b]], signal=(fc == FG - 1))
                                          yk = (yak, dc)
                                          if yfirst[dc]:
                                              yfirst[dc] = False
                                              M.op('dve', lambda e, dc=dc, b=b: e.tensor_copy(out=yacc[:, dc, sc_], in_=ps[b][:]), [PK[b]], [yk])
                                          else:
                                              M.op('dve', lambda e, dc=dc, b=b: e.tensor_tensor(out=yacc[:, dc, sc_], in0=ps[b][:], in1=yacc[:, dc, sc_], op=ALU.add),
                                                   [PK[b], yk], [yk])
                                  return f
                              pend = mk_pend()

                      def mk_fin(i=i, yacc=yacc, yak=yak):
                          def f():
                              for dc in range(8):
                                  M.op('act', lambda e, dc=dc: e.activation(out=yacc[:, dc, :], in_=yacc[:, dc, :], func=AF.Copy, scale=G2[:, dc:dc + 1]),
                                       [(yak, dc)] + PKEY, [(yak, dc)])
                              for u in range(NSUB):
                                  ysb, ysk = ys_r.get()
                                  for half in range(2):
                                      bt = tb[tbi[0] % 2]
                                      tbi[0] += 1
                                      for q in range(4):
                                          dc = half * 4 + q
                                          M.op('pe', lambda e, q=q, dc=dc, u=u: e.transpose(ps[bt][:, q * 128:(q + 1) * 128], yacc[:, dc, u * 128:(u + 1) * 128], id32),
                                               [(yak, dc), "cst"], [PK[bt]], signal=(q == 3))
                                      M.op('act', lambda e, half=half, ysb=ysb: e.copy(out=ysb[:, half * 512:(half + 1) * 512], in_=ps[bt][:]), [PK[bt]], [ysk])
                                  r0 = i * TS + u * 128
                                  M.dma('sp', YSLOT[r0:r0 + 128, :], ysb[:], reads=[ysk], writes=[("YSLOT", i, u)])
                          return f
                      fin = mk_fin()
                  pend()
                  fin()
                  M.barrier()
                with contextlib.ExitStack() as es2:
                  xs_r = Ring(es2, nc, "xs", [128, 8, 512], F32, 2)
                  y1_r = Ring(es2, nc, "y1", [128, D], F32, 4)
                  y2_r = Ring(es2, nc, "y2", [128, D], F32, 4)
                  ot_r = Ring(es2, nc, "ot", [128, D], F32, 3)
                  for j in range(NB):
                      xs, xsk = xs_r.get()
                      M.dma('sp', xs[:], xTv[:, :, j * 512:(j + 1) * 512], writes=[xsk])
                      for tt in range(4):
                          t_ = j * 4 + tt
                          y1, y1k = y1_r.get()
                          y2, y2k = y2_r.get()
                          M.dmai(y1[:], None, YSLOT[:, :], bass.IndirectOffsetOnAxis(slot1i[:, t_:t_ + 1], 0), reads=["slot1i"], writes=[y1k])
                          M.dmai(y2[:], None, YSLOT[:, :], bass.IndirectOffsetOnAxis(slot2i[:, t_:t_ + 1], 0), reads=["slot2i"], writes=[y2k])
                          ot, otk = ot_r.get()
                          for half in range(2):
                              b = ub[ubi[0] % 4]
                              ubi[0] += 1
                              for q in range(4):
                                  dc = half * 4 + q
                                  M.op('pe', lambda e, b=b, q=q, dc=dc, tt=tt: e.transpose(ps[b][:, q * 128:(q + 1) * 128], xs[:, dc, tt * 128:(tt + 1) * 128], id32),
                                       [xsk, "cst"], [PK[b]], signal=(q == 3))
                              hc = slice(half * 512, (half + 1) * 512)
                              M.op('dve', lambda e, b=b, hc=hc: e.scalar_tensor_tensor(out=ot[:, hc], in0=y1[:, hc], scalar=c1[:, t_:t_ + 1], in1=ps[b][:], op0=ALU.mult, op1=ALU.add),
                                   [y1k, "c1", PK[b]], [(otk, half)])
                              M.op('dve', lambda e, hc=hc: e.scalar_tensor_tensor(out=ot[:, hc], in0=y2[:, hc], scalar=c2[:, t_:t_ + 1], in1=ot[:, hc], op0=ALU.mult, op1=ALU.add),
                                   [y2k, "c2", (otk, half)], [(otk, half)])
                          M.dma('sp', out[t_ * 128:(t_ + 1) * 128, :], ot[:], reads=[(otk, 0), (otk, 1)], writes=[("out", t_)])
                  M.barrier()
        M.finish()
    build.stats = (M.nops, M.nwaits)
    return nc


def make_consts():
    c = np.zeros((128, NCST), np.float32)
    p = np.arange(128)
    c[:, C_ID:C_ID + 128] = np.eye(128, dtype=np.float32)
    c[:, C_BLK:C_BLK + 128] = (p[:, None] // 64 == p[None, :] // 64).astype(np.float32)
    s = np.arange(32)
    m = (s[:, None] <= s[None, :]).astype(np.float32)
    c[0:32, C_M32:C_M32 + 128] = np.tile(m, (1, 4))
    c[:, C_TRI:C_TRI + 128] = (p[None, :] >= p[:, None]).astype(np.float32)
    rm = np.ones(512, np.float32)
    rm[0::32] = 0.0
    c[:, C_RM:C_RM + 512] = rm[None, :]
    for h in range(NH):
        for idx in range(32):
            c[:, C_KB + h * 32 + idx] = SLOPES[h] * (p + 128.0 * (idx - 28))
    c[:, C_US:C_US + 128] = (p[:, None] < p[None, :]).astype(np.float32)
    c[:, C_TH:C_TH + 8] = (np.arange(8) * TS).astype(np.float32)[None, :]
    c[:, C_TV:C_TV + 64] = (np.arange(64) * TS).astype(np.float32)[None, :]
    c[:, C_PI] = p.astype(np.float32)
    qi = np.arange(512)
    lo = (qi % 256).astype(np.float32)
    hi = (qi - qi % 256).astype(np.float32)
    qr = np.zeros((2, NH * 512), np.float32)
    for h in range(NH):
        qr[0, h * 512:(h + 1) * 512] = -SLOPES[h] * lo
        qr[1, h * 512:(h + 1) * 512] = -SLOPES[h] * hi
    return c, qr


def make_par(b, c, ada_b, norm_mix_g, norm_ffn_g, lb_logits, hgrn_norm_g, qn_g, kn_g, lam, subln_g):
    par = np.zeros((128, NPAR), np.float32)
    par[:, 0:8] = c[b].reshape(8, 128).T
    for l in range(2):
        o = 8 + l * PL
        par[:, o:o + 48] = ada_b[l].reshape(48, 128).T
        par[:, o + 48:o + 56] = norm_mix_g[l].reshape(8, 128).T
        par[:, o + 56:o + 64] = norm_ffn_g[l].reshape(8, 128).T
        par[:, o + 64:o + 68] = lb_logits[0].reshape(4, 128).T
        par[:, o + 68:o + 72] = lb_logits[1].reshape(4, 128).T
        par[:, o + 72] = hgrn_norm_g[l]
        par[:, o + 73] = np.tile(qn_g[l], 2)
        par[:, o + 74] = np.tile(kn_g[l], 2)
        par[:, o + 75] = subln_g[l]
        par[:, o + 76:o + 332] = lam[l].reshape(1, 256)
    return par


_NC_CACHE = {}


def kernel(x, c, ada_w, ada_b, norm_mix_g, norm_ffn_g, w_in, hgrn_lb_logits, hgrn_norm_g,
           da_qnorm_g, da_knorm_g, da_lambda, da_subln_g, w_branch_a, w_branch_b, w_out,
           ffn_w1, ffn_w3, ffn_w2, moe_router, moe_w1, moe_w3, moe_w2):
    f = lambda a: np.ascontiguousarray(np.asarray(a, dtype=np.float32))
    x = f(x)
    B, S, _ = x.shape
    cst, qr = make_consts()
    if S not in _NC_CACHE:
        _NC_CACHE[S] = build(S)
    nc = _NC_CACHE[S]
    shared = dict(cst=cst, qrows=qr, ada_w=f(ada_w), w_in=f(w_in), w_pa=f(w_branch_a), w_pb=f(w_branch_b), w_o=f(w_out),
                  ffn_w1=f(ffn_w1), ffn_w3=f(ffn_w3), ffn_w2=f(ffn_w2), router=f(moe_router),
                  moe_w1=f(moe_w1), moe_w3=f(moe_w3), moe_w2=f(moe_w2))
    args = [f(a) for a in (c, ada_b, norm_mix_g, norm_ffn_g, hgrn_lb_logits, hgrn_norm_g, da_qnorm_g, da_knorm_g, da_lambda, da_subln_g)]
    in_maps = []
    for b in range(B):
        m = dict(shared)
        m["x"] = x[b]
        m["par"] = make_par(b, *args)
        in_maps.append(m)
    res = run_bass_kernel_spmd(nc, in_maps, core_ids=list(range(B)))
    return np.stack([np.asarray(r["out"], dtype=np.float32) for r in res.results], axis=0)
```
